# Optimizing a Trainium2 kernel written in Bass

```python
import math
import jax, jax.numpy as jnp
from jax import lax
import numpy as np

D_MODEL = 2048
BATCH = 2
SEQ = 8192
DEPTH = 2

N_MEM = 256
A_HEADS = 8
A_KV_HEADS = 2
A_HEAD_DIM = 128
IDX_HEADS = 4
IDX_DIM = 64
TOPK_MAX = 256
Q_BLOCK = 128
LRU_WIDTH = 1024
LRU_BLOCKS = 8
CONV_WIDTH = 4
LRU_C = 8.0
X_HEADS = 4
X_HEAD_DIM = 256
N_BRANCH = 3
BRANCH_WIDTH = 1024
D_FF = 5504
N_BUCKETS = 32
MAX_DISTANCE = 128
EPS = 1e-6

A_Q = A_HEADS * A_HEAD_DIM
A_KV = A_KV_HEADS * A_HEAD_DIM
SPLIT_SIZES = (A_Q, A_KV, A_KV, IDX_HEADS * IDX_DIM, IDX_DIM, IDX_HEADS,
               LRU_WIDTH, LRU_WIDTH, X_HEADS * X_HEAD_DIM, N_BRANCH * D_MODEL)
D_IN = sum(SPLIT_SIZES)

kernel_name = "hybrid_dsa_rglru_memory_macaron"


def rmsnorm(x, g):
    xf = x.astype(jnp.float32)
    y = xf * lax.rsqrt(jnp.mean(xf * xf, axis=-1, keepdims=True) + EPS)
    return (y * g.astype(jnp.float32)).astype(x.dtype)


def layernorm(x, g, b):
    xf = x.astype(jnp.float32)
    mu = jnp.mean(xf, axis=-1, keepdims=True)
    var = jnp.mean(jnp.square(xf - mu), axis=-1, keepdims=True)
    y = (xf - mu) * lax.rsqrt(var + EPS)
    return (y * g.astype(jnp.float32) + b.astype(jnp.float32)).astype(x.dtype)


def swiglu(x, w_gu, w_down):
    g, u = jnp.split(x @ w_gu, 2, axis=-1)
    return (jax.nn.silu(g) * u) @ w_down


def split_points():
    pts, acc = [], 0
    for n in SPLIT_SIZES[:-1]:
        acc += n
        pts.append(acc)
    return pts


def t5_bucket(dist):
    max_exact = N_BUCKETS // 2
    d = jnp.maximum(dist, 0)
    df = jnp.maximum(d, 1).astype(jnp.float32)
    large = max_exact + (jnp.log(df / max_exact) / math.log(MAX_DISTANCE / max_exact)
                         * (N_BUCKETS - max_exact)).astype(jnp.int32)
    large = jnp.minimum(large, N_BUCKETS - 1)
    return jnp.where(d < max_exact, d, large)


def dsa_attention(q, k, v, q_idx, k_idx, w_idx, rel_bias):
    b, s = q.shape[0], q.shape[1]
    topk = min(TOPK_MAX, s // 4)
    n_blocks = s // Q_BLOCK
    group = A_HEADS // A_KV_HEADS
    key_pos = jnp.arange(s, dtype=jnp.int32)
    gather = jax.vmap(lambda t, i: t[i])
    idx_scale = IDX_DIM ** -0.5
    w_scale = IDX_HEADS ** -0.5
    att_scale = A_HEAD_DIM ** -0.5

    def block(i):
        start = i * Q_BLOCK
        q_pos = start + jnp.arange(Q_BLOCK, dtype=jnp.int32)
        qb = lax.dynamic_slice_in_dim(q, start, Q_BLOCK, axis=1)
        qib = lax.dynamic_slice_in_dim(q_idx, start, Q_BLOCK, axis=1)
        wb = lax.dynamic_slice_in_dim(w_idx, start, Q_BLOCK, axis=1)
        dots = jnp.einsum('bqhd,bsd->bqhs', qib, k_idx).astype(jnp.float32) * idx_scale
        score = jnp.einsum('bqh,bqhs->bqs', wb.astype(jnp.float32) * w_scale, jax.nn.relu(dots))
        causal = key_pos[None, None, :] <= q_pos[None, :, None]
        score = jnp.where(causal, score, -jnp.inf)
        _, sel = lax.top_k(score, topk)
        kg = gather(k, sel)
        vg = gather(v, sel)
        qg = qb.reshape(b, Q_BLOCK, A_KV_HEADS, group, A_HEAD_DIM)
        logits = jnp.einsum('bqngd,bqknd->bqngk', qg, kg).astype(jnp.float32) * att_scale
        dist = q_pos[None, :, None] - sel
        bias = rel_bias[t5_bucket(dist)].astype(jnp.float32)
        bias = bias.reshape(b, Q_BLOCK, topk, A_KV_HEADS, group).transpose(0, 1, 3, 4, 2)
        valid = (dist >= 0)[:, :, None, None, :]
        logits = jnp.where(valid, logits + bias, -jnp.inf)
        p = jax.nn.softmax(logits, axis=-1).astype(v.dtype)
        out = jnp.einsum('bqngk,bqknd->bqngd', p, vg)
        return out.reshape(b, Q_BLOCK, A_Q)

    outs = lax.map(block, jnp.arange(n_blocks, dtype=jnp.int32))
    return outs.transpose(1, 0, 2, 3).reshape(b, s, A_Q)


def rglru_branch(xb, gate, conv_w, conv_b, w_a, b_a, w_i, b_i, lam):
    b, s, _ = xb.shape
    xp = jnp.pad(xb, ((0, 0), (CONV_WIDTH - 1, 0), (0, 0)))
    xc = conv_b + sum(xp[:, j:j + s] * conv_w[j] for j in range(CONV_WIDTH))
    xr = xc.reshape(b, s, LRU_BLOCKS, LRU_WIDTH // LRU_BLOCKS)
    r = jax.nn.sigmoid(jnp.einsum('bsnc,ncd->bsnd', xr, w_a).reshape(b, s, LRU_WIDTH) + b_a)
    i = jax.nn.sigmoid(jnp.einsum('bsnc,ncd->bsnd', xr, w_i).reshape(b, s, LRU_WIDTH) + b_i)
    log_a = -LRU_C * r.astype(jnp.float32) * jax.nn.softplus(-lam.astype(jnp.float32))
    a = jnp.exp(log_a)
    mult = jnp.sqrt(jnp.maximum(-jnp.expm1(2.0 * log_a), 0.0))
    u = mult * (i * xc).astype(jnp.float32)

    def combine(left, right):
        a1, b1 = left
        a2, b2 = right
        return a1 * a2, a2 * b1 + b2

    _, h = lax.associative_scan(combine, (a, u), axis=1)
    return h.astype(xb.dtype) * jax.nn.gelu(gate)


def memory_attention(q, mem_k, mem_v):
    b, s = q.shape[0], q.shape[1]
    logits = jnp.einsum('bshd,bmhd->bhsm', q, mem_k).astype(jnp.float32) * (X_HEAD_DIM ** -0.5)
    p = jax.nn.softmax(logits, axis=-1).astype(q.dtype)
    return jnp.einsum('bhsm,bmhd->bshd', p, mem_v).reshape(b, s, X_HEADS * X_HEAD_DIM)


def hybrid_layer(x, mem, rel_bias, norm_ff1, w_ff1_gu, w_ff1_down, norm_mix, w_in,
                 conv_w, conv_b, w_a, b_a, w_i, b_i, lam, idx_ln_g, idx_ln_b,
                 mem_norm, w_mem_kv, w_branch, w_out, norm_ff2, w_ff2_gu, w_ff2_down):
    b, s, _ = x.shape
    x = x + 0.5 * swiglu(rmsnorm(x, norm_ff1), w_ff1_gu, w_ff1_down)
    h = rmsnorm(x, norm_mix)
    proj = h @ w_in
    (q_a, k_a, v_a, q_idx, k_idx, w_idx, x_lru, g_lru, q_mem, gates) = jnp.split(proj, split_points(), axis=-1)
    y_a = dsa_attention(q_a.reshape(b, s, A_HEADS, A_HEAD_DIM),
                        k_a.reshape(b, s, A_KV_HEADS, A_HEAD_DIM),
                        v_a.reshape(b, s, A_KV_HEADS, A_HEAD_DIM),
                        q_idx.reshape(b, s, IDX_HEADS, IDX_DIM),
                        layernorm(k_idx, idx_ln_g, idx_ln_b), w_idx, rel_bias)
    y_b = rglru_branch(x_lru, g_lru, conv_w, conv_b, w_a, b_a, w_i, b_i, lam)
    mk, mv = jnp.split(rmsnorm(mem, mem_norm) @ w_mem_kv, 2, axis=-1)
    m = mem.shape[1]
    y_c = memory_attention(q_mem.reshape(b, s, X_HEADS, X_HEAD_DIM),
                           mk.reshape(b, m, X_HEADS, X_HEAD_DIM),
                           mv.reshape(b, m, X_HEADS, X_HEAD_DIM))
    branches = jnp.stack([y_a, y_b, y_c], axis=2)
    branch_d = jnp.einsum('bsjc,jcd->bsjd', branches, w_branch)
    g = jax.nn.sigmoid(gates.reshape(b, s, N_BRANCH, D_MODEL))
    merged = jnp.einsum('bsjd,bsjd->bsd', g, branch_d)
    x = x + merged @ w_out
    x = x + 0.5 * swiglu(rmsnorm(x, norm_ff2), w_ff2_gu, w_ff2_down)
    return x


def setup_inputs(seed: int = 0) -> dict:
    key = jax.random.key(seed)
    ks = jax.random.split(key, 32)
    f32 = jnp.float32
    nrm = lambda k, shape, scale: jax.random.normal(k, shape, f32) * scale
    gain = lambda k, shape: 1.0 + 0.02 * jax.random.normal(k, shape, f32)
    L = DEPTH
    base = jnp.sqrt(jax.random.uniform(ks[12], (L, LRU_WIDTH), f32, 0.81, 0.998))
    lam = jnp.log(base) - jnp.log1p(-base)
    bw = LRU_WIDTH // LRU_BLOCKS
    return {
        "x": nrm(ks[0], (BATCH, SEQ, D_MODEL), 1.0),
        "mem": nrm(ks[1], (BATCH, N_MEM, D_MODEL), 1.0),
        "rel_bias": nrm(ks[2], (N_BUCKETS, A_HEADS), 0.5),
        "final_norm": gain(ks[3], (D_MODEL,)),
        "norm_ff1": gain(ks[4], (L, D_MODEL)),
        "w_ff1_gu": nrm(ks[5], (L, D_MODEL, 2 * D_FF), D_MODEL ** -0.5),
        "w_ff1_down": nrm(ks[6], (L, D_FF, D_MODEL), D_FF ** -0.5),
        "norm_mix": gain(ks[7], (L, D_MODEL)),
        "w_in": nrm(ks[8], (L, D_MODEL, D_IN), D_MODEL ** -0.5),
        "conv_w": nrm(ks[9], (L, CONV_WIDTH, LRU_WIDTH), CONV_WIDTH ** -0.5),
        "conv_b": nrm(ks[10], (L, LRU_WIDTH), 0.01),
        "w_a": nrm(ks[11], (L, LRU_BLOCKS, bw, bw), bw ** -0.5),
        "b_a": nrm(ks[13], (L, LRU_WIDTH), 0.01),
        "w_i": nrm(ks[14], (L, LRU_BLOCKS, bw, bw), bw ** -0.5),
        "b_i": nrm(ks[15], (L, LRU_WIDTH), 0.01),
        "lam": lam,
        "idx_ln_g": gain(ks[16], (L, IDX_DIM)),
        "idx_ln_b": nrm(ks[17], (L, IDX_DIM), 0.01),
        "mem_norm": gain(ks[18], (L, D_MODEL)),
        "w_mem_kv": nrm(ks[19], (L, D_MODEL, 2 * X_HEADS * X_HEAD_DIM), D_MODEL ** -0.5),
        "w_branch": nrm(ks[20], (L, N_BRANCH, BRANCH_WIDTH, D_MODEL), BRANCH_WIDTH ** -0.5),
        "w_out": nrm(ks[21], (L, D_MODEL, D_MODEL), D_MODEL ** -0.5),
        "norm_ff2": gain(ks[22], (L, D_MODEL)),
        "w_ff2_gu": nrm(ks[23], (L, D_MODEL, 2 * D_FF), D_MODEL ** -0.5),
        "w_ff2_down": nrm(ks[24], (L, D_FF, D_MODEL), D_FF ** -0.5),
    }


def reference(x, mem, rel_bias, final_norm, norm_ff1, w_ff1_gu, w_ff1_down, norm_mix, w_in,
              conv_w, conv_b, w_a, b_a, w_i, b_i, lam, idx_ln_g, idx_ln_b, mem_norm,
              w_mem_kv, w_branch, w_out, norm_ff2, w_ff2_gu, w_ff2_down):
    for l in range(DEPTH):
        x = hybrid_layer(x, mem, rel_bias,
                         norm_ff1[l], w_ff1_gu[l], w_ff1_down[l], norm_mix[l], w_in[l],
                         conv_w[l], conv_b[l], w_a[l], b_a[l], w_i[l], b_i[l], lam[l],
                         idx_ln_g[l], idx_ln_b[l], mem_norm[l], w_mem_kv[l],
                         w_branch[l], w_out[l], norm_ff2[l], w_ff2_gu[l], w_ff2_down[l])
    return rmsnorm(x, final_norm)
```

```python
import math
import os
import numpy as np
import concourse.bass as bass
import concourse.mybir as mybir
from concourse.bass_utils import run_bass_kernel_spmd

F32 = mybir.dt.float32
F32R = mybir.dt.float32r
BF16 = mybir.dt.bfloat16
AF = mybir.ActivationFunctionType
ALU = mybir.AluOpType
AX = mybir.AxisListType

NCORES = 8
D = 2048
KC = 16
DFF = 5504
FC = 43
EPS = 1e-6
S = 8192
NT = 2048
TT = 512
NTT = NT // TT
DEPTH = 2
NMEM = 256
TOPK = 256
NEG = -1.0e30
BISECT = 18

CH_QA, CH_KA, CH_VA, CH_QI, CH_KI, CH_WI, CH_XL, CH_GL, CH_QM, CH_GT = 0, 8, 10, 12, 14, 15, 16, 24, 32, 40
NCH = 88
R_GU1, R_WIN, R_MKV, R_WBR, R_OUT, R_GU2 = 0, 5504, 5504 + 5632, 5504 + 5632 + 1024, 5504 + 5632 + 1024 + 1536, 5504 + 5632 + 1024 + 1536 + 1024
R4 = R_GU2 + 5504
R2 = 2 * 5504
SEC4 = [(R_GU1, 5504), (R_WIN, 5632), (R_MKV, 1024), (R_WBR, 1536), (R_OUT, 1024), (R_GU2, 5504)]
SEC2 = [(0, 5504), (5504, 5504)]


class Buf:
    def __init__(self, name, t=None):
        self.name = name
        self.t = t
        self.w = None
        self.r = {}
        self.lsem = None
        self.ltot = 0
        self.ssem = None
        self.stot = 0

    def __getitem__(self, k):
        return self.t[k]


class B:
    def __init__(self, nc):
        self.nc = nc
        self.E = {'pe': nc.tensor, 'dve': nc.vector, 'act': nc.scalar, 'pool': nc.gpsimd, 'sp': nc.sync}
        self.sem = {k: nc.alloc_semaphore('c_' + k) for k in self.E}
        self.cnt = {k: 0 for k in self.E}
        self.seen = {k: {} for k in self.E}
        self.dsems = {}
        self.free_dsems = []
        self.csems = []
        self.ninst = 0
        self.nwait = 0
        self.uid = 0

    def view(self, name, ap):
        self.uid += 1
        return Buf('%s_%d' % (name, self.uid), ap)

    def ps(self, name, shape=(128, 512), dtype=F32):
        return Buf(name, self.nc.alloc_psum_tensor(name, list(shape), dtype))

    def dram(self, name):
        return Buf(name, None)

    def _wait(self, e, toks):
        best = {}
        for t in toks:
            n = t[2]
            if n not in best or best[n][1] < t[1]:
                best[n] = t
        se = self.seen[e]
        for t in sorted(best.values(), key=lambda t: -len(t[3])):
            s, v, n, snap = t
            if se.get(n, 0) < v:
                self.E[e].wait_ge(s, v)
                se[n] = v
                self.ninst += 1
                self.nwait += 1
            for k, kv in snap.items():
                if se.get(k, 0) < kv:
                    se[k] = kv

    def _deps(self, e, reads, writes):
        toks = []
        for b in reads:
            if b.w is not None:
                toks.append(b.w)
        for b in writes:
            if b.w is not None:
                toks.append(b.w)
            toks.extend(b.r.values())
        if e == 'pe':
            toks = [t for t in toks if t[2] != 'c_pe']
        return toks

    def _commit(self, tok, reads, writes):
        for b in writes:
            b.w = tok
            b.r = {}
        for b in reads:
            if b in writes:
                continue
            b.r[tok[2]] = tok

    def op(self, e, fn, reads=(), writes=()):
        self._wait(e, self._deps(e, reads, writes))
        ins = fn(self.E[e])
        self.cnt[e] += 1
        ins.then_inc(self.sem[e], 1)
        self.ninst += 1
        snap = dict(self.seen[e])
        snap['c_' + e] = self.cnt[e]
        self._commit((self.sem[e], self.cnt[e], 'c_' + e, snap), reads, writes)
        return ins

    def _dsem(self, key):
        if key not in self.dsems:
            if self.free_dsems:
                ent = self.free_dsems.pop()
            else:
                ent = [self.nc.alloc_semaphore('d%d' % len(self.dsems)), 0, 'd%d' % len(self.dsems)]
            self.dsems[key] = ent
        return self.dsems[key]

    def dma(self, q, out, in_, reads=(), writes=(), owner=None, **kw):
        if owner is None:
            owner = [b for b in list(writes) + list(reads) if b.t is not None][0]
        kind = 'l' if owner in writes else 's'
        self._wait(q, self._deps(q, reads, writes))
        ins = self.E[q].dma_start(out=out, in_=in_, **kw)
        ent = self._dsem(kind + owner.name)
        ent[1] += 16
        ins.then_inc(ent[0], 16)
        self.ninst += 1
        self._commit((ent[0], ent[1], ent[2], dict(self.seen[q])), reads, writes)
        return ins

    def custom(self, e, ins_fn, sem_inc, reads=(), writes=()):
        self._wait(e, self._deps(e, reads, writes))
        ins = ins_fn(self.E[e])
        self.uid += 1
        nm = 'cc%d' % self.uid
        ent = [self.nc.alloc_semaphore(nm), sem_inc, nm]
        self.csems.append(ent)
        ins.then_inc(ent[0], sem_inc)
        self.ninst += 1
        self._commit((ent[0], ent[1], ent[2], dict(self.seen[e])), reads, writes)
        return ins

    def drain(self, e='sp'):
        toks = [(v[0], v[1], v[2], {}) for v in list(self.dsems.values()) + self.csems if v[1]]
        for k in self.E:
            if self.cnt[k]:
                toks.append((self.sem[k], self.cnt[k], 'c_' + k, {}))
        self._wait(e, toks)

    def barrier(self):
        self.drain('sp')
        self.nc.all_engine_barrier()
        for e in self.E:
            self.seen[e] = dict(self.seen['sp'])
        for k in list(self.dsems.keys()):
            self.free_dsems.append(self.dsems.pop(k))


class Arena:
    def __init__(self, b, nc, nwords, name):
        self.b = b
        self.nc = nc
        self.n = nwords
        nc.alloc_sbuf_tensor(name, [128, nwords], F32)
        self.base = nc.sbuf_base - nwords * 4
        self.off = 0

    def reset(self):
        self.off = 0

    def take(self, name, words, shape=None, dtype=F32, parts=128):
        words = (words + 7) // 8 * 8
        assert self.off + words <= self.n, (name, self.off, words, self.n)
        nel = words * (2 if dtype == BF16 else 1)
        if shape is None:
            shp = [parts, nel]
        else:
            shp = [parts] + list(shape[1:])
            assert int(np.prod(shp[1:])) <= nel, (name, shp, nel)
        self.b.uid += 1
        t = self.nc.alloc_sbuf_tensor_at('%s_%d' % (name, self.b.uid), shp, dtype, offset=self.base + self.off * 4)
        self.off += words
        return Buf('%s_%d' % (name, self.b.uid), t)


def r32(ap):
    return ap.bitcast(F32R)


class WSplit:
    def __init__(self, secs):
        self.secs = secs

    def __getitem__(self, key):
        rs, cs = key
        for (r0, rn, t) in self.secs:
            if r0 <= rs.start and rs.stop <= r0 + rn:
                return t[rs.start - r0:rs.stop - r0, cs]
        raise KeyError(key)


class Prog:
    def __init__(self, nlayers=DEPTH, debug=()):
        self.debug = set(debug)
        nc = bass.Bass("TRN2", target_bir_lowering=False)
        nc.dge_precook = False
        self.nc = nc
        self.nl = nlayers
        dt = nc.dram_tensor

        def ext_in(name, shape, dtype=F32):
            return dt(name, list(shape), dtype, kind="ExternalInput").ap()

        def ext_out(name, shape, dtype=F32):
            return dt(name, list(shape), dtype, kind="ExternalOutput").ap()

        def internal(name, shape, dtype=F32):
            return dt(name, list(shape), dtype)

        self.xin = ext_in("xin", [D, NT])
        self.memT = ext_in("memT", [D, NMEM])
        self.w4s = ext_in("w4s", [nlayers, R4 // 8, 4096])
        self.w2s = ext_in("w2s", [nlayers, R2 // 8, 2048])
        self.vec16 = ext_in("vec16", [nlayers, 128, 64])
        self.fnorm = ext_in("fnorm", [128, 16])
        self.lrup = ext_in("lrup", [nlayers, 128, 64])
        self.wai = ext_in("wai", [nlayers, 128, 2048])
        self.lnp = ext_in("lnp", [nlayers, 64, 2])
        self.mtab = ext_in("mtab", [128, 8 * 2 * 128])
        self.b31 = ext_in("b31", [1, 8])
        self.qposr = ext_in("qposr", [1, NT])
        self.qposc = ext_in("qposc", [128, 16])
        self.sel = ext_in("sel", [1, 8])
        self.yout = ext_out("yout", [D, NT])
        self.xout = ext_out("xout", [D, NT]) if nlayers == 1 else None
        self.W4 = [WSplit([(r0, rn, internal("W4_%d_%d" % (l, r0), [rn, 4096])) for (r0, rn) in SEC4]) for l in range(nlayers)]
        self.W2 = [WSplit([(r0, rn, internal("W2_%d_%d" % (l, r0), [rn, 2048])) for (r0, rn) in SEC2]) for l in range(nlayers)]
        self.w4b = internal("w4b", [nlayers, R4 // 8, 4096])
        self.w2b = internal("w2b", [nlayers, R2 // 8, 2048])
        self.xres = internal("xres", [D, NT]).ap()
        self.qaT = internal("qaT", [1024, NT], BF16).ap()
        self.kT_own = internal("kT_own", [256, NT], BF16)
        self.kT_all = internal("kT_all", [4 * 256, NT], BF16)
        self.v_own = internal("v_own", [NT, 256], BF16)
        self.v_all = internal("v_all", [4 * NT, 256], BF16)
        self.ki_own = internal("ki_own", [64, NT])
        self.ki_all = internal("ki_all", [4 * 64, NT])
        self.tail_own = internal("tail_own", [128, 32])
        self.tail_all = internal("tail_all", [4 * 128, 32])
        self.ends_own = internal("ends_own", [128, 16])
        self.ends_all = internal("ends_all", [4 * 128, 16])
        self.qiT = internal("qiT", [256, NT]).ap()
        self.ws = internal("ws", [NT, 4]).ap()
        self.xlT = internal("xlT", [1024, NT]).ap()
        self.glT = internal("glT", [1024, NT]).ap()
        self.qmT = internal("qmT", [1024, NT], BF16).ap()
        self.gtT = internal("gtT", [6144, NT]).ap()
        self.hlT = internal("hlT", [1024, NT]).ap()
        self.pcT = internal("pcT", [1024, NT]).ap()
        self.yaT = internal("yaT", [1024, NT]).ap()
        self.ybT = internal("ybT", [1024, NT]).ap()
        self.ycT = internal("ycT", [1024, NT]).ap()
        self.mskT = internal("mskT", [64, 128, NT], BF16).ap()
        self.dbg = {}
        for name, shape in (("dbg_x1", [D, NT]), ("dbg_ki", [256, NT]), ("dbg_ya", [1024, NT]), ("dbg_yb", [1024, NT]),
                            ("dbg_yc", [1024, NT]), ("dbg_x2", [D, NT])):
            if name in self.debug:
                self.dbg[name] = ext_out(name, shape)

        with nc.cleanup_on_exit():
            self.b = B(nc)
            self.build()
            self.b.barrier()

    def build(self):
        b, nc = self.b, self.nc
        ARW = 49664
        self.ar = Arena(b, nc, ARW, "arena")
        self.pa = Arena(b, nc, 3072, "persist")
        pa = self.pa
        self.ps = [b.ps('ps%d' % i) for i in range(8)]
        self.ones0 = pa.take('ones0', 128)
        self.ones = pa.take('ones', 128)
        self.epsb = pa.take('epsb', 8)
        self.zero = pa.take('zero', 512)
        self.iota = pa.take('iota', 512)
        self.pidx = pa.take('pidx', 8)
        self.qpc = pa.take('qpc', 16)
        self.selt = pa.take('selt', 8)
        self.b31t = pa.take('b31t', 8)
        self.fng = pa.take('fng', 16)
        self.v16 = pa.take('v16', 64)
        self.lrp = pa.take('lrp', 64)
        self.clru = pa.take('clru', 8)
        self.lnpt = pa.take('lnpt', 8, parts=64)
        self.small = pa.take('small', 256)
        self.ident = pa.take('ident', 64, shape=None, dtype=BF16)
        self.identf = pa.take('identf', 128)
        b.op('dve', lambda e: e.memset(self.ones0[:], 1.0), writes=[self.ones0])
        b.op('dve', lambda e: e.tensor_copy(out=r32(self.ones[:]), in_=self.ones0[:]), reads=[self.ones0], writes=[self.ones])
        b.op('dve', lambda e: e.memset(self.epsb[:], EPS), writes=[self.epsb])
        b.op('dve', lambda e: e.memset(self.zero[:], 0.0), writes=[self.zero])
        b.op('pool', lambda e: e.iota(self.iota[:], pattern=[[1, 512]], base=0, channel_multiplier=0,
                                      allow_small_or_imprecise_dtypes=True), writes=[self.iota])
        b.op('pool', lambda e: e.iota(self.pidx[:, 0:1], pattern=[[0, 1]], base=0, channel_multiplier=1,
                                      allow_small_or_imprecise_dtypes=True), writes=[self.pidx])
        b.op('dve', lambda e: e.tensor_scalar(out=self.identf[:], in0=self.iota[:, 0:128], scalar1=self.pidx[:, 0:1],
                                              scalar2=None, op0=ALU.is_equal), reads=[self.iota, self.pidx], writes=[self.identf])
        b.op('dve', lambda e: e.tensor_copy(out=self.ident[:], in_=self.identf[:]), reads=[self.identf], writes=[self.ident])
        b.dma('pool', self.qpc[:], self.qposc, writes=[self.qpc])
        b.dma('pool', self.selt[:], self.sel[0].partition_broadcast(128), writes=[self.selt])
        b.dma('pool', self.b31t[:], self.b31[0].partition_broadcast(128), writes=[self.b31t])
        b.dma('pool', self.fng[:], self.fnorm, writes=[self.fng])

        self.Wd = b.dram('Wd')
        g8 = [list(range(NCORES))]
        wtmp = b.dram('wtmp')
        for l in range(self.nl):
            for (src, dst, nr) in ((self.w4s, self.w4b, R4 // 8), (self.w2s, self.w2b, R2 // 8)):
                for r0 in range(0, nr, 256):
                    r1 = min(nr, r0 + 256)
                    b.dma('sp', dst[l, r0:r1, :], src[l, r0:r1, :], reads=[], writes=[wtmp], owner=self.small)
        b.barrier()
        for l in range(self.nl):
            for which, secs in ((4, SEC4), (2, SEC2)):
                for (r0, rn) in secs:
                    src = (self.w4b if which == 4 else self.w2b)
                    dst = (self.W4[l] if which == 4 else self.W2[l])
                    i_ap = src[l, r0 // 8:(r0 + rn) // 8, :]
                    o_ap = dst[r0:r0 + rn, :]
                    b.custom('pool', lambda e, i_ap=i_ap, o_ap=o_ap: e.collective_compute(
                        "AllGather", ALU.bypass, replica_groups=g8, ins=[i_ap.opt()], outs=[o_ap.opt()]), 1,
                        reads=[wtmp], writes=[self.Wd])
        b.barrier()

        for l in range(self.nl):
            self.layer(l)
        self.final_norm()

    def rmsnorm_T(self, xt, hT, g_ap, gbuf, n=TT):
        b = self.b
        ps_s = self.ps[6]
        b.op('act', lambda e: e.activation(out=r32(hT[:, :, 0:n]), in_=xt[:, :, 0:n], func=AF.Square), reads=[xt], writes=[hT])
        for k in range(KC):
            b.op('pe', lambda e, k=k: e.matmul(ps_s[:, 0:n], r32(self.ones[:]), r32(hT[:, k, 0:n]), start=(k == 0), stop=(k == KC - 1)),
                 reads=[self.ones, hT], writes=[ps_s])
        b.op('act', lambda e: e.activation(out=self.rs[:, 0:n], in_=ps_s[:, 0:n], func=AF.Sqrt, bias=self.epsb[:, 0:1], scale=1.0 / D),
             reads=[ps_s, self.epsb], writes=[self.rs])
        b.op('dve', lambda e: e.reciprocal(out=self.rstd[:, 0:n], in_=self.rs[:, 0:n]), reads=[self.rs], writes=[self.rstd])
        for k in range(KC):
            b.op('dve', lambda e, k=k: e.scalar_tensor_tensor(out=r32(hT[:, k, 0:n]), in0=xt[:, k, 0:n], scalar=g_ap[:, k:k + 1],
                                                              in1=self.rstd[:, 0:n], op0=ALU.mult, op1=ALU.mult),
                 reads=[xt, gbuf, self.rstd], writes=[hT])

    def wload(self, wb, W, row0, nrows=128, width=4096, q='sp'):
        self.b.dma(q, r32(wb[:, 0:width]), r32(W[row0:row0 + nrows, :]), reads=[self.Wd], writes=[wb])

    def ffn_tile(self, xt, hT, l, which):
        b = self.b
        W4, W2 = self.W4[l], self.W2[l]
        rg = R_GU1 if which == 1 else R_GU2
        rd = 0 if which == 1 else 5504
        G = 4
        groups = [list(range(s, min(s + G, FC))) for s in range(0, FC, G)]
        it = 0
        for gi, grp in enumerate(groups):
            ag = self.actg[gi % 2]
            wds = []
            for jj, j in enumerate(grp):
                wb = self.wb[self.wbi % 3]
                self.wbi += 1
                self.wload(wb, W4, rg + j * 128)
                wd = self.wd[self.wdi % 6]
                self.wdi += 1
                self.wload(wd, W2, rd + j * 128, width=2048)
                wds.append(wd)
                pg, pu = self.ps[(it % 2) * 2], self.ps[(it % 2) * 2 + 1]
                wv = wb[:].rearrange("p (k c) -> p k c", k=KC)
                for k in range(KC):
                    b.op('pe', lambda e, k=k: e.matmul(pg[:], r32(wv[:, k, 0:128]), r32(hT[:, k, :]), start=(k == 0), stop=(k == KC - 1)),
                         reads=[wb, hT], writes=[pg])
                for k in range(KC):
                    b.op('pe', lambda e, k=k: e.matmul(pu[:], r32(wv[:, k, 128:256]), r32(hT[:, k, :]), start=(k == 0), stop=(k == KC - 1)),
                         reads=[wb, hT], writes=[pu])
                sg = self.sg[it % 2]
                b.op('act', lambda e: e.activation(out=sg[:], in_=pg[:], func=AF.Silu), reads=[pg], writes=[sg])
                b.op('dve', lambda e, jj=jj: e.tensor_tensor(out=r32(ag[:, jj, :]), in0=sg[:], in1=pu[:], op=ALU.mult),
                     reads=[sg, pu], writes=[ag])
                it += 1
            for d in range(KC):
                pd = self.ps[4 + d % 2]
                for jj in range(len(grp)):
                    b.op('pe', lambda e, jj=jj, d=d: e.matmul(pd[:], r32(wds[jj][:, d * 128:(d + 1) * 128]), r32(ag[:, jj, :]),
                                                              start=(jj == 0), stop=(jj == len(grp) - 1)),
                         reads=[wds[jj], ag], writes=[pd])
                b.op('dve', lambda e, d=d: e.scalar_tensor_tensor(out=xt[:, d, :], in0=pd[:], scalar=0.5, in1=xt[:, d, :],
                                                                  op0=ALU.mult, op1=ALU.add),
                     reads=[pd, xt], writes=[xt])

    def ffn_bufs(self):
        ar = self.ar
        ar.reset()
        self.xt = ar.take('xt', KC * TT, (128, KC, TT))
        self.hT = ar.take('hT', KC * TT, (128, KC, TT))
        self.wb = [ar.take('wb%d' % i, 4096) for i in range(3)]
        self.wd = [ar.take('wd%d' % i, 2048) for i in range(6)]
        self.actg = [ar.take('ag%d' % i, 4 * TT, (128, 4, TT)) for i in range(2)]
        self.sg = [ar.take('sg%d' % i, TT) for i in range(2)]
        self.stage = [ar.take('st%d' % i, TT) for i in range(3)]
        self.rs = ar.take('rs', TT)
        self.rstd = ar.take('rstd', TT)
        self.kxr = ar.take('kxr', TT)
        self.sqr = ar.take('sqr', TT)
        self.wbi = 0
        self.wdi = 0

    def xview(self, ap, tt):
        return ap.rearrange("(k p) t -> p k t", p=128)[:, :, tt * TT:(tt + 1) * TT]

    def layer(self, l):
        b = self.b
        b.dma('pool', self.v16[:], self.vec16[l], writes=[self.v16])
        b.dma('pool', self.lrp[:], self.lrup[l], writes=[self.lrp])
        b.dma('pool', self.lnpt[:, 0:2], self.lnp[l], writes=[self.lnpt])
        self.phase_a(l)
        b.barrier()
        self.exchange1()
        b.barrier()
        self.phase_lru1(l)
        b.barrier()
        self.exchange2()
        b.barrier()
        self.phase_lru2(l)
        b.barrier()
        self.phase_sel(l)
        b.barrier()
        self.phase_attn(l)
        b.barrier()
        self.phase_mem(l)
        b.barrier()
        self.phase_merge(l)
        b.barrier()

    def phase_a(self, l):
        b = self.b
        self.ffn_bufs()
        xsrc = self.xin if l == 0 else self.xres
        xd = b.dram('xd')
        pd_ = b.dram('projd')
        xt, hT = self.xt, self.hT
        lng, lnb = self.lnpt[:, 0:1], self.lnpt[:, 1:2]
        W4 = self.W4[l]
        att_scale = 128 ** -0.5
        for tt in range(NTT):
            tsl = slice(tt * TT, (tt + 1) * TT)
            b.dma('pool', xt[:], self.xview(xsrc, tt), reads=[xd], writes=[xt])
            self.rmsnorm_T(xt, hT, self.v16[:, 0:16], self.v16)
            self.ffn_tile(xt, hT, l, 1)
            b.dma('pool', self.xview(self.xres, tt), xt[:], reads=[xt], writes=[xd])
            if l == 0 and 'dbg_x1' in self.dbg:
                b.dma('pool', self.xview(self.dbg['dbg_x1'], tt), xt[:], reads=[xt], writes=[xd])
            self.rmsnorm_T(xt, hT, self.v16[:, 16:32], self.v16)
            si = 0
            for ti in range(NCH // 2):
                wb = self.wb[self.wbi % 3]
                self.wbi += 1
                self.wload(wb, W4, R_WIN + ti * 128)
                wv = wb[:].rearrange("p (k c) -> p k c", k=KC)
                for h in range(2):
                    ch = ti * 2 + h
                    if CH_VA <= ch < CH_VA + 2 or ch == CH_WI:
                        if ch == CH_VA + 1:
                            continue
                        ncol = 256 if ch == CH_VA else 4
                        c0 = 0 if ch == CH_VA else 128
                        for ts in range(TT // 128):
                            pp = self.ps[(ts % 2) * 2]
                            for k in range(KC):
                                b.op('pe', lambda e, k=k, ts=ts: e.matmul(pp[:, 0:ncol], r32(hT[:, k, ts * 128:(ts + 1) * 128]),
                                                                          r32(wv[:, k, c0:c0 + ncol]), start=(k == 0), stop=(k == KC - 1)),
                                     reads=[wb, hT], writes=[pp])
                            st = self.stage[si % 3]
                            si += 1
                            r0 = tt * TT + ts * 128
                            if ch == CH_VA:
                                stb = st[:].bitcast(BF16)
                                b.op('act', lambda e: e.copy(out=stb[:, 0:256], in_=pp[:, 0:256]), reads=[pp], writes=[st])
                                b.dma('pool', self.v_own[r0:r0 + 128, :], stb[:, 0:256], reads=[st], writes=[pd_])
                            else:
                                b.op('act', lambda e: e.activation(out=st[:, 0:4], in_=pp[:, 0:4], func=AF.Copy, scale=0.5 * 0.125),
                                     reads=[pp], writes=[st])
                                b.dma('pool', self.ws[r0:r0 + 128, :], st[:, 0:4], reads=[st], writes=[pd_])
                        continue
                    pp = self.ps[(ch % 2) * 2]
                    for k in range(KC):
                        b.op('pe', lambda e, k=k, h=h: e.matmul(pp[:], r32(wv[:, k, h * 128:(h + 1) * 128]), r32(hT[:, k, :]),
                                                                start=(k == 0), stop=(k == KC - 1)),
                             reads=[wb, hT], writes=[pp])
                    st = self.stage[si % 3]
                    si += 1
                    if ch < CH_KA:
                        stb = st[:].bitcast(BF16)
                        b.op('act', lambda e: e.activation(out=stb[:, 0:TT], in_=pp[:], func=AF.Copy, scale=att_scale), reads=[pp], writes=[st])
                        b.dma('pool', self.qaT[ch * 128:(ch + 1) * 128, tsl], stb[:, 0:TT], reads=[st], writes=[pd_])
                    elif ch < CH_VA:
                        stb = st[:].bitcast(BF16)
                        b.op('act', lambda e: e.copy(out=stb[:, 0:TT], in_=pp[:]), reads=[pp], writes=[st])
                        c = ch - CH_KA
                        b.dma('pool', self.kT_own[c * 128:(c + 1) * 128, tsl], stb[:, 0:TT], reads=[st], writes=[pd_])
                    elif ch < CH_KI:
                        b.op('act', lambda e: e.copy(out=st[:], in_=pp[:]), reads=[pp], writes=[st])
                        c = ch - CH_QI
                        b.dma('pool', self.qiT[c * 128:(c + 1) * 128, tsl], st[:], reads=[st], writes=[pd_])
                    elif ch == CH_KI:
                        kx, xc_, sq_, kr_, sr_ = self.stage[0], self.stage[1], self.stage[2], self.kxr, self.sqr
                        si = 0
                        p2 = self.ps[6]
                        b.op('act', lambda e: e.copy(out=r32(kr_[0:64, :]), in_=pp[0:64, :]), reads=[pp], writes=[kr_])
                        b.op('act', lambda e: e.copy(out=kx[0:64, :], in_=pp[0:64, :]), reads=[pp], writes=[kx])
                        b.op('pe', lambda e: e.matmul(p2[0:64, :], r32(self.ones[0:64, 0:64]), r32(kr_[0:64, :]), start=True, stop=True),
                             reads=[self.ones, kr_], writes=[p2])
                        b.op('dve', lambda e: e.scalar_tensor_tensor(out=xc_[0:64, :], in0=p2[0:64, :], scalar=-1.0 / 64, in1=kx[0:64, :],
                                                                     op0=ALU.mult, op1=ALU.add), reads=[p2, kx], writes=[xc_])
                        b.op('act', lambda e: e.activation(out=r32(sr_[0:64, :]), in_=xc_[0:64, :], func=AF.Square), reads=[xc_], writes=[sr_])
                        b.op('pe', lambda e: e.matmul(p2[0:64, :], r32(self.ones[0:64, 0:64]), r32(sr_[0:64, :]), start=True, stop=True),
                             reads=[self.ones, sr_], writes=[p2])
                        b.op('act', lambda e: e.activation(out=sq_[0:64, :], in_=p2[0:64, :], func=AF.Sqrt, bias=self.epsb[0:64, 0:1], scale=1.0 / 64),
                             reads=[p2, self.epsb], writes=[sq_])
                        b.op('dve', lambda e: e.reciprocal(out=kx[0:64, :], in_=sq_[0:64, :]), reads=[sq_], writes=[kx])
                        b.op('dve', lambda e: e.tensor_tensor(out=xc_[0:64, :], in0=xc_[0:64, :], in1=kx[0:64, :], op=ALU.mult),
                             reads=[xc_, kx], writes=[xc_])
                        b.op('dve', lambda e: e.tensor_scalar(out=xc_[0:64, :], in0=xc_[0:64, :], scalar1=lng[0:64, :], scalar2=lnb[0:64, :],
                                                              op0=ALU.mult, op1=ALU.add), reads=[xc_, self.lnpt], writes=[xc_])
                        b.dma('pool', self.ki_own[0:64, tsl], xc_[0:64, :], reads=[xc_], writes=[pd_])
                        if l == 0 and 'dbg_ki' in self.dbg:
                            b.dma('pool', self.dbg['dbg_ki'][0:64, tsl], xc_[0:64, :], reads=[xc_], writes=[pd_])
                    elif ch < CH_GL:
                        b.op('act', lambda e: e.copy(out=st[:], in_=pp[:]), reads=[pp], writes=[st])
                        c = ch - CH_XL
                        b.dma('pool', self.xlT[c * 128:(c + 1) * 128, tsl], st[:], reads=[st], writes=[pd_])
                        if tt == NTT - 1:
                            b.dma('pool', self.tail_own[:, c * 4:(c + 1) * 4], st[:, TT - 4:TT], reads=[st], writes=[pd_])
                    elif ch < CH_QM:
                        b.op('act', lambda e: e.copy(out=st[:], in_=pp[:]), reads=[pp], writes=[st])
                        c = ch - CH_GL
                        b.dma('pool', self.glT[c * 128:(c + 1) * 128, tsl], st[:], reads=[st], writes=[pd_])
                    elif ch < CH_GT:
                        stb = st[:].bitcast(BF16)
                        b.op('act', lambda e: e.activation(out=stb[:, 0:TT], in_=pp[:], func=AF.Copy, scale=1.0 / 16), reads=[pp], writes=[st])
                        c = ch - CH_QM
                        b.dma('pool', self.qmT[c * 128:(c + 1) * 128, tsl], stb[:, 0:TT], reads=[st], writes=[pd_])
                    else:
                        b.op('act', lambda e: e.activation(out=st[:], in_=pp[:], func=AF.Sigmoid), reads=[pp], writes=[st])
                        c = ch - CH_GT
                        b.dma('pool', self.gtT[c * 128:(c + 1) * 128, tsl], st[:], reads=[st], writes=[pd_])

    def exchange1(self):
        b = self.b
        g4 = [[0, 1, 2, 3], [4, 5, 6, 7]]
        x = b.dram('ex1')
        for (i_t, o_t) in ((self.kT_own, self.kT_all), (self.v_own, self.v_all), (self.ki_own, self.ki_all), (self.tail_own, self.tail_all)):
            b.custom('pool', lambda e, i_t=i_t, o_t=o_t: e.collective_compute(
                "AllGather", ALU.bypass, replica_groups=g4, ins=[i_t.ap().opt()], outs=[o_t.ap().opt()]), 1, reads=[], writes=[x])

    def exchange2(self):
        b = self.b
        g4 = [[0, 1, 2, 3], [4, 5, 6, 7]]
        x = b.dram('ex2')
        b.custom('pool', lambda e: e.collective_compute(
            "AllGather", ALU.bypass, replica_groups=g4, ins=[self.ends_own.ap().opt()], outs=[self.ends_all.ap().opt()]), 1, reads=[], writes=[x])

    def phase_lru1(self, l):
        b, ar = self.b, self.ar
        ar.reset()
        T = TT
        xl = ar.take('xl', 8 * (T + 4), (128, 8, T + 4))
        xc = ar.take('xc', 8 * T, (128, 8, T))
        xcf = ar.take('xcf', 8 * T, (128, 8, T))
        sr = ar.take('sr', 8 * T, (128, 8, T))
        si = ar.take('si', 8 * T, (128, 8, T))
        a = ar.take('a', 8 * T, (128, 8, T))
        u = ar.take('u', 8 * T, (128, 8, T))
        hl = ar.take('hl', 8 * T, (128, 8, T))
        pc = ar.take('pc', 8 * T, (128, 8, T))
        wai = ar.take('wai', 2048)
        wair = ar.take('wair', 2048)
        tl = ar.take('tl', 128, (128, 4, 32))
        car = ar.take('car', 16)
        lrp = self.lrp
        cw = [lrp[:, j * 8:(j + 1) * 8] for j in range(4)]
        cb, ba, bi, lam = lrp[:, 32:40], lrp[:, 40:48], lrp[:, 48:56], lrp[:, 56:64]
        d_ = b.dram('lru_d')
        b.dma('pool', wai[:], self.wai[l], writes=[wai])
        b.op('dve', lambda e: e.tensor_copy(out=r32(wair[:]), in_=wai[:]), reads=[wai], writes=[wair])
        wa = r32(wair[:]).rearrange("p (w n d) -> p w n d", w=2, n=8)
        b.op('act', lambda e: e.activation(out=self.clru[:], in_=lam, func=AF.Exp, scale=-1.0), reads=[lrp], writes=[self.clru])
        b.op('act', lambda e: e.activation(out=self.clru[:], in_=self.clru[:], func=AF.Ln, bias=self.ones0[:, 0:1], scale=1.0),
             reads=[self.clru, self.ones0], writes=[self.clru])
        b.op('dve', lambda e: e.tensor_scalar(out=self.clru[:], in0=self.clru[:], scalar1=-8.0, scalar2=None, op0=ALU.mult),
             reads=[self.clru], writes=[self.clru])
        b.dma('pool', tl[:], self.tail_all.ap().rearrange("(r p) q -> p r q", p=128), reads=[d_], writes=[tl])
        halo = xl
        b.op('dve', lambda e: e.tensor_scalar(out=xl[:, :, 0:4], in0=tl[:, 0, :].rearrange("p (n q) -> p n q", q=4), scalar1=self.selt[:, 0:1],
                                              scalar2=None, op0=ALU.mult), reads=[tl, self.selt], writes=[xl])
        for r in range(1, 4):
            b.op('dve', lambda e, r=r: e.scalar_tensor_tensor(out=xl[:, :, 0:4], in0=tl[:, r, :].rearrange("p (n q) -> p n q", q=4),
                                                              scalar=self.selt[:, r:r + 1], in1=xl[:, :, 0:4], op0=ALU.mult, op1=ALU.add),
                 reads=[tl, self.selt, xl], writes=[xl])
        b.op('dve', lambda e: e.memset(car[:, 0:8], 1.0), writes=[car])
        b.op('dve', lambda e: e.memset(car[:, 8:16], 0.0), writes=[car])
        for tt in range(NT // T):
            tsl = slice(tt * T, (tt + 1) * T)
            b.dma('pool', xl[:, :, 4:4 + T], self.xlT.rearrange("(n p) t -> p n t", p=128)[:, :, tsl], reads=[d_], writes=[xl])
            for n in range(8):
                b.op('dve', lambda e, n=n: e.tensor_scalar(out=xcf[:, n, :], in0=xl[:, n, 1:1 + T], scalar1=cw[0][:, n:n + 1], scalar2=cb[:, n:n + 1],
                                                           op0=ALU.mult, op1=ALU.add), reads=[xl, lrp], writes=[xcf])
                for j in range(1, 4):
                    b.op('dve', lambda e, n=n, j=j: e.scalar_tensor_tensor(out=xcf[:, n, :], in0=xl[:, n, j + 1:j + 1 + T], scalar=cw[j][:, n:n + 1],
                                                                           in1=xcf[:, n, :], op0=ALU.mult, op1=ALU.add),
                         reads=[xl, lrp, xcf], writes=[xcf])
            b.op('act', lambda e: e.copy(out=r32(xc[:]), in_=xcf[:]), reads=[xcf], writes=[xc])
            for n in range(8):
                pr, pi = self.ps[(n % 2) * 2], self.ps[(n % 2) * 2 + 1]
                b.op('pe', lambda e, n=n: e.matmul(pr[:], wa[:, 0, n, :], r32(xc[:, n, :]), start=True, stop=True), reads=[wair, xc], writes=[pr])
                b.op('pe', lambda e, n=n: e.matmul(pi[:], wa[:, 1, n, :], r32(xc[:, n, :]), start=True, stop=True), reads=[wair, xc], writes=[pi])
                b.op('act', lambda e, n=n: e.activation(out=sr[:, n, :], in_=pr[:], func=AF.Sigmoid, bias=ba[:, n:n + 1], scale=1.0),
                     reads=[pr, lrp], writes=[sr])
                b.op('act', lambda e, n=n: e.activation(out=si[:, n, :], in_=pi[:], func=AF.Sigmoid, bias=bi[:, n:n + 1], scale=1.0),
                     reads=[pi, lrp], writes=[si])
            for n in range(8):
                b.op('act', lambda e, n=n: e.activation(out=a[:, n, :], in_=sr[:, n, :], func=AF.Exp, scale=self.clru[:, n:n + 1]),
                     reads=[sr, self.clru], writes=[a])
            b.op('dve', lambda e: e.tensor_tensor(out=u[:], in0=a[:], in1=a[:], op=ALU.mult), reads=[a], writes=[u])
            b.op('dve', lambda e: e.tensor_scalar(out=u[:], in0=u[:], scalar1=-1.0, scalar2=1.0, op0=ALU.mult, op1=ALU.add), reads=[u], writes=[u])
            b.op('dve', lambda e: e.tensor_scalar(out=u[:], in0=u[:], scalar1=0.0, scalar2=None, op0=ALU.max), reads=[u], writes=[u])
            b.op('act', lambda e: e.activation(out=u[:], in_=u[:], func=AF.Sqrt), reads=[u], writes=[u])
            b.op('dve', lambda e: e.tensor_tensor(out=u[:], in0=u[:], in1=si[:], op=ALU.mult), reads=[u, si], writes=[u])
            b.op('dve', lambda e: e.tensor_tensor(out=u[:], in0=u[:], in1=xcf[:], op=ALU.mult), reads=[u, xcf], writes=[u])
            for n in range(8):
                b.op('dve', lambda e, n=n: e.tensor_tensor_scan(out=hl[:, n, :], data0=a[:, n, :], data1=u[:, n, :], initial=car[:, 8 + n:9 + n],
                                                                op0=ALU.mult, op1=ALU.add), reads=[a, u, car], writes=[hl])
                b.op('dve', lambda e, n=n: e.tensor_tensor_scan(out=pc[:, n, :], data0=a[:, n, :], data1=self.zero[:, 0:T], initial=car[:, n:n + 1],
                                                                op0=ALU.mult, op1=ALU.add), reads=[a, self.zero, car], writes=[pc])
            b.op('dve', lambda e: e.tensor_copy(out=car[:, 0:8], in_=pc[:, :, T - 1]), reads=[pc], writes=[car])
            b.op('dve', lambda e: e.tensor_copy(out=car[:, 8:16], in_=hl[:, :, T - 1]), reads=[hl], writes=[car])
            b.dma('pool', self.hlT.rearrange("(n p) t -> p n t", p=128)[:, :, tsl], hl[:], reads=[hl], writes=[d_])
            b.dma('pool', self.pcT.rearrange("(n p) t -> p n t", p=128)[:, :, tsl], pc[:], reads=[pc], writes=[d_])
            if tt + 1 < NT // T:
                b.op('dve', lambda e: e.tensor_copy(out=xl[:, :, 0:4], in_=xl[:, :, T:T + 4]), reads=[xl], writes=[xl])
        b.dma('pool', self.ends_own.ap(), car[:], reads=[car], writes=[d_])

    def phase_lru2(self, l):
        b, ar = self.b, self.ar
        ar.reset()
        T = TT
        hl = ar.take('hl', 8 * T, (128, 8, T))
        pc = ar.take('pc', 8 * T, (128, 8, T))
        gl = ar.take('gl', 8 * T, (128, 8, T))
        t1 = ar.take('t1', 8 * T, (128, 8, T))
        en = ar.take('en', 64, (128, 4, 16))
        hin = ar.take('hin', 8)
        hnw = ar.take('hnw', 8)
        d_ = b.dram('lru2_d')
        b.dma('pool', en[:], self.ends_all.ap().rearrange("(r p) q -> p r q", p=128), reads=[d_], writes=[en])
        b.op('dve', lambda e: e.memset(hin[:], 0.0), writes=[hin])
        for r in range(3):
            b.op('dve', lambda e, r=r: e.tensor_tensor(out=hnw[:], in0=en[:, r, 0:8], in1=hin[:], op=ALU.mult), reads=[en, hin], writes=[hnw])
            b.op('dve', lambda e, r=r: e.tensor_tensor(out=hnw[:], in0=hnw[:], in1=en[:, r, 8:16], op=ALU.add), reads=[en, hnw], writes=[hnw])
            b.op('dve', lambda e: e.tensor_tensor(out=hnw[:], in0=hnw[:], in1=hin[:], op=ALU.subtract), reads=[hnw, hin], writes=[hnw])
            b.op('dve', lambda e, r=r: e.scalar_tensor_tensor(out=hin[:], in0=hnw[:], scalar=self.selt[:, 4 + r:5 + r], in1=hin[:],
                                                              op0=ALU.mult, op1=ALU.add), reads=[hnw, self.selt, hin], writes=[hin])
        c1 = math.sqrt(2.0 / math.pi)
        for tt in range(NT // T):
            tsl = slice(tt * T, (tt + 1) * T)
            for (dst, src) in ((hl, self.hlT), (pc, self.pcT), (gl, self.glT)):
                b.dma('pool', dst[:], src.rearrange("(n p) t -> p n t", p=128)[:, :, tsl], reads=[d_], writes=[dst])
            for n in range(8):
                b.op('dve', lambda e, n=n: e.scalar_tensor_tensor(out=hl[:, n, :], in0=pc[:, n, :], scalar=hin[:, n:n + 1], in1=hl[:, n, :],
                                                                  op0=ALU.mult, op1=ALU.add), reads=[pc, hin, hl], writes=[hl])
            b.op('act', lambda e: e.activation(out=t1[:], in_=gl[:], func=AF.Square), reads=[gl], writes=[t1])
            b.op('dve', lambda e: e.tensor_scalar(out=t1[:], in0=t1[:], scalar1=0.044715, scalar2=1.0, op0=ALU.mult, op1=ALU.add), reads=[t1], writes=[t1])
            b.op('dve', lambda e: e.tensor_tensor(out=t1[:], in0=t1[:], in1=gl[:], op=ALU.mult), reads=[t1, gl], writes=[t1])
            b.op('act', lambda e: e.activation(out=t1[:], in_=t1[:], func=AF.Sigmoid, scale=2.0 * c1), reads=[t1], writes=[t1])
            b.op('dve', lambda e: e.tensor_tensor(out=t1[:], in0=t1[:], in1=gl[:], op=ALU.mult), reads=[t1, gl], writes=[t1])
            b.op('dve', lambda e: e.tensor_tensor(out=t1[:], in0=t1[:], in1=hl[:], op=ALU.mult), reads=[t1, hl], writes=[t1])
            b.dma('pool', self.ybT.rearrange("(n p) t -> p n t", p=128)[:, :, tsl], t1[:], reads=[t1], writes=[d_])
            if l == 0 and 'dbg_yb' in self.dbg:
                b.dma('pool', self.dbg['dbg_yb'].rearrange("(n p) t -> p n t", p=128)[:, :, tsl], t1[:], reads=[t1], writes=[d_])

    def phase_sel(self, l):
        b, ar = self.b, self.ar
        ar.reset()
        KI = ar.take('KI', S, parts=64)
        sc = ar.take('sc', S)
        pf = ar.take('pf', S)
        mA = ar.take('mA', S // 2, dtype=BF16)
        mC = ar.take('mC', S // 2, dtype=BF16)
        qi = ar.take('qi', 4 * 128, (64, 4, 128), parts=64)
        qir = ar.take('qir', 4 * 128, (64, 4, 128), parts=64)
        rl = [ar.take('rl%d' % i, 512) for i in range(3)]
        wst = ar.take('wst', 64, (128, 16, 4))
        wab = ar.take('wab', 64, (128, 16, 4))
        wsg = ar.take('wsg', 64, (128, 16, 4))
        qrel = ar.take('qrel', 256, (128, 16, 16))
        sm = ar.take('sm', 16)
        tst = [ar.take('tst%d' % i, 512, dtype=BF16) for i in range(2)]
        d_ = b.dram('sel_d')
        for r in range(4):
            b.dma('pool', r32(KI[0:64, r * NT:(r + 1) * NT]), r32(self.ki_all[r * 64:(r + 1) * 64, :]), reads=[d_], writes=[KI])
        b.dma('pool', wst[:], self.ws.rearrange("(i p) h -> p i h", p=128), reads=[d_], writes=[wst])
        b.op('act', lambda e: e.activation(out=wab[:], in_=wst[:], func=AF.Abs), reads=[wst], writes=[wab])
        b.op('act', lambda e: e.activation(out=wsg[:], in_=wst[:], func=AF.Sign), reads=[wst], writes=[wsg])
        for cc in range(16):
            b.op('dve', lambda e, cc=cc: e.tensor_scalar(out=qrel[:, :, cc], in0=self.qpc[:], scalar1=-512.0 * cc, scalar2=None, op0=ALU.add),
                 reads=[self.qpc], writes=[qrel])
        lo, hi, mid, cnt, mm, tmp, need = (sm[:, i:i + 1] for i in range(7))
        pst = self.ps[7]
        pT = self.ps[5]
        for i in range(NT // 128):
            b.dma('pool', qi[:], self.qiT.rearrange("(h d) t -> d h t", d=64)[:, :, i * 128:(i + 1) * 128], reads=[d_], writes=[qi])
            b.op('dve', lambda e: e.tensor_copy(out=r32(qir[:]), in_=qi[:]), reads=[qi], writes=[qir])
            for cc in range(16):
                csl = slice(cc * 512, (cc + 1) * 512)
                b.op('dve', lambda e, cc=cc, csl=csl, i=i: e.tensor_scalar(out=sc[:, csl], in0=self.iota[:], scalar1=qrel[:, i, cc:cc + 1], scalar2=NEG,
                                                                          op0=ALU.is_gt, op1=ALU.mult), reads=[self.iota, qrel], writes=[sc])
                for h in range(4):
                    pp = self.ps[h % 4]
                    b.op('pe', lambda e, h=h, csl=csl: e.matmul(pp[:], r32(qir[0:64, h, :]), r32(KI[0:64, csl]), start=True, stop=True),
                         reads=[qir, KI], writes=[pp])
                    rr = rl[h % 3]
                    b.op('act', lambda e, h=h, i=i: e.activation(out=rr[:], in_=pp[:], func=AF.Relu, scale=wab[:, i, h:h + 1]),
                         reads=[pp, wab], writes=[rr])
                    b.op('dve', lambda e, h=h, i=i, csl=csl: e.scalar_tensor_tensor(out=sc[:, csl], in0=rr[:], scalar=wsg[:, i, h:h + 1], in1=sc[:, csl],
                                                                                    op0=ALU.mult, op1=ALU.add), reads=[rr, wsg, sc], writes=[sc])
            b.op('dve', lambda e: e.tensor_reduce(out=hi, in_=sc[:], axis=AX.X, op=ALU.max), reads=[sc], writes=[sm])
            b.op('dve', lambda e: e.tensor_scalar(out=pf[:], in0=sc[:], scalar1=0.5 * NEG, scalar2=-2.0 * NEG, op0=ALU.is_lt, op1=ALU.mult),
                 reads=[sc], writes=[pf])
            b.op('dve', lambda e: e.tensor_tensor(out=pf[:], in0=pf[:], in1=sc[:], op=ALU.add), reads=[pf, sc], writes=[pf])
            b.op('dve', lambda e: e.tensor_reduce(out=lo, in_=pf[:], axis=AX.X, op=ALU.min), reads=[pf], writes=[sm])
            b.op('dve', lambda e: e.tensor_scalar(out=lo, in0=lo, scalar1=-1.0, scalar2=None, op0=ALU.add), reads=[sm], writes=[sm])
            for it in range(BISECT):
                if it == 0:
                    b.op('dve', lambda e: e.memset(mid, 0.0), writes=[sm])
                    b.op('dve', lambda e: e.tensor_tensor(out=mid, in0=mid, in1=lo, op=ALU.max), reads=[sm], writes=[sm])
                    b.op('dve', lambda e: e.tensor_tensor(out=mid, in0=mid, in1=hi, op=ALU.min), reads=[sm], writes=[sm])
                else:
                    b.op('dve', lambda e: e.tensor_tensor(out=mid, in0=lo, in1=hi, op=ALU.add), reads=[sm], writes=[sm])
                    b.op('dve', lambda e: e.tensor_scalar(out=mid, in0=mid, scalar1=0.5, scalar2=None, op0=ALU.mult), reads=[sm], writes=[sm])
                b.op('dve', lambda e: e.tensor_scalar(out=mC[:], in0=sc[:], scalar1=mid, scalar2=0.0, op0=ALU.is_gt, op1=ALU.add, accum_out=cnt),
                     reads=[sc, sm], writes=[mC, sm])
                b.op('dve', lambda e: e.tensor_scalar(out=mm, in0=cnt, scalar1=float(TOPK), scalar2=None, op0=ALU.is_gt), reads=[sm], writes=[sm])
                b.op('dve', lambda e: e.tensor_tensor(out=tmp, in0=mid, in1=lo, op=ALU.subtract), reads=[sm], writes=[sm])
                b.op('dve', lambda e: e.scalar_tensor_tensor(out=lo, in0=tmp, scalar=mm, in1=lo, op0=ALU.mult, op1=ALU.add), reads=[sm], writes=[sm])
                b.op('dve', lambda e: e.tensor_tensor(out=tmp, in0=hi, in1=mid, op=ALU.subtract), reads=[sm], writes=[sm])
                b.op('dve', lambda e: e.scalar_tensor_tensor(out=hi, in0=tmp, scalar=mm, in1=mid, op0=ALU.mult, op1=ALU.add), reads=[sm], writes=[sm])
            b.op('dve', lambda e: e.tensor_scalar(out=mA[:], in0=sc[:], scalar1=hi, scalar2=0.0, op0=ALU.is_gt, op1=ALU.add, accum_out=cnt),
                 reads=[sc, sm], writes=[mA, sm])
            b.op('dve', lambda e: e.tensor_scalar(out=need, in0=cnt, scalar1=-1.0, scalar2=float(TOPK), op0=ALU.mult, op1=ALU.add), reads=[sm], writes=[sm])
            b.op('dve', lambda e: e.scalar_tensor_tensor(out=mC[:], in0=sc[:], scalar=lo, in1=mA[:], op0=ALU.is_gt, op1=ALU.subtract),
                 reads=[sc, sm, mA], writes=[mC])
            b.op('dve', lambda e: e.tensor_tensor_scan(out=pf[:], data0=mC[:], data1=mC[:], initial=0.0, op0=ALU.add, op1=ALU.max),
                 reads=[mC], writes=[pf])
            b.op('dve', lambda e: e.scalar_tensor_tensor(out=mC[:], in0=pf[:], scalar=need, in1=mC[:], op0=ALU.is_le, op1=ALU.mult),
                 reads=[pf, sm, mC], writes=[mC])
            b.op('dve', lambda e: e.tensor_tensor(out=mA[:], in0=mA[:], in1=mC[:], op=ALU.add), reads=[mA, mC], writes=[mA])
            for jg in range(8):
                ts_ = tst[jg % 2]
                pTb = pT.t[:].bitcast(BF16)
                for jj in range(8):
                    j = jg * 8 + jj
                    b.op('pe', lambda e, j=j, jj=jj: e.transpose(pTb[:, jj * 128:(jj + 1) * 128], mA[:, j * 128:(j + 1) * 128], self.ident[:]),
                         reads=[mA, self.ident], writes=[pT])
                b.op('act', lambda e: e.copy(out=ts_[:], in_=pTb[:, 0:1024]), reads=[pT], writes=[ts_])
                b.dma('pool', self.mskT[jg * 8:(jg + 1) * 8, :, i * 128:(i + 1) * 128].rearrange("j s t -> s j t"),
                      ts_[:].rearrange("s (j t) -> s j t", j=8), reads=[ts_], writes=[d_])

    def phase_attn(self, l):
        b, ar = self.b, self.ar
        ar.reset()
        KT = ar.take('KT', S, (128, 2, S), dtype=BF16)
        VT = ar.take('VT', 64 * 128, (128, 64, 256), dtype=BF16)
        QT = ar.take('QT', 8 * 256, (128, 8, TT), dtype=BF16)
        MTf = ar.take('MTf', 2048, (128, 8, 2, 128))
        MT = ar.take('MT', 1024, (128, 8, 2, 128), dtype=BF16)
        E0 = ar.take('E0', TT)
        qpr = ar.take('qpr', NT)
        onesb = ar.take('onesb', 64, dtype=BF16)
        mk = [ar.take('mk%d' % i, TT // 2, dtype=BF16) for i in range(4)]
        oh = [ar.take('oh%d' % i, TT // 2, dtype=BF16) for i in range(3)]
        ee = [ar.take('ee%d' % i, TT // 2, dtype=BF16) for i in range(4)]
        pp_ = [ar.take('pp%d' % i, TT // 2, dtype=BF16) for i in range(4)]
        rc = [ar.take('rc%d' % i, TT) for i in range(2)]
        yo = [ar.take('yo%d' % i, TT) for i in range(2)]
        d_ = b.dram('att_d')
        for r in range(4):
            for kv in range(2):
                b.dma('pool', KT[:, kv, r * NT:(r + 1) * NT], self.kT_all[r * 256 + kv * 128:r * 256 + (kv + 1) * 128, :], reads=[d_], writes=[KT])
        b.dma('pool', VT[:], self.v_all.ap().rearrange("(j p) f -> p j f", p=128), reads=[d_], writes=[VT])
        b.dma('pool', MTf[:].rearrange("p h u s -> p (h u s)"), self.mtab, reads=[d_], writes=[MTf])
        b.dma('pool', qpr[:], self.qposr[0].partition_broadcast(128), reads=[d_], writes=[qpr])
        b.op('dve', lambda e: e.tensor_copy(out=onesb[:], in_=self.ones0[:]), reads=[self.ones0], writes=[onesb])
        for h in range(8):
            b.op('dve', lambda e, h=h: e.tensor_scalar(out=MT[:, h], in0=MTf[:, h], scalar1=self.b31t[:, h:h + 1], scalar2=None, op0=ALU.subtract),
                 reads=[MTf, self.b31t], writes=[MT])
        mi = 0
        for m in range(NTT):
            tsl = slice(m * TT, (m + 1) * TT)
            b.dma('pool', QT[:], self.qaT.rearrange("(h d) t -> d h t", d=128)[:, :, tsl], reads=[d_], writes=[QT])
            b.op('dve', lambda e: e.tensor_scalar(out=E0[:], in0=qpr[:, tsl], scalar1=self.pidx[:, 0:1], scalar2=None, op0=ALU.subtract),
                 reads=[qpr, self.pidx], writes=[E0])
            for hp in range(4):
                acc = [self.ps[0], self.ps[1], self.ps[2], self.ps[3]]
                ohq = {}

                def get_oh(j):
                    if j not in ohq:
                        o = oh[j % 3]
                        b.op('pool', lambda e, j=j, o=o: e.tensor_scalar(out=o[:], in0=E0[:], scalar1=128.0 * j, scalar2=None, op0=ALU.is_equal),
                             reads=[E0], writes=[o])
                        ohq[j] = o
                    return ohq[j]
                for j in range(64):
                    mt = mk[mi % 4]
                    mi += 1
                    b.dma('sp', mt[:], self.mskT[j, :, tsl], reads=[d_], writes=[mt])
                    o0, o1 = get_oh(j), get_oh(j + 1)
                    for hh in range(2):
                        h = hp * 2 + hh
                        kv = h // 4
                        pl = self.ps[4 + (j * 2 + hh) % 3]
                        b.op('pe', lambda e, h=h, kv=kv, j=j: e.matmul(pl[:], KT[:, kv, j * 128:(j + 1) * 128], QT[:, h, :], start=True, stop=False),
                             reads=[KT, QT], writes=[pl])
                        b.op('pe', lambda e, h=h: e.matmul(pl[:], MT[:, h, 0, :], o0[:], start=False, stop=False), reads=[MT, o0], writes=[pl])
                        b.op('pe', lambda e, h=h: e.matmul(pl[:], MT[:, h, 1, :], o1[:], start=False, stop=True), reads=[MT, o1], writes=[pl])
                        et = ee[(j * 2 + hh) % 4]
                        b.op('act', lambda e, h=h, et=et: e.activation(out=et[:], in_=pl[:], func=AF.Exp, bias=self.b31t[:, h:h + 1], scale=1.0),
                             reads=[pl, self.b31t], writes=[et])
                        pt_ = pp_[(j * 2 + hh) % 4]
                        b.op('dve', lambda e, et=et, pt_=pt_: e.tensor_tensor(out=pt_[:], in0=et[:], in1=mt[:], op=ALU.mult), reads=[et, mt], writes=[pt_])
                        b.op('pe', lambda e, kv=kv, j=j, hh=hh, pt_=pt_: e.matmul(acc[hh * 2][:], VT[:, j, kv * 128:(kv + 1) * 128], pt_[:],
                                                                                  start=(j == 0), stop=(j == 63)), reads=[VT, pt_], writes=[acc[hh * 2]])
                        b.op('pe', lambda e, j=j, hh=hh, pt_=pt_: e.matmul(acc[hh * 2 + 1][:], onesb[:], pt_[:], start=(j == 0), stop=(j == 63)),
                             reads=[onesb, pt_], writes=[acc[hh * 2 + 1]])
                for hh in range(2):
                    h = hp * 2 + hh
                    rcp, y = rc[hh], yo[hh]
                    b.op('dve', lambda e: e.reciprocal(out=rcp[:], in_=acc[hh * 2 + 1][:]), reads=[acc[hh * 2 + 1]], writes=[rcp])
                    b.op('dve', lambda e: e.tensor_tensor(out=y[:], in0=acc[hh * 2][:], in1=rcp[:], op=ALU.mult), reads=[acc[hh * 2], rcp], writes=[y])
                    b.dma('pool', self.yaT[h * 128:(h + 1) * 128, tsl], y[:], reads=[y], writes=[d_])
                    if l == 0 and 'dbg_ya' in self.dbg:
                        b.dma('pool', self.dbg['dbg_ya'][h * 128:(h + 1) * 128, tsl], y[:], reads=[y], writes=[d_])

    def phase_mem(self, l):
        b, ar = self.b, self.ar
        ar.reset()
        mx = ar.take('mx', KC * NMEM, (128, KC, NMEM))
        mh = ar.take('mh', KC * NMEM, (128, KC, NMEM))
        wb = [ar.take('mwb%d' % i, 4096) for i in range(2)]
        mkT = ar.take('mkT', 8 * NMEM // 2, (128, 8, NMEM), dtype=BF16)
        mv = ar.take('mv', 2 * 1024 // 2, (128, 2, 1024), dtype=BF16)
        qm = ar.take('qm', 8 * TT // 2, (128, 8, TT), dtype=BF16)
        pe_ = [ar.take('pe%d' % i, TT // 2, dtype=BF16) for i in range(4)]
        onesb = ar.take('onesb', 64, dtype=BF16)
        rc = [ar.take('rc%d' % i, TT) for i in range(2)]
        yo = [ar.take('yo%d' % i, TT) for i in range(2)]
        self.rs = ar.take('rs', TT)
        self.rstd = ar.take('rstd', TT)
        d_ = b.dram('mem_d')
        b.op('dve', lambda e: e.tensor_copy(out=onesb[:], in_=self.ones0[:]), reads=[self.ones0], writes=[onesb])
        b.dma('pool', mx[:], self.memT.rearrange("(k p) m -> p k m", p=128), reads=[d_], writes=[mx])
        self.rmsnorm_T(mx, mh, self.v16[:, 48:64], self.v16, n=NMEM)
        W4 = self.W4[l]
        for ti in range(8):
            w = wb[ti % 2]
            self.wload(w, W4, R_MKV + ti * 128)
            wv = w[:].rearrange("p (k c) -> p k c", k=KC)
            if ti < 4:
                for h2 in range(2):
                    ch = ti * 2 + h2
                    pp = self.ps[ch % 2]
                    for k in range(KC):
                        b.op('pe', lambda e, k=k, h2=h2: e.matmul(pp[:, 0:NMEM], r32(wv[:, k, h2 * 128:(h2 + 1) * 128]), r32(mh[:, k, :]),
                                                                  start=(k == 0), stop=(k == KC - 1)), reads=[w, mh], writes=[pp])
                    b.op('act', lambda e, ch=ch: e.copy(out=mkT[:, ch, :], in_=pp[:, 0:NMEM]), reads=[pp], writes=[mkT])
            else:
                f0 = (ti - 4) * 256
                for mt in range(2):
                    pp = self.ps[2 + mt]
                    for k in range(KC):
                        b.op('pe', lambda e, k=k, mt=mt: e.matmul(pp[:, 0:256], r32(mh[:, k, mt * 128:(mt + 1) * 128]), r32(wv[:, k, :]),
                                                                  start=(k == 0), stop=(k == KC - 1)), reads=[w, mh], writes=[pp])
                    b.op('act', lambda e, mt=mt, f0=f0: e.copy(out=mv[:, mt, f0:f0 + 256], in_=pp[:, 0:256]), reads=[pp], writes=[mv])
        it = 0
        for tt in range(NTT):
            tsl = slice(tt * TT, (tt + 1) * TT)
            b.dma('pool', qm[:], self.qmT.rearrange("(c d) t -> d c t", d=128)[:, :, tsl], reads=[d_], writes=[qm])
            for h in range(4):
                pts = []
                for mt in range(2):
                    pl = self.ps[4 + mt]
                    for dc in range(2):
                        b.op('pe', lambda e, h=h, mt=mt, dc=dc: e.matmul(pl[:], mkT[:, h * 2 + dc, mt * 128:(mt + 1) * 128], qm[:, h * 2 + dc, :],
                                                                         start=(dc == 0), stop=(dc == 1)), reads=[mkT, qm], writes=[pl])
                    pt_ = pe_[it % 4]
                    it += 1
                    b.op('act', lambda e, pt_=pt_: e.activation(out=pt_[:], in_=pl[:], func=AF.Exp), reads=[pl], writes=[pt_])
                    pts.append(pt_)
                pden = self.ps[6]
                for mt in range(2):
                    b.op('pe', lambda e, mt=mt: e.matmul(pden[:], onesb[:], pts[mt][:], start=(mt == 0), stop=(mt == 1)), reads=[onesb, pts[mt]], writes=[pden])
                rcp = rc[h % 2]
                b.op('dve', lambda e: e.reciprocal(out=rcp[:], in_=pden[:]), reads=[pden], writes=[rcp])
                for dc in range(2):
                    po = self.ps[dc]
                    for mt in range(2):
                        b.op('pe', lambda e, h=h, mt=mt, dc=dc: e.matmul(po[:], mv[:, mt, h * 256 + dc * 128:h * 256 + (dc + 1) * 128], pts[mt][:],
                                                                         start=(mt == 0), stop=(mt == 1)), reads=[mv, pts[mt]], writes=[po])
                    y = yo[dc]
                    b.op('dve', lambda e: e.tensor_tensor(out=y[:], in0=po[:], in1=rcp[:], op=ALU.mult), reads=[po, rcp], writes=[y])
                    r0 = h * 256 + dc * 128
                    b.dma('pool', self.ycT[r0:r0 + 128, tsl], y[:], reads=[y], writes=[d_])
                    if l == 0 and 'dbg_yc' in self.dbg:
                        b.dma('pool', self.dbg['dbg_yc'][r0:r0 + 128, tsl], y[:], reads=[y], writes=[d_])

    def phase_merge(self, l):
        b = self.b
        self.ffn_bufs()
        xt, hT = self.xt, self.hT
        d_ = b.dram('mrg_d')
        xd = b.dram('mrg_x')
        W4 = self.W4[l]
        ysrc = (self.yaT, self.ybT, self.ycT)
        gts = self.stage
        for tt in range(NTT):
            tsl = slice(tt * TT, (tt + 1) * TT)
            b.dma('pool', xt[:], self.xview(self.xres, tt), reads=[xd], writes=[xt])
            for j in range(3):
                for half in range(2):
                    wdb = self.wd[2 * j + half]
                    b.dma('pool', r32(wdb[:].rearrange("p (c t) -> p c t", c=4)),
                          r32(ysrc[j][half * 512:(half + 1) * 512, tsl].rearrange("(c p) t -> p c t", p=128)), reads=[d_], writes=[wdb])
            for dq in range(4):
                wbs = []
                for j in range(3):
                    wb = self.wb[self.wbi % 3]
                    self.wbi += 1
                    self.wload(wb, W4, R_WBR + (j * 4 + dq) * 128)
                    wbs.append(wb)
                for dd in range(4):
                    dch = dq * 4 + dd
                    for j in range(3):
                        pb = self.ps[j]
                        wv = wbs[j][:].rearrange("p (c d) -> p c d", c=8)
                        for c in range(8):
                            ybuf = self.wd[2 * j + c // 4]
                            b.op('pe', lambda e, c=c, dd=dd, wv=wv, ybuf=ybuf: e.matmul(pb[:], r32(wv[:, c, dd * 128:(dd + 1) * 128]),
                                                                                        r32(ybuf[:, (c % 4) * 512:(c % 4 + 1) * 512]),
                                                                                        start=(c == 0), stop=(c == 7)), reads=[wbs[j], ybuf], writes=[pb])
                    for j in range(3):
                        g = gts[j]
                        b.dma('pool', g[:], self.gtT[j * 2048 + dch * 128:j * 2048 + (dch + 1) * 128, tsl], reads=[d_], writes=[g])
                    b.op('dve', lambda e: e.tensor_tensor(out=self.rs[:], in0=self.ps[0][:], in1=gts[0][:], op=ALU.mult), reads=[self.ps[0], gts[0]], writes=[self.rs])
                    b.op('dve', lambda e: e.tensor_tensor(out=self.sg[0][:], in0=self.ps[1][:], in1=gts[1][:], op=ALU.mult), reads=[self.ps[1], gts[1]], writes=[self.sg[0]])
                    b.op('dve', lambda e: e.tensor_tensor(out=self.sg[1][:], in0=self.ps[2][:], in1=gts[2][:], op=ALU.mult), reads=[self.ps[2], gts[2]], writes=[self.sg[1]])
                    b.op('dve', lambda e: e.tensor_tensor(out=self.rs[:], in0=self.rs[:], in1=self.sg[0][:], op=ALU.add), reads=[self.rs, self.sg[0]], writes=[self.rs])
                    b.op('dve', lambda e, dch=dch: e.tensor_tensor(out=r32(hT[:, dch, :]), in0=self.rs[:], in1=self.sg[1][:], op=ALU.add),
                         reads=[self.rs, self.sg[1]], writes=[hT])
            for ti in range(8):
                wb = self.wb[self.wbi % 3]
                self.wbi += 1
                self.wload(wb, W4, R_OUT + ti * 128)
                wv = wb[:].rearrange("p (k c) -> p k c", k=KC)
                for h2 in range(2):
                    dch = ti * 2 + h2
                    po = self.ps[4 + dch % 2]
                    for k in range(KC):
                        b.op('pe', lambda e, k=k, h2=h2: e.matmul(po[:], r32(wv[:, k, h2 * 128:(h2 + 1) * 128]), r32(hT[:, k, :]),
                                                                  start=(k == 0), stop=(k == KC - 1)), reads=[wb, hT], writes=[po])
                    b.op('dve', lambda e, dch=dch: e.tensor_tensor(out=xt[:, dch, :], in0=po[:], in1=xt[:, dch, :], op=ALU.add), reads=[po, xt], writes=[xt])
            if l == 0 and 'dbg_x2' in self.dbg:
                b.dma('pool', self.xview(self.dbg['dbg_x2'], tt), xt[:], reads=[xt], writes=[xd])
            self.rmsnorm_T(xt, hT, self.v16[:, 32:48], self.v16)
            self.ffn_tile(xt, hT, l, 2)
            b.dma('pool', self.xview(self.xres, tt), xt[:], reads=[xt], writes=[xd])
            if self.xout is not None:
                b.dma('pool', self.xview(self.xout, tt), xt[:], reads=[xt], writes=[xd])

    def final_norm(self):
        b = self.b
        self.ffn_bufs()
        xd = b.dram('fin_x')
        for tt in range(NTT):
            b.dma('pool', self.xt[:], self.xview(self.xres, tt), reads=[xd], writes=[self.xt])
            self.rmsnorm_T(self.xt, self.hT, self.fng[:], self.fng)
            b.dma('pool', self.xview(self.yout, tt), self.hT[:], reads=[self.hT], writes=[xd])


def _t5_bucket(d):
    d = np.maximum(d, 0)
    df = np.maximum(d, 1).astype(np.float32)
    large = 16 + (np.log(df / 16) / math.log(8) * 16).astype(np.int32)
    large = np.minimum(large, 31)
    return np.where(d < 16, d, large)


def _tile16(w, ncol_tiles):
    return np.ascontiguousarray(w.reshape(16, 128, ncol_tiles, 256).transpose(2, 1, 0, 3)).reshape(ncol_tiles * 128, 4096)


def _prep_weights(inp, layers):
    nl = len(layers)
    w4 = np.zeros((nl, R4, 4096), np.float32)
    w2 = np.zeros((nl, R2, 2048), np.float32)
    for li, l in enumerate(layers):
        for (r0, key) in ((R_GU1, 'w_ff1_gu'), (R_GU2, 'w_ff2_gu')):
            wg = inp[key][l]
            w4[li, r0:r0 + 5504] = np.ascontiguousarray(wg.reshape(16, 128, 2, FC, 128).transpose(3, 1, 0, 2, 4)).reshape(5504, 4096)
        w2[li, 0:5504] = inp['w_ff1_down'][l]
        w2[li, 5504:] = inp['w_ff2_down'][l]
        wi = inp['w_in'][l]
        wr = np.zeros((2048, NCH * 128), np.float32)
        wr[:, 0:1792] = wi[:, 0:1792]
        wr[:, CH_KI * 128:CH_KI * 128 + 64] = wi[:, 1792:1856]
        wr[:, CH_WI * 128:CH_WI * 128 + 4] = wi[:, 1856:1860]
        wr[:, CH_XL * 128:] = wi[:, 1860:]
        w4[li, R_WIN:R_WIN + 5632] = _tile16(wr, NCH // 2)
        w4[li, R_MKV:R_MKV + 1024] = _tile16(inp['w_mem_kv'][l], 8)
        wb = inp['w_branch'][l]
        w4[li, R_WBR:R_WBR + 1536] = np.ascontiguousarray(wb.reshape(3, 8, 128, 4, 512).transpose(0, 3, 2, 1, 4)).reshape(1536, 4096)
        w4[li, R_OUT:R_OUT + 1024] = _tile16(inp['w_out'][l], 8)
    return w4, w2


def _shard(w, secs, c):
    return np.concatenate([w[:, r0 + c * (rn // 8): r0 + (c + 1) * (rn // 8)] for (r0, rn) in secs], axis=1)


_PROG_CACHE = {}


def _get_prog(nl, debug=()):
    key = (nl, tuple(sorted(debug)))
    if key not in _PROG_CACHE:
        _PROG_CACHE[key] = Prog(nl, debug)
    return _PROG_CACHE[key]


def _pk(v):
    return np.ascontiguousarray(np.asarray(v, np.float32).reshape(-1, 128).T)


def make_in_maps(inp, layers=None, x_shards=None):
    inp = {k: np.asarray(v) for k, v in inp.items()}
    if layers is None:
        layers = list(range(DEPTH))
    nl = len(layers)
    w4, w2 = _prep_weights(inp, layers)
    x = inp['x'].reshape(2 * S, D)
    vec16 = np.stack([np.concatenate([_pk(inp['norm_ff1'][l]), _pk(inp['norm_mix'][l]), _pk(inp['norm_ff2'][l]), _pk(inp['mem_norm'][l])], axis=1)
                      for l in layers])
    lrup = np.stack([np.concatenate([_pk(inp['conv_w'][l][j]) for j in range(4)] + [_pk(inp['conv_b'][l]), _pk(inp['b_a'][l]), _pk(inp['b_i'][l]),
                                                                                       _pk(inp['lam'][l])], axis=1) for l in layers])
    wai = np.stack([np.concatenate([np.ascontiguousarray(inp['w_a'][l].transpose(1, 0, 2)).reshape(128, 1024),
                                    np.ascontiguousarray(inp['w_i'][l].transpose(1, 0, 2)).reshape(128, 1024)], axis=1) for l in layers])
    lnp = np.stack([np.stack([inp['idx_ln_g'][l], inp['idx_ln_b'][l]], axis=1) for l in layers])
    rb = inp['rel_bias']
    u = np.arange(256)[:, None]
    s_ = np.arange(128)[None, :]
    bk = _t5_bucket(u - s_)
    mt = rb[bk]
    mtab = np.ascontiguousarray(mt.reshape(2, 128, 128, 8).transpose(1, 3, 0, 2)).reshape(128, 8 * 2 * 128)
    b31 = np.ascontiguousarray(rb[31:32, :])
    maps = []
    for c in range(NCORES):
        bi, cl = c // 4, c % 4
        pos = (cl * NT + np.arange(NT)).astype(np.float32)
        sel = np.zeros((1, 8), np.float32)
        if cl > 0:
            sel[0, cl - 1] = 1.0
        sel[0, 4:4 + cl] = 1.0
        m = dict(
            xin=(np.ascontiguousarray(x[c * NT:(c + 1) * NT].T) if x_shards is None else np.ascontiguousarray(x_shards[c])),
            memT=np.ascontiguousarray(inp['mem'][bi].T),
            w4s=np.ascontiguousarray(_shard(w4, SEC4, c)),
            w2s=np.ascontiguousarray(_shard(w2, SEC2, c)),
            vec16=vec16.astype(np.float32), fnorm=_pk(inp['final_norm']), lrup=lrup.astype(np.float32), wai=wai.astype(np.float32),
            lnp=lnp.astype(np.float32), mtab=mtab.astype(np.float32), b31=b31.astype(np.float32),
            qposr=pos[None, :].copy(), qposc=np.ascontiguousarray(pos.reshape(16, 128).T), sel=sel,
        )
        maps.append(m)
    return maps


def kernel(**inputs):
    prog = _get_prog(DEPTH)
    maps = make_in_maps(inputs)
    res = run_bass_kernel_spmd(prog.nc, maps, core_ids=list(range(NCORES)))
    out = np.concatenate([np.asarray(r["yout"]).T for r in res.results], axis=0)
    return out.reshape(2, S, D).astype(np.float32)
```

```python
import math
import os
import numpy as np
import concourse.bass as bass
import concourse.mybir as mybir
from concourse.bass_utils import run_bass_kernel_spmd

F32 = mybir.dt.float32
F32R = mybir.dt.float32r
BF16 = mybir.dt.bfloat16
AF = mybir.ActivationFunctionType
ALU = mybir.AluOpType
AX = mybir.AxisListType

NCORES = 8
D = 2048
KC = 16
DFF = 5504
FC = 43
EPS = 1e-6
S = 8192
NT = 2048
TT = 512
NTT = NT // TT
DEPTH = 2
NMEM = 256
TOPK = 256
NEG = -1.0e30
BISECT = 18

CH_QA, CH_KA, CH_VA, CH_QI, CH_KI, CH_WI, CH_XL, CH_GL, CH_QM, CH_GT = 0, 8, 10, 12, 14, 15, 16, 24, 32, 40
NCH = 88
R_GU1, R_WIN, R_MKV, R_WBR, R_OUT, R_GU2 = 0, 5504, 5504 + 5632, 5504 + 5632 + 1024, 5504 + 5632 + 1024 + 1536, 5504 + 5632 + 1024 + 1536 + 1024
R4 = R_GU2 + 5504
R2 = 2 * 5504
SEC4 = [(R_GU1, 5504), (R_WIN, 5632), (R_MKV, 1024), (R_WBR, 1536), (R_OUT, 1024), (R_GU2, 5504)]
SEC2 = [(0, 5504), (5504, 5504)]


class Buf:
    def __init__(self, name, t=None):
        self.name = name
        self.t = t
        self.w = None
        self.r = {}
        self.lsem = None
        self.ltot = 0
        self.ssem = None
        self.stot = 0

    def __getitem__(self, k):
        return self.t[k]


class B:
    def __init__(self, nc):
        self.nc = nc
        self.E = {'pe': nc.tensor, 'dve': nc.vector, 'act': nc.scalar, 'pool': nc.gpsimd, 'sp': nc.sync}
        self.sem = {k: nc.alloc_semaphore('c_' + k) for k in self.E}
        self.cnt = {k: 0 for k in self.E}
        self.seen = {k: {} for k in self.E}
        self.dsems = {}
        self.free_dsems = []
        self.csems = []
        self.ninst = 0
        self.nwait = 0
        self.uid = 0

    def view(self, name, ap):
        self.uid += 1
        return Buf('%s_%d' % (name, self.uid), ap)

    def ps(self, name, shape=(128, 512), dtype=F32):
        return Buf(name, self.nc.alloc_psum_tensor(name, list(shape), dtype))

    def dram(self, name):
        return Buf(name, None)

    def _wait(self, e, toks):
        best = {}
        for t in toks:
            n = t[2]
            if n not in best or best[n][1] < t[1]:
                best[n] = t
        se = self.seen[e]
        for t in sorted(best.values(), key=lambda t: -len(t[3])):
            s, v, n, snap = t
            if se.get(n, 0) < v:
                self.E[e].wait_ge(s, v)
                se[n] = v
                self.ninst += 1
                self.nwait += 1
            for k, kv in snap.items():
                if se.get(k, 0) < kv:
                    se[k] = kv

    def _deps(self, e, reads, writes):
        toks = []
        for b in reads:
            if b.w is not None:
                toks.append(b.w)
        for b in writes:
            if b.w is not None:
                toks.append(b.w)
            toks.extend(b.r.values())
        if e == 'pe':
            toks = [t for t in toks if t[2] != 'c_pe']
        return toks

    def _commit(self, tok, reads, writes):
        for b in writes:
            b.w = tok
            b.r = {}
        for b in reads:
            if b in writes:
                continue
            b.r[tok[2]] = tok

    def op(self, e, fn, reads=(), writes=()):
        self._wait(e, self._deps(e, reads, writes))
        ins = fn(self.E[e])
        self.cnt[e] += 1
        ins.then_inc(self.sem[e], 1)
        self.ninst += 1
        snap = dict(self.seen[e])
        snap['c_' + e] = self.cnt[e]
        self._commit((self.sem[e], self.cnt[e], 'c_' + e, snap), reads, writes)
        return ins

    def _dsem(self, key):
        if key not in self.dsems:
            if self.free_dsems:
                ent = self.free_dsems.pop()
            else:
                ent = [self.nc.alloc_semaphore('d%d' % len(self.dsems)), 0, 'd%d' % len(self.dsems)]
            self.dsems[key] = ent
        return self.dsems[key]

    def dma(self, q, out, in_, reads=(), writes=(), owner=None, **kw):
        if owner is None:
            owner = [b for b in list(writes) + list(reads) if b.t is not None][0]
        kind = 'l' if owner in writes else 's'
        self._wait(q, self._deps(q, reads, writes))
        ins = self.E[q].dma_start(out=out, in_=in_, **kw)
        ent = self._dsem(kind + owner.name)
        ent[1] += 16
        ins.then_inc(ent[0], 16)
        self.ninst += 1
        self._commit((ent[0], ent[1], ent[2], dict(self.seen[q])), reads, writes)
        return ins

    def custom(self, e, ins_fn, sem_inc, reads=(), writes=()):
        self._wait(e, self._deps(e, reads, writes))
        ins = ins_fn(self.E[e])
        self.uid += 1
        nm = 'cc%d' % self.uid
        ent = [self.nc.alloc_semaphore(nm), sem_inc, nm]
        self.csems.append(ent)
        ins.then_inc(ent[0], sem_inc)
        self.ninst += 1
        self._commit((ent[0], ent[1], ent[2], dict(self.seen[e])), reads, writes)
        return ins

    def drain(self, e='sp'):
        toks = [(v[0], v[1], v[2], {}) for v in list(self.dsems.values()) + self.csems if v[1]]
        for k in self.E:
            if self.cnt[k]:
                toks.append((self.sem[k], self.cnt[k], 'c_' + k, {}))
        self._wait(e, toks)

    def barrier(self):
        self.drain('sp')
        self.nc.all_engine_barrier()
        for e in self.E:
            self.seen[e] = dict(self.seen['sp'])
        for k in list(self.dsems.keys()):
            self.free_dsems.append(self.dsems.pop(k))


class Arena:
    def __init__(self, b, nc, nwords, name):
        self.b = b
        self.nc = nc
        self.n = nwords
        nc.alloc_sbuf_tensor(name, [128, nwords], F32)
        self.base = nc.sbuf_base - nwords * 4
        self.off = 0

    def reset(self):
        self.off = 0

    def take(self, name, words, shape=None, dtype=F32, parts=128):
        words = (words + 7) // 8 * 8
        assert self.off + words <= self.n, (name, self.off, words, self.n)
        nel = words * (2 if dtype == BF16 else 1)
        if shape is None:
            shp = [parts, nel]
        else:
            shp = [parts] + list(shape[1:])
            assert int(np.prod(shp[1:])) <= nel, (name, shp, nel)
        self.b.uid += 1
        t = self.nc.alloc_sbuf_tensor_at('%s_%d' % (name, self.b.uid), shp, dtype, offset=self.base + self.off * 4)
        self.off += words
        return Buf('%s_%d' % (name, self.b.uid), t)


def r32(ap):
    return ap.bitcast(F32R)


class WSplit:
    def __init__(self, secs):
        self.secs = secs
        self.bufs = {}

    def buf_for(self, row):
        for (r0, rn, t) in self.secs:
            if r0 <= row < r0 + rn:
                return self.bufs[r0]
        raise KeyError(row)

    def __getitem__(self, key):
        rs, cs = key
        for (r0, rn, t) in self.secs:
            if r0 <= rs.start and rs.stop <= r0 + rn:
                return t[rs.start - r0:rs.stop - r0, cs]
        raise KeyError(key)


class Prog:
    def __init__(self, nlayers=DEPTH, debug=()):
        self.debug = set(debug)
        nc = bass.Bass("TRN2", target_bir_lowering=False)
        nc.dge_precook = False
        self.nc = nc
        self.nl = nlayers
        dt = nc.dram_tensor

        def ext_in(name, shape, dtype=F32):
            return dt(name, list(shape), dtype, kind="ExternalInput").ap()

        def ext_out(name, shape, dtype=F32):
            return dt(name, list(shape), dtype, kind="ExternalOutput").ap()

        def internal(name, shape, dtype=F32):
            return dt(name, list(shape), dtype)

        self.xin = ext_in("xin", [D, NT])
        self.memT = ext_in("memT", [D, NMEM])
        self.w4s = ext_in("w4s", [nlayers, R4 // 8, 4096])
        self.w2s = ext_in("w2s", [nlayers, R2 // 8, 2048])
        self.vec16 = ext_in("vec16", [nlayers, 128, 64])
        self.fnorm = ext_in("fnorm", [128, 16])
        self.lrup = ext_in("lrup", [nlayers, 128, 64])
        self.wai = ext_in("wai", [nlayers, 128, 2048])
        self.lnp = ext_in("lnp", [nlayers, 64, 2])
        self.mtab = ext_in("mtab", [128, 8 * 2 * 128])
        self.b31 = ext_in("b31", [1, 8])
        self.qposr = ext_in("qposr", [1, NT])
        self.qposc = ext_in("qposc", [128, 16])
        self.sel = ext_in("sel", [1, 8])
        self.yout = ext_out("yout", [D, NT])
        self.xout = ext_out("xout", [D, NT]) if nlayers == 1 else None
        self.W4 = [WSplit([(r0, rn, internal("W4_%d_%d" % (l, r0), [rn, 4096])) for (r0, rn) in SEC4]) for l in range(nlayers)]
        self.W2 = [WSplit([(r0, rn, internal("W2_%d_%d" % (l, r0), [rn, 2048])) for (r0, rn) in SEC2]) for l in range(nlayers)]
        self.w4b = internal("w4b", [nlayers, R4 // 8, 4096])
        self.w2b = internal("w2b", [nlayers, R2 // 8, 2048])
        self.xres = internal("xres", [D, NT]).ap()
        self.qaT = internal("qaT", [1024, NT], BF16).ap()
        self.kT_own = internal("kT_own", [256, NT], BF16)
        self.kT_all = internal("kT_all", [4 * 256, NT], BF16)
        self.v_own = internal("v_own", [NT, 256], BF16)
        self.v_all = internal("v_all", [4 * NT, 256], BF16)
        self.ki_own = internal("ki_own", [64, NT])
        self.ki_all = internal("ki_all", [4 * 64, NT])
        self.tail_own = internal("tail_own", [128, 32])
        self.tail_all = internal("tail_all", [4 * 128, 32])
        self.ends_own = internal("ends_own", [128, 16])
        self.ends_all = internal("ends_all", [4 * 128, 16])
        self.qiT = internal("qiT", [256, NT]).ap()
        self.ws = internal("ws", [NT, 4]).ap()
        self.xlT = internal("xlT", [1024, NT]).ap()
        self.glT = internal("glT", [1024, NT]).ap()
        self.qmT = internal("qmT", [1024, NT], BF16).ap()
        self.gtT = internal("gtT", [6144, NT]).ap()
        self.hlT = internal("hlT", [1024, NT]).ap()
        self.pcT = internal("pcT", [1024, NT]).ap()
        self.yaT = internal("yaT", [1024, NT]).ap()
        self.ybT = internal("ybT", [1024, NT]).ap()
        self.ycT = internal("ycT", [1024, NT]).ap()
        self.mskT = internal("mskT", [64, 128, NT], BF16).ap()
        self.dbg = {}
        for name, shape in (("dbg_x1", [D, NT]), ("dbg_ki", [256, NT]), ("dbg_ya", [1024, NT]), ("dbg_yb", [1024, NT]),
                            ("dbg_yc", [1024, NT]), ("dbg_x2", [D, NT])):
            if name in self.debug:
                self.dbg[name] = ext_out(name, shape)

        with nc.cleanup_on_exit():
            self.b = B(nc)
            self.build()
            self.b.barrier()

    def build(self):
        b, nc = self.b, self.nc
        ARW = 49664
        self.ar = Arena(b, nc, ARW, "arena")
        self.pa = Arena(b, nc, 3072, "persist")
        pa = self.pa
        self.ps = [b.ps('ps%d' % i) for i in range(8)]
        self.ones0 = pa.take('ones0', 128)
        self.ones = pa.take('ones', 128)
        self.epsb = pa.take('epsb', 8)
        self.zero = pa.take('zero', 512)
        self.iota = pa.take('iota', 512)
        self.pidx = pa.take('pidx', 8)
        self.qpc = pa.take('qpc', 16)
        self.selt = pa.take('selt', 8)
        self.b31t = pa.take('b31t', 8)
        self.fng = pa.take('fng', 16)
        self.v16 = pa.take('v16', 64)
        self.lrp = pa.take('lrp', 64)
        self.clru = pa.take('clru', 8)
        self.lnpt = pa.take('lnpt', 8, parts=64)
        self.small = pa.take('small', 256)
        self.ident = pa.take('ident', 64, shape=None, dtype=BF16)
        self.identf = pa.take('identf', 128)
        b.op('dve', lambda e: e.memset(self.ones0[:], 1.0), writes=[self.ones0])
        b.op('dve', lambda e: e.tensor_copy(out=r32(self.ones[:]), in_=self.ones0[:]), reads=[self.ones0], writes=[self.ones])
        b.op('dve', lambda e: e.memset(self.epsb[:], EPS), writes=[self.epsb])
        b.op('dve', lambda e: e.memset(self.zero[:], 0.0), writes=[self.zero])
        b.op('pool', lambda e: e.iota(self.iota[:], pattern=[[1, 512]], base=0, channel_multiplier=0,
                                      allow_small_or_imprecise_dtypes=True), writes=[self.iota])
        b.op('pool', lambda e: e.iota(self.pidx[:, 0:1], pattern=[[0, 1]], base=0, channel_multiplier=1,
                                      allow_small_or_imprecise_dtypes=True), writes=[self.pidx])
        b.op('dve', lambda e: e.tensor_scalar(out=self.identf[:], in0=self.iota[:, 0:128], scalar1=self.pidx[:, 0:1],
                                              scalar2=None, op0=ALU.is_equal), reads=[self.iota, self.pidx], writes=[self.identf])
        b.op('dve', lambda e: e.tensor_copy(out=self.ident[:], in_=self.identf[:]), reads=[self.identf], writes=[self.ident])
        b.dma('pool', self.qpc[:], self.qposc, writes=[self.qpc])
        b.dma('pool', self.selt[:], self.sel[0].partition_broadcast(128), writes=[self.selt])
        b.dma('pool', self.b31t[:], self.b31[0].partition_broadcast(128), writes=[self.b31t])
        b.dma('pool', self.fng[:], self.fnorm, writes=[self.fng])

        g8 = [list(range(NCORES))]
        wtmp = b.dram('wtmp')
        for l in range(self.nl):
            for (src, dst, nr) in ((self.w4s, self.w4b, R4 // 8), (self.w2s, self.w2b, R2 // 8)):
                for r0 in range(0, nr, 256):
                    r1 = min(nr, r0 + 256)
                    b.dma('sp', dst[l, r0:r1, :], src[l, r0:r1, :], reads=[], writes=[wtmp], owner=self.small)
        order = [(4, SEC4[0]), (2, SEC2[0]), (4, SEC4[1]), (4, SEC4[2]), (4, SEC4[3]), (4, SEC4[4]), (4, SEC4[5]), (2, SEC2[1])]
        for l in range(self.nl):
            for which, (r0, rn) in order:
                src = (self.w4b if which == 4 else self.w2b)
                dst = (self.W4[l] if which == 4 else self.W2[l])
                wbuf = b.dram('W%d_%d_%d' % (which, l, r0))
                dst.bufs[r0] = wbuf
                i_ap = src[l, r0 // 8:(r0 + rn) // 8, :]
                o_ap = dst[r0:r0 + rn, :]
                b.custom('pool', lambda e, i_ap=i_ap, o_ap=o_ap: e.collective_compute(
                    "AllGather", ALU.bypass, replica_groups=g8, ins=[i_ap.opt()], outs=[o_ap.opt()]), 1,
                    reads=[wtmp], writes=[wbuf])

        for l in range(self.nl):
            self.layer(l)
        self.final_norm()

    def rmsnorm_T(self, xt, hT, g_ap, gbuf, n=TT):
        b = self.b
        ps_s = self.ps[6]
        b.op('act', lambda e: e.activation(out=r32(hT[:, :, 0:n]), in_=xt[:, :, 0:n], func=AF.Square), reads=[xt], writes=[hT])
        for k in range(KC):
            b.op('pe', lambda e, k=k: e.matmul(ps_s[:, 0:n], r32(self.ones[:]), r32(hT[:, k, 0:n]), start=(k == 0), stop=(k == KC - 1)),
                 reads=[self.ones, hT], writes=[ps_s])
        b.op('act', lambda e: e.activation(out=self.rs[:, 0:n], in_=ps_s[:, 0:n], func=AF.Sqrt, bias=self.epsb[:, 0:1], scale=1.0 / D),
             reads=[ps_s, self.epsb], writes=[self.rs])
        b.op('dve', lambda e: e.reciprocal(out=self.rstd[:, 0:n], in_=self.rs[:, 0:n]), reads=[self.rs], writes=[self.rstd])
        for k in range(KC):
            b.op('dve', lambda e, k=k: e.scalar_tensor_tensor(out=r32(hT[:, k, 0:n]), in0=xt[:, k, 0:n], scalar=g_ap[:, k:k + 1],
                                                              in1=self.rstd[:, 0:n], op0=ALU.mult, op1=ALU.mult),
                 reads=[xt, gbuf, self.rstd], writes=[hT])

    def wload(self, wb, W, row0, nrows=128, width=4096, q='sp'):
        self.b.dma(q, r32(wb[:, 0:width]), r32(W[row0:row0 + nrows, :]), reads=[W.buf_for(row0)], writes=[wb])

    def ffn_tile(self, xt, hT, l, which):
        b = self.b
        W4, W2 = self.W4[l], self.W2[l]
        rg = R_GU1 if which == 1 else R_GU2
        rd = 0 if which == 1 else 5504
        G = 4
        groups = [list(range(s, min(s + G, FC))) for s in range(0, FC, G)]
        it = 0
        for gi, grp in enumerate(groups):
            ag = self.actg[gi % 2]
            wds = []
            for jj, j in enumerate(grp):
                wb = self.wb[self.wbi % 3]
                self.wbi += 1
                self.wload(wb, W4, rg + j * 128)
                wd = self.wd[self.wdi % 6]
                self.wdi += 1
                self.wload(wd, W2, rd + j * 128, width=2048)
                wds.append(wd)
                pg, pu = self.ps[(it % 2) * 2], self.ps[(it % 2) * 2 + 1]
                wv = wb[:].rearrange("p (k c) -> p k c", k=KC)
                for k in range(KC):
                    b.op('pe', lambda e, k=k: e.matmul(pg[:], r32(wv[:, k, 0:128]), r32(hT[:, k, :]), start=(k == 0), stop=(k == KC - 1)),
                         reads=[wb, hT], writes=[pg])
                for k in range(KC):
                    b.op('pe', lambda e, k=k: e.matmul(pu[:], r32(wv[:, k, 128:256]), r32(hT[:, k, :]), start=(k == 0), stop=(k == KC - 1)),
                         reads=[wb, hT], writes=[pu])
                sg = self.sg[it % 2]
                b.op('act', lambda e: e.activation(out=sg[:], in_=pg[:], func=AF.Silu), reads=[pg], writes=[sg])
                b.op('dve', lambda e, jj=jj: e.tensor_tensor(out=r32(ag[:, jj, :]), in0=sg[:], in1=pu[:], op=ALU.mult),
                     reads=[sg, pu], writes=[ag])
                it += 1
            for d in range(KC):
                pd = self.ps[4 + d % 2]
                for jj in range(len(grp)):
                    b.op('pe', lambda e, jj=jj, d=d: e.matmul(pd[:], r32(wds[jj][:, d * 128:(d + 1) * 128]), r32(ag[:, jj, :]),
                                                              start=(jj == 0), stop=(jj == len(grp) - 1)),
                         reads=[wds[jj], ag], writes=[pd])
                b.op('dve', lambda e, d=d: e.scalar_tensor_tensor(out=xt[:, d, :], in0=pd[:], scalar=0.5, in1=xt[:, d, :],
                                                                  op0=ALU.mult, op1=ALU.add),
                     reads=[pd, xt], writes=[xt])

    def ffn_bufs(self):
        ar = self.ar
        ar.reset()
        self.xt = ar.take('xt', KC * TT, (128, KC, TT))
        self.hT = ar.take('hT', KC * TT, (128, KC, TT))
        self.wb = [ar.take('wb%d' % i, 4096) for i in range(3)]
        self.wd = [ar.take('wd%d' % i, 2048) for i in range(6)]
        self.actg = [ar.take('ag%d' % i, 4 * TT, (128, 4, TT)) for i in range(2)]
        self.sg = [ar.take('sg%d' % i, TT) for i in range(2)]
        self.stage = [ar.take('st%d' % i, TT) for i in range(3)]
        self.rs = ar.take('rs', TT)
        self.rstd = ar.take('rstd', TT)
        self.kxr = ar.take('kxr', TT)
        self.sqr = ar.take('sqr', TT)
        self.wbi = 0
        self.wdi = 0

    def xview(self, ap, tt):
        return ap.rearrange("(k p) t -> p k t", p=128)[:, :, tt * TT:(tt + 1) * TT]

    def layer(self, l):
        b = self.b
        b.dma('pool', self.v16[:], self.vec16[l], writes=[self.v16])
        b.dma('pool', self.lrp[:], self.lrup[l], writes=[self.lrp])
        b.dma('pool', self.lnpt[:, 0:2], self.lnp[l], writes=[self.lnpt])
        self.phase_a(l)
        b.barrier()
        self.exchange1()
        b.barrier()
        self.phase_lru1(l)
        b.barrier()
        self.exchange2()
        b.barrier()
        self.phase_lru2(l)
        b.barrier()
        self.phase_sel(l)
        b.barrier()
        self.phase_attn(l)
        b.barrier()
        self.phase_mem(l)
        b.barrier()
        self.phase_merge(l)
        b.barrier()

    def phase_a(self, l):
        b = self.b
        self.ffn_bufs()
        xsrc = self.xin if l == 0 else self.xres
        xd = b.dram('xd')
        pd_ = b.dram('projd')
        xt, hT = self.xt, self.hT
        lng, lnb = self.lnpt[:, 0:1], self.lnpt[:, 1:2]
        W4 = self.W4[l]
        att_scale = 128 ** -0.5
        for tt in range(NTT):
            tsl = slice(tt * TT, (tt + 1) * TT)
            b.dma('pool', xt[:], self.xview(xsrc, tt), reads=[xd], writes=[xt])
            self.rmsnorm_T(xt, hT, self.v16[:, 0:16], self.v16)
            self.ffn_tile(xt, hT, l, 1)
            b.dma('pool', self.xview(self.xres, tt), xt[:], reads=[xt], writes=[xd])
            if l == 0 and 'dbg_x1' in self.dbg:
                b.dma('pool', self.xview(self.dbg['dbg_x1'], tt), xt[:], reads=[xt], writes=[xd])
            self.rmsnorm_T(xt, hT, self.v16[:, 16:32], self.v16)
            si = 0
            for ti in range(NCH // 2):
                wb = self.wb[self.wbi % 3]
                self.wbi += 1
                self.wload(wb, W4, R_WIN + ti * 128)
                wv = wb[:].rearrange("p (k c) -> p k c", k=KC)
                for h in range(2):
                    ch = ti * 2 + h
                    if CH_VA <= ch < CH_VA + 2 or ch == CH_WI:
                        if ch == CH_VA + 1:
                            continue
                        ncol = 256 if ch == CH_VA else 4
                        c0 = 0 if ch == CH_VA else 128
                        for ts in range(TT // 128):
                            pp = self.ps[(ts % 2) * 2]
                            for k in range(KC):
                                b.op('pe', lambda e, k=k, ts=ts: e.matmul(pp[:, 0:ncol], r32(hT[:, k, ts * 128:(ts + 1) * 128]),
                                                                          r32(wv[:, k, c0:c0 + ncol]), start=(k == 0), stop=(k == KC - 1)),
                                     reads=[wb, hT], writes=[pp])
                            st = self.stage[si % 3]
                            si += 1
                            r0 = tt * TT + ts * 128
                            if ch == CH_VA:
                                stb = st[:].bitcast(BF16)
                                b.op('act', lambda e: e.copy(out=stb[:, 0:256], in_=pp[:, 0:256]), reads=[pp], writes=[st])
                                b.dma('pool', self.v_own[r0:r0 + 128, :], stb[:, 0:256], reads=[st], writes=[pd_])
                            else:
                                b.op('act', lambda e: e.activation(out=st[:, 0:4], in_=pp[:, 0:4], func=AF.Copy, scale=0.5 * 0.125),
                                     reads=[pp], writes=[st])
                                b.dma('pool', self.ws[r0:r0 + 128, :], st[:, 0:4], reads=[st], writes=[pd_])
                        continue
                    pp = self.ps[(ch % 2) * 2]
                    for k in range(KC):
                        b.op('pe', lambda e, k=k, h=h: e.matmul(pp[:], r32(wv[:, k, h * 128:(h + 1) * 128]), r32(hT[:, k, :]),
                                                                start=(k == 0), stop=(k == KC - 1)),
                             reads=[wb, hT], writes=[pp])
                    st = self.stage[si % 3]
                    si += 1
                    if ch < CH_KA:
                        stb = st[:].bitcast(BF16)
                        b.op('act', lambda e: e.activation(out=stb[:, 0:TT], in_=pp[:], func=AF.Copy, scale=att_scale), reads=[pp], writes=[st])
                        b.dma('pool', self.qaT[ch * 128:(ch + 1) * 128, tsl], stb[:, 0:TT], reads=[st], writes=[pd_])
                    elif ch < CH_VA:
                        stb = st[:].bitcast(BF16)
                        b.op('act', lambda e: e.copy(out=stb[:, 0:TT], in_=pp[:]), reads=[pp], writes=[st])
                        c = ch - CH_KA
                        b.dma('pool', self.kT_own[c * 128:(c + 1) * 128, tsl], stb[:, 0:TT], reads=[st], writes=[pd_])
                    elif ch < CH_KI:
                        b.op('act', lambda e: e.copy(out=st[:], in_=pp[:]), reads=[pp], writes=[st])
                        c = ch - CH_QI
                        b.dma('pool', self.qiT[c * 128:(c + 1) * 128, tsl], st[:], reads=[st], writes=[pd_])
                    elif ch == CH_KI:
                        kx, xc_, sq_, kr_, sr_ = self.stage[0], self.stage[1], self.stage[2], self.kxr, self.sqr
                        si = 0
                        p2 = self.ps[6]
                        b.op('act', lambda e: e.copy(out=r32(kr_[0:64, :]), in_=pp[0:64, :]), reads=[pp], writes=[kr_])
                        b.op('act', lambda e: e.copy(out=kx[0:64, :], in_=pp[0:64, :]), reads=[pp], writes=[kx])
                        b.op('pe', lambda e: e.matmul(p2[0:64, :], r32(self.ones[0:64, 0:64]), r32(kr_[0:64, :]), start=True, stop=True),
                             reads=[self.ones, kr_], writes=[p2])
                        b.op('dve', lambda e: e.scalar_tensor_tensor(out=xc_[0:64, :], in0=p2[0:64, :], scalar=-1.0 / 64, in1=kx[0:64, :],
                                                                     op0=ALU.mult, op1=ALU.add), reads=[p2, kx], writes=[xc_])
                        b.op('act', lambda e: e.activation(out=r32(sr_[0:64, :]), in_=xc_[0:64, :], func=AF.Square), reads=[xc_], writes=[sr_])
                        b.op('pe', lambda e: e.matmul(p2[0:64, :], r32(self.ones[0:64, 0:64]), r32(sr_[0:64, :]), start=True, stop=True),
                             reads=[self.ones, sr_], writes=[p2])
                        b.op('act', lambda e: e.activation(out=sq_[0:64, :], in_=p2[0:64, :], func=AF.Sqrt, bias=self.epsb[0:64, 0:1], scale=1.0 / 64),
                             reads=[p2, self.epsb], writes=[sq_])
                        b.op('dve', lambda e: e.reciprocal(out=kx[0:64, :], in_=sq_[0:64, :]), reads=[sq_], writes=[kx])
                        b.op('dve', lambda e: e.tensor_tensor(out=xc_[0:64, :], in0=xc_[0:64, :], in1=kx[0:64, :], op=ALU.mult),
                             reads=[xc_, kx], writes=[xc_])
                        b.op('dve', lambda e: e.tensor_scalar(out=xc_[0:64, :], in0=xc_[0:64, :], scalar1=lng[0:64, :], scalar2=lnb[0:64, :],
                                                              op0=ALU.mult, op1=ALU.add), reads=[xc_, self.lnpt], writes=[xc_])
                        b.dma('pool', self.ki_own[0:64, tsl], xc_[0:64, :], reads=[xc_], writes=[pd_])
                        if l == 0 and 'dbg_ki' in self.dbg:
                            b.dma('pool', self.dbg['dbg_ki'][0:64, tsl], xc_[0:64, :], reads=[xc_], writes=[pd_])
                    elif ch < CH_GL:
                        b.op('act', lambda e: e.copy(out=st[:], in_=pp[:]), reads=[pp], writes=[st])
                        c = ch - CH_XL
                        b.dma('pool', self.xlT[c * 128:(c + 1) * 128, tsl], st[:], reads=[st], writes=[pd_])
                        if tt == NTT - 1:
                            b.dma('pool', self.tail_own[:, c * 4:(c + 1) * 4], st[:, TT - 4:TT], reads=[st], writes=[pd_])
                    elif ch < CH_QM:
                        b.op('act', lambda e: e.copy(out=st[:], in_=pp[:]), reads=[pp], writes=[st])
                        c = ch - CH_GL
                        b.dma('pool', self.glT[c * 128:(c + 1) * 128, tsl], st[:], reads=[st], writes=[pd_])
                    elif ch < CH_GT:
                        stb = st[:].bitcast(BF16)
                        b.op('act', lambda e: e.activation(out=stb[:, 0:TT], in_=pp[:], func=AF.Copy, scale=1.0 / 16), reads=[pp], writes=[st])
                        c = ch - CH_QM
                        b.dma('pool', self.qmT[c * 128:(c + 1) * 128, tsl], stb[:, 0:TT], reads=[st], writes=[pd_])
                    else:
                        b.op('act', lambda e: e.activation(out=st[:], in_=pp[:], func=AF.Sigmoid), reads=[pp], writes=[st])
                        c = ch - CH_GT
                        b.dma('pool', self.gtT[c * 128:(c + 1) * 128, tsl], st[:], reads=[st], writes=[pd_])

    def exchange1(self):
        b = self.b
        g4 = [[0, 1, 2, 3], [4, 5, 6, 7]]
        x = b.dram('ex1')
        for (i_t, o_t) in ((self.kT_own, self.kT_all), (self.v_own, self.v_all), (self.ki_own, self.ki_all), (self.tail_own, self.tail_all)):
            b.custom('pool', lambda e, i_t=i_t, o_t=o_t: e.collective_compute(
                "AllGather", ALU.bypass, replica_groups=g4, ins=[i_t.ap().opt()], outs=[o_t.ap().opt()]), 1, reads=[], writes=[x])

    def exchange2(self):
        b = self.b
        g4 = [[0, 1, 2, 3], [4, 5, 6, 7]]
        x = b.dram('ex2')
        b.custom('pool', lambda e: e.collective_compute(
            "AllGather", ALU.bypass, replica_groups=g4, ins=[self.ends_own.ap().opt()], outs=[self.ends_all.ap().opt()]), 1, reads=[], writes=[x])

    def phase_lru1(self, l):
        b, ar = self.b, self.ar
        ar.reset()
        T = TT
        xl = ar.take('xl', 8 * (T + 4), (128, 8, T + 4))
        xc = ar.take('xc', 8 * T, (128, 8, T))
        xcf = ar.take('xcf', 8 * T, (128, 8, T))
        sr = ar.take('sr', 8 * T, (128, 8, T))
        si = ar.take('si', 8 * T, (128, 8, T))
        a = ar.take('a', 8 * T, (128, 8, T))
        u = ar.take('u', 8 * T, (128, 8, T))
        hl = ar.take('hl', 8 * T, (128, 8, T))
        pc = ar.take('pc', 8 * T, (128, 8, T))
        wai = ar.take('wai', 2048)
        wair = ar.take('wair', 2048)
        tl = ar.take('tl', 128, (128, 4, 32))
        car = ar.take('car', 16)
        lrp = self.lrp
        cw = [lrp[:, j * 8:(j + 1) * 8] for j in range(4)]
        cb, ba, bi, lam = lrp[:, 32:40], lrp[:, 40:48], lrp[:, 48:56], lrp[:, 56:64]
        d_ = b.dram('lru_d')
        b.dma('pool', wai[:], self.wai[l], writes=[wai])
        b.op('dve', lambda e: e.tensor_copy(out=r32(wair[:]), in_=wai[:]), reads=[wai], writes=[wair])
        wa = r32(wair[:]).rearrange("p (w n d) -> p w n d", w=2, n=8)
        b.op('act', lambda e: e.activation(out=self.clru[:], in_=lam, func=AF.Exp, scale=-1.0), reads=[lrp], writes=[self.clru])
        b.op('act', lambda e: e.activation(out=self.clru[:], in_=self.clru[:], func=AF.Ln, bias=self.ones0[:, 0:1], scale=1.0),
             reads=[self.clru, self.ones0], writes=[self.clru])
        b.op('dve', lambda e: e.tensor_scalar(out=self.clru[:], in0=self.clru[:], scalar1=-8.0, scalar2=None, op0=ALU.mult),
             reads=[self.clru], writes=[self.clru])
        b.dma('pool', tl[:], self.tail_all.ap().rearrange("(r p) q -> p r q", p=128), reads=[d_], writes=[tl])
        halo = xl
        b.op('dve', lambda e: e.tensor_scalar(out=xl[:, :, 0:4], in0=tl[:, 0, :].rearrange("p (n q) -> p n q", q=4), scalar1=self.selt[:, 0:1],
                                              scalar2=None, op0=ALU.mult), reads=[tl, self.selt], writes=[xl])
        for r in range(1, 4):
            b.op('dve', lambda e, r=r: e.scalar_tensor_tensor(out=xl[:, :, 0:4], in0=tl[:, r, :].rearrange("p (n q) -> p n q", q=4),
                                                              scalar=self.selt[:, r:r + 1], in1=xl[:, :, 0:4], op0=ALU.mult, op1=ALU.add),
                 reads=[tl, self.selt, xl], writes=[xl])
        b.op('dve', lambda e: e.memset(car[:, 0:8], 1.0), writes=[car])
        b.op('dve', lambda e: e.memset(car[:, 8:16], 0.0), writes=[car])
        for tt in range(NT // T):
            tsl = slice(tt * T, (tt + 1) * T)
            b.dma('pool', xl[:, :, 4:4 + T], self.xlT.rearrange("(n p) t -> p n t", p=128)[:, :, tsl], reads=[d_], writes=[xl])
            for n in range(8):
                b.op('dve', lambda e, n=n: e.tensor_scalar(out=xcf[:, n, :], in0=xl[:, n, 1:1 + T], scalar1=cw[0][:, n:n + 1], scalar2=cb[:, n:n + 1],
                                                           op0=ALU.mult, op1=ALU.add), reads=[xl, lrp], writes=[xcf])
                for j in range(1, 4):
                    b.op('dve', lambda e, n=n, j=j: e.scalar_tensor_tensor(out=xcf[:, n, :], in0=xl[:, n, j + 1:j + 1 + T], scalar=cw[j][:, n:n + 1],
                                                                           in1=xcf[:, n, :], op0=ALU.mult, op1=ALU.add),
                         reads=[xl, lrp, xcf], writes=[xcf])
            b.op('act', lambda e: e.copy(out=r32(xc[:]), in_=xcf[:]), reads=[xcf], writes=[xc])
            for n in range(8):
                pr, pi = self.ps[(n % 2) * 2], self.ps[(n % 2) * 2 + 1]
                b.op('pe', lambda e, n=n: e.matmul(pr[:], wa[:, 0, n, :], r32(xc[:, n, :]), start=True, stop=True), reads=[wair, xc], writes=[pr])
                b.op('pe', lambda e, n=n: e.matmul(pi[:], wa[:, 1, n, :], r32(xc[:, n, :]), start=True, stop=True), reads=[wair, xc], writes=[pi])
                b.op('act', lambda e, n=n: e.activation(out=sr[:, n, :], in_=pr[:], func=AF.Sigmoid, bias=ba[:, n:n + 1], scale=1.0),
                     reads=[pr, lrp], writes=[sr])
                b.op('act', lambda e, n=n: e.activation(out=si[:, n, :], in_=pi[:], func=AF.Sigmoid, bias=bi[:, n:n + 1], scale=1.0),
                     reads=[pi, lrp], writes=[si])
            for n in range(8):
                b.op('act', lambda e, n=n: e.activation(out=a[:, n, :], in_=sr[:, n, :], func=AF.Exp, scale=self.clru[:, n:n + 1]),
                     reads=[sr, self.clru], writes=[a])
            b.op('dve', lambda e: e.tensor_tensor(out=u[:], in0=a[:], in1=a[:], op=ALU.mult), reads=[a], writes=[u])
            b.op('dve', lambda e: e.tensor_scalar(out=u[:], in0=u[:], scalar1=-1.0, scalar2=1.0, op0=ALU.mult, op1=ALU.add), reads=[u], writes=[u])
            b.op('dve', lambda e: e.tensor_scalar(out=u[:], in0=u[:], scalar1=0.0, scalar2=None, op0=ALU.max), reads=[u], writes=[u])
            b.op('act', lambda e: e.activation(out=u[:], in_=u[:], func=AF.Sqrt), reads=[u], writes=[u])
            b.op('dve', lambda e: e.tensor_tensor(out=u[:], in0=u[:], in1=si[:], op=ALU.mult), reads=[u, si], writes=[u])
            b.op('dve', lambda e: e.tensor_tensor(out=u[:], in0=u[:], in1=xcf[:], op=ALU.mult), reads=[u, xcf], writes=[u])
            for n in range(8):
                b.op('dve', lambda e, n=n: e.tensor_tensor_scan(out=hl[:, n, :], data0=a[:, n, :], data1=u[:, n, :], initial=car[:, 8 + n:9 + n],
                                                                op0=ALU.mult, op1=ALU.add), reads=[a, u, car], writes=[hl])
                b.op('dve', lambda e, n=n: e.tensor_tensor_scan(out=pc[:, n, :], data0=a[:, n, :], data1=self.zero[:, 0:T], initial=car[:, n:n + 1],
                                                                op0=ALU.mult, op1=ALU.add), reads=[a, self.zero, car], writes=[pc])
            b.op('dve', lambda e: e.tensor_copy(out=car[:, 0:8], in_=pc[:, :, T - 1]), reads=[pc], writes=[car])
            b.op('dve', lambda e: e.tensor_copy(out=car[:, 8:16], in_=hl[:, :, T - 1]), reads=[hl], writes=[car])
            b.dma('pool', self.hlT.rearrange("(n p) t -> p n t", p=128)[:, :, tsl], hl[:], reads=[hl], writes=[d_])
            b.dma('pool', self.pcT.rearrange("(n p) t -> p n t", p=128)[:, :, tsl], pc[:], reads=[pc], writes=[d_])
            if tt + 1 < NT // T:
                b.op('dve', lambda e: e.tensor_copy(out=xl[:, :, 0:4], in_=xl[:, :, T:T + 4]), reads=[xl], writes=[xl])
        b.dma('pool', self.ends_own.ap(), car[:], reads=[car], writes=[d_])

    def phase_lru2(self, l):
        b, ar = self.b, self.ar
        ar.reset()
        T = TT
        hl = ar.take('hl', 8 * T, (128, 8, T))
        pc = ar.take('pc', 8 * T, (128, 8, T))
        gl = ar.take('gl', 8 * T, (128, 8, T))
        t1 = ar.take('t1', 8 * T, (128, 8, T))
        en = ar.take('en', 64, (128, 4, 16))
        hin = ar.take('hin', 8)
        hnw = ar.take('hnw', 8)
        d_ = b.dram('lru2_d')
        b.dma('pool', en[:], self.ends_all.ap().rearrange("(r p) q -> p r q", p=128), reads=[d_], writes=[en])
        b.op('dve', lambda e: e.memset(hin[:], 0.0), writes=[hin])
        for r in range(3):
            b.op('dve', lambda e, r=r: e.tensor_tensor(out=hnw[:], in0=en[:, r, 0:8], in1=hin[:], op=ALU.mult), reads=[en, hin], writes=[hnw])
            b.op('dve', lambda e, r=r: e.tensor_tensor(out=hnw[:], in0=hnw[:], in1=en[:, r, 8:16], op=ALU.add), reads=[en, hnw], writes=[hnw])
            b.op('dve', lambda e: e.tensor_tensor(out=hnw[:], in0=hnw[:], in1=hin[:], op=ALU.subtract), reads=[hnw, hin], writes=[hnw])
            b.op('dve', lambda e, r=r: e.scalar_tensor_tensor(out=hin[:], in0=hnw[:], scalar=self.selt[:, 4 + r:5 + r], in1=hin[:],
                                                              op0=ALU.mult, op1=ALU.add), reads=[hnw, self.selt, hin], writes=[hin])
        c1 = math.sqrt(2.0 / math.pi)
        for tt in range(NT // T):
            tsl = slice(tt * T, (tt + 1) * T)
            for (dst, src) in ((hl, self.hlT), (pc, self.pcT), (gl, self.glT)):
                b.dma('pool', dst[:], src.rearrange("(n p) t -> p n t", p=128)[:, :, tsl], reads=[d_], writes=[dst])
            for n in range(8):
                b.op('dve', lambda e, n=n: e.scalar_tensor_tensor(out=hl[:, n, :], in0=pc[:, n, :], scalar=hin[:, n:n + 1], in1=hl[:, n, :],
                                                                  op0=ALU.mult, op1=ALU.add), reads=[pc, hin, hl], writes=[hl])
            b.op('act', lambda e: e.activation(out=t1[:], in_=gl[:], func=AF.Square), reads=[gl], writes=[t1])
            b.op('dve', lambda e: e.tensor_scalar(out=t1[:], in0=t1[:], scalar1=0.044715, scalar2=1.0, op0=ALU.mult, op1=ALU.add), reads=[t1], writes=[t1])
            b.op('dve', lambda e: e.tensor_tensor(out=t1[:], in0=t1[:], in1=gl[:], op=ALU.mult), reads=[t1, gl], writes=[t1])
            b.op('act', lambda e: e.activation(out=t1[:], in_=t1[:], func=AF.Sigmoid, scale=2.0 * c1), reads=[t1], writes=[t1])
            b.op('dve', lambda e: e.tensor_tensor(out=t1[:], in0=t1[:], in1=gl[:], op=ALU.mult), reads=[t1, gl], writes=[t1])
            b.op('dve', lambda e: e.tensor_tensor(out=t1[:], in0=t1[:], in1=hl[:], op=ALU.mult), reads=[t1, hl], writes=[t1])
            b.dma('pool', self.ybT.rearrange("(n p) t -> p n t", p=128)[:, :, tsl], t1[:], reads=[t1], writes=[d_])
            if l == 0 and 'dbg_yb' in self.dbg:
                b.dma('pool', self.dbg['dbg_yb'].rearrange("(n p) t -> p n t", p=128)[:, :, tsl], t1[:], reads=[t1], writes=[d_])

    def phase_sel(self, l):
        b, ar = self.b, self.ar
        ar.reset()
        KI = ar.take('KI', S, parts=64)
        sc = ar.take('sc', S)
        pf = ar.take('pf', S)
        mA = ar.take('mA', S // 2, dtype=BF16)
        mC = ar.take('mC', S // 2, dtype=BF16)
        qi = ar.take('qi', 4 * 128, (64, 4, 128), parts=64)
        qir = ar.take('qir', 4 * 128, (64, 4, 128), parts=64)
        rl = [ar.take('rl%d' % i, 512) for i in range(3)]
        wst = ar.take('wst', 64, (128, 16, 4))
        wab = ar.take('wab', 64, (128, 16, 4))
        wsg = ar.take('wsg', 64, (128, 16, 4))
        qrel = ar.take('qrel', 256, (128, 16, 16))
        sm = ar.take('sm', 16)
        tst = [ar.take('tst%d' % i, 512, dtype=BF16) for i in range(2)]
        d_ = b.dram('sel_d')
        for r in range(4):
            b.dma('pool', r32(KI[0:64, r * NT:(r + 1) * NT]), r32(self.ki_all[r * 64:(r + 1) * 64, :]), reads=[d_], writes=[KI])
        b.dma('pool', wst[:], self.ws.rearrange("(i p) h -> p i h", p=128), reads=[d_], writes=[wst])
        b.op('act', lambda e: e.activation(out=wab[:], in_=wst[:], func=AF.Abs), reads=[wst], writes=[wab])
        b.op('act', lambda e: e.activation(out=wsg[:], in_=wst[:], func=AF.Sign), reads=[wst], writes=[wsg])
        for cc in range(16):
            b.op('dve', lambda e, cc=cc: e.tensor_scalar(out=qrel[:, :, cc], in0=self.qpc[:], scalar1=-512.0 * cc, scalar2=None, op0=ALU.add),
                 reads=[self.qpc], writes=[qrel])
        lo, hi, mid, cnt, mm, tmp, need = (sm[:, i:i + 1] for i in range(7))
        pst = self.ps[7]
        pT = self.ps[5]
        for i in range(NT // 128):
            b.dma('pool', qi[:], self.qiT.rearrange("(h d) t -> d h t", d=64)[:, :, i * 128:(i + 1) * 128], reads=[d_], writes=[qi])
            b.op('dve', lambda e: e.tensor_copy(out=r32(qir[:]), in_=qi[:]), reads=[qi], writes=[qir])
            for cc in range(16):
                csl = slice(cc * 512, (cc + 1) * 512)
                b.op('dve', lambda e, cc=cc, csl=csl, i=i: e.tensor_scalar(out=sc[:, csl], in0=self.iota[:], scalar1=qrel[:, i, cc:cc + 1], scalar2=NEG,
                                                                          op0=ALU.is_gt, op1=ALU.mult), reads=[self.iota, qrel], writes=[sc])
                for h in range(4):
                    pp = self.ps[h % 4]
                    b.op('pe', lambda e, h=h, csl=csl: e.matmul(pp[:], r32(qir[0:64, h, :]), r32(KI[0:64, csl]), start=True, stop=True),
                         reads=[qir, KI], writes=[pp])
                    rr = rl[h % 3]
                    b.op('act', lambda e, h=h, i=i: e.activation(out=rr[:], in_=pp[:], func=AF.Relu, scale=wab[:, i, h:h + 1]),
                         reads=[pp, wab], writes=[rr])
                    b.op('dve', lambda e, h=h, i=i, csl=csl: e.scalar_tensor_tensor(out=sc[:, csl], in0=rr[:], scalar=wsg[:, i, h:h + 1], in1=sc[:, csl],
                                                                                    op0=ALU.mult, op1=ALU.add), reads=[rr, wsg, sc], writes=[sc])
            b.op('dve', lambda e: e.tensor_reduce(out=hi, in_=sc[:], axis=AX.X, op=ALU.max), reads=[sc], writes=[sm])
            b.op('dve', lambda e: e.tensor_scalar(out=pf[:], in0=sc[:], scalar1=0.5 * NEG, scalar2=-2.0 * NEG, op0=ALU.is_lt, op1=ALU.mult),
                 reads=[sc], writes=[pf])
            b.op('dve', lambda e: e.tensor_tensor(out=pf[:], in0=pf[:], in1=sc[:], op=ALU.add), reads=[pf, sc], writes=[pf])
            b.op('dve', lambda e: e.tensor_reduce(out=lo, in_=pf[:], axis=AX.X, op=ALU.min), reads=[pf], writes=[sm])
            b.op('dve', lambda e: e.tensor_scalar(out=lo, in0=lo, scalar1=-1.0, scalar2=None, op0=ALU.add), reads=[sm], writes=[sm])
            for it in range(BISECT):
                if it == 0:
                    b.op('dve', lambda e: e.memset(mid, 0.0), writes=[sm])
                    b.op('dve', lambda e: e.tensor_tensor(out=mid, in0=mid, in1=lo, op=ALU.max), reads=[sm], writes=[sm])
                    b.op('dve', lambda e: e.tensor_tensor(out=mid, in0=mid, in1=hi, op=ALU.min), reads=[sm], writes=[sm])
                else:
                    b.op('dve', lambda e: e.tensor_tensor(out=mid, in0=lo, in1=hi, op=ALU.add), reads=[sm], writes=[sm])
                    b.op('dve', lambda e: e.tensor_scalar(out=mid, in0=mid, scalar1=0.5, scalar2=None, op0=ALU.mult), reads=[sm], writes=[sm])
                b.op('dve', lambda e: e.tensor_scalar(out=mC[:], in0=sc[:], scalar1=mid, scalar2=0.0, op0=ALU.is_gt, op1=ALU.add, accum_out=cnt),
                     reads=[sc, sm], writes=[mC, sm])
                b.op('dve', lambda e: e.tensor_scalar(out=mm, in0=cnt, scalar1=float(TOPK), scalar2=None, op0=ALU.is_gt), reads=[sm], writes=[sm])
                b.op('dve', lambda e: e.tensor_tensor(out=tmp, in0=mid, in1=lo, op=ALU.subtract), reads=[sm], writes=[sm])
                b.op('dve', lambda e: e.scalar_tensor_tensor(out=lo, in0=tmp, scalar=mm, in1=lo, op0=ALU.mult, op1=ALU.add), reads=[sm], writes=[sm])
                b.op('dve', lambda e: e.tensor_tensor(out=tmp, in0=hi, in1=mid, op=ALU.subtract), reads=[sm], writes=[sm])
                b.op('dve', lambda e: e.scalar_tensor_tensor(out=hi, in0=tmp, scalar=mm, in1=mid, op0=ALU.mult, op1=ALU.add), reads=[sm], writes=[sm])
            b.op('dve', lambda e: e.tensor_scalar(out=mA[:], in0=sc[:], scalar1=hi, scalar2=0.0, op0=ALU.is_gt, op1=ALU.add, accum_out=cnt),
                 reads=[sc, sm], writes=[mA, sm])
            b.op('dve', lambda e: e.tensor_scalar(out=need, in0=cnt, scalar1=-1.0, scalar2=float(TOPK), op0=ALU.mult, op1=ALU.add), reads=[sm], writes=[sm])
            b.op('dve', lambda e: e.scalar_tensor_tensor(out=mC[:], in0=sc[:], scalar=lo, in1=mA[:], op0=ALU.is_gt, op1=ALU.subtract),
                 reads=[sc, sm, mA], writes=[mC])
            b.op('dve', lambda e: e.tensor_tensor_scan(out=pf[:], data0=mC[:], data1=mC[:], initial=0.0, op0=ALU.add, op1=ALU.max),
                 reads=[mC], writes=[pf])
            b.op('dve', lambda e: e.scalar_tensor_tensor(out=mC[:], in0=pf[:], scalar=need, in1=mC[:], op0=ALU.is_le, op1=ALU.mult),
                 reads=[pf, sm, mC], writes=[mC])
            b.op('dve', lambda e: e.tensor_tensor(out=mA[:], in0=mA[:], in1=mC[:], op=ALU.add), reads=[mA, mC], writes=[mA])
            for jg in range(8):
                ts_ = tst[jg % 2]
                pTb = pT.t[:].bitcast(BF16)
                for jj in range(8):
                    j = jg * 8 + jj
                    b.op('pe', lambda e, j=j, jj=jj: e.transpose(pTb[:, jj * 128:(jj + 1) * 128], mA[:, j * 128:(j + 1) * 128], self.ident[:]),
                         reads=[mA, self.ident], writes=[pT])
                b.op('act', lambda e: e.copy(out=ts_[:], in_=pTb[:, 0:1024]), reads=[pT], writes=[ts_])
                b.dma('pool', self.mskT[jg * 8:(jg + 1) * 8, :, i * 128:(i + 1) * 128].rearrange("j s t -> s j t"),
                      ts_[:].rearrange("s (j t) -> s j t", j=8), reads=[ts_], writes=[d_])

    def phase_attn(self, l):
        b, ar = self.b, self.ar
        ar.reset()
        KT = ar.take('KT', S, (128, 2, S), dtype=BF16)
        VT = ar.take('VT', 64 * 128, (128, 64, 256), dtype=BF16)
        QT = ar.take('QT', 8 * 256, (128, 8, TT), dtype=BF16)
        MTf = ar.take('MTf', 2048, (128, 8, 2, 128))
        MT = ar.take('MT', 1024, (128, 8, 2, 128), dtype=BF16)
        E0 = ar.take('E0', TT)
        qpr = ar.take('qpr', NT)
        onesb = ar.take('onesb', 64, dtype=BF16)
        mk = [ar.take('mk%d' % i, TT // 2, dtype=BF16) for i in range(4)]
        OH = ar.take('OH', 65 * TT // 2, (128, 65, TT), dtype=BF16)
        ee = [ar.take('ee%d' % i, TT // 2, dtype=BF16) for i in range(6)]
        pp_ = [ar.take('pp%d' % i, TT // 2, dtype=BF16) for i in range(6)]
        rc = [ar.take('rc%d' % i, TT) for i in range(2)]
        yo = [ar.take('yo%d' % i, TT) for i in range(2)]
        d_ = b.dram('att_d')
        for r in range(4):
            for kv in range(2):
                b.dma('pool', KT[:, kv, r * NT:(r + 1) * NT], self.kT_all[r * 256 + kv * 128:r * 256 + (kv + 1) * 128, :], reads=[d_], writes=[KT])
        b.dma('pool', VT[:], self.v_all.ap().rearrange("(j p) f -> p j f", p=128), reads=[d_], writes=[VT])
        b.dma('pool', MTf[:].rearrange("p h u s -> p (h u s)"), self.mtab, reads=[d_], writes=[MTf])
        b.dma('pool', qpr[:], self.qposr[0].partition_broadcast(128), reads=[d_], writes=[qpr])
        b.op('dve', lambda e: e.tensor_copy(out=onesb[:], in_=self.ones0[:]), reads=[self.ones0], writes=[onesb])
        for h in range(8):
            b.op('dve', lambda e, h=h: e.tensor_scalar(out=MT[:, h], in0=MTf[:, h], scalar1=self.b31t[:, h:h + 1], scalar2=None, op0=ALU.subtract),
                 reads=[MTf, self.b31t], writes=[MT])
        mi = 0
        for m in range(NTT):
            tsl = slice(m * TT, (m + 1) * TT)
            b.dma('pool', QT[:], self.qaT.rearrange("(h d) t -> d h t", d=128)[:, :, tsl], reads=[d_], writes=[QT])
            b.op('dve', lambda e: e.tensor_scalar(out=E0[:], in0=qpr[:, tsl], scalar1=self.pidx[:, 0:1], scalar2=None, op0=ALU.subtract),
                 reads=[qpr, self.pidx], writes=[E0])
            for j in range(65):
                b.op('dve', lambda e, j=j: e.tensor_scalar(out=OH[:, j, :], in0=E0[:], scalar1=128.0 * j, scalar2=None, op0=ALU.is_equal),
                     reads=[E0], writes=[OH])
            for hp in range(4):
                acc = [self.ps[0], self.ps[1], self.ps[2], self.ps[3]]
                for j in range(64):
                    mt = mk[mi % 4]
                    mi += 1
                    b.dma('sp', mt[:], self.mskT[j, :, tsl], reads=[d_], writes=[mt])
                    for hh in range(2):
                        h = hp * 2 + hh
                        kv = h // 4
                        pl = self.ps[4 + (j * 2 + hh) % 4]
                        b.op('pe', lambda e, h=h, kv=kv, j=j: e.matmul(pl[:], KT[:, kv, j * 128:(j + 1) * 128], QT[:, h, :], start=True, stop=False),
                             reads=[KT, QT], writes=[pl])
                        b.op('pe', lambda e, h=h, j=j: e.matmul(pl[:], MT[:, h, 0, :], OH[:, j, :], start=False, stop=False), reads=[MT, OH], writes=[pl])
                        b.op('pe', lambda e, h=h, j=j: e.matmul(pl[:], MT[:, h, 1, :], OH[:, j + 1, :], start=False, stop=True), reads=[MT, OH], writes=[pl])
                        et = ee[(j * 2 + hh) % 6]
                        b.op('act', lambda e, h=h, et=et: e.activation(out=et[:], in_=pl[:], func=AF.Exp, bias=self.b31t[:, h:h + 1], scale=1.0),
                             reads=[pl, self.b31t], writes=[et])
                        pt_ = pp_[(j * 2 + hh) % 6]
                        b.op('dve', lambda e, et=et, pt_=pt_: e.tensor_tensor(out=pt_[:], in0=et[:], in1=mt[:], op=ALU.mult), reads=[et, mt], writes=[pt_])
                        b.op('pe', lambda e, kv=kv, j=j, hh=hh, pt_=pt_: e.matmul(acc[hh * 2][:], VT[:, j, kv * 128:(kv + 1) * 128], pt_[:],
                                                                                  start=(j == 0), stop=(j == 63)), reads=[VT, pt_], writes=[acc[hh * 2]])
                        b.op('pe', lambda e, j=j, hh=hh, pt_=pt_: e.matmul(acc[hh * 2 + 1][:], onesb[:], pt_[:], start=(j == 0), stop=(j == 63)),
                             reads=[onesb, pt_], writes=[acc[hh * 2 + 1]])
                for hh in range(2):
                    h = hp * 2 + hh
                    rcp, y = rc[hh], yo[hh]
                    b.op('dve', lambda e: e.reciprocal(out=rcp[:], in_=acc[hh * 2 + 1][:]), reads=[acc[hh * 2 + 1]], writes=[rcp])
                    b.op('dve', lambda e: e.tensor_tensor(out=y[:], in0=acc[hh * 2][:], in1=rcp[:], op=ALU.mult), reads=[acc[hh * 2], rcp], writes=[y])
                    b.dma('pool', self.yaT[h * 128:(h + 1) * 128, tsl], y[:], reads=[y], writes=[d_])
                    if l == 0 and 'dbg_ya' in self.dbg:
                        b.dma('pool', self.dbg['dbg_ya'][h * 128:(h + 1) * 128, tsl], y[:], reads=[y], writes=[d_])

    def phase_mem(self, l):
        b, ar = self.b, self.ar
        ar.reset()
        mx = ar.take('mx', KC * NMEM, (128, KC, NMEM))
        mh = ar.take('mh', KC * NMEM, (128, KC, NMEM))
        wb = [ar.take('mwb%d' % i, 4096) for i in range(2)]
        mkT = ar.take('mkT', 8 * NMEM // 2, (128, 8, NMEM), dtype=BF16)
        mv = ar.take('mv', 2 * 1024 // 2, (128, 2, 1024), dtype=BF16)
        qm = ar.take('qm', 8 * TT // 2, (128, 8, TT), dtype=BF16)
        pe_ = [ar.take('pe%d' % i, TT // 2, dtype=BF16) for i in range(4)]
        onesb = ar.take('onesb', 64, dtype=BF16)
        rc = [ar.take('rc%d' % i, TT) for i in range(2)]
        yo = [ar.take('yo%d' % i, TT) for i in range(2)]
        self.rs = ar.take('rs', TT)
        self.rstd = ar.take('rstd', TT)
        d_ = b.dram('mem_d')
        b.op('dve', lambda e: e.tensor_copy(out=onesb[:], in_=self.ones0[:]), reads=[self.ones0], writes=[onesb])
        b.dma('pool', mx[:], self.memT.rearrange("(k p) m -> p k m", p=128), reads=[d_], writes=[mx])
        self.rmsnorm_T(mx, mh, self.v16[:, 48:64], self.v16, n=NMEM)
        W4 = self.W4[l]
        for ti in range(8):
            w = wb[ti % 2]
            self.wload(w, W4, R_MKV + ti * 128)
            wv = w[:].rearrange("p (k c) -> p k c", k=KC)
            if ti < 4:
                for h2 in range(2):
                    ch = ti * 2 + h2
                    pp = self.ps[ch % 2]
                    for k in range(KC):
                        b.op('pe', lambda e, k=k, h2=h2: e.matmul(pp[:, 0:NMEM], r32(wv[:, k, h2 * 128:(h2 + 1) * 128]), r32(mh[:, k, :]),
                                                                  start=(k == 0), stop=(k == KC - 1)), reads=[w, mh], writes=[pp])
                    b.op('act', lambda e, ch=ch: e.copy(out=mkT[:, ch, :], in_=pp[:, 0:NMEM]), reads=[pp], writes=[mkT])
            else:
                f0 = (ti - 4) * 256
                for mt in range(2):
                    pp = self.ps[2 + mt]
                    for k in range(KC):
                        b.op('pe', lambda e, k=k, mt=mt: e.matmul(pp[:, 0:256], r32(mh[:, k, mt * 128:(mt + 1) * 128]), r32(wv[:, k, :]),
                                                                  start=(k == 0), stop=(k == KC - 1)), reads=[w, mh], writes=[pp])
                    b.op('act', lambda e, mt=mt, f0=f0: e.copy(out=mv[:, mt, f0:f0 + 256], in_=pp[:, 0:256]), reads=[pp], writes=[mv])
        it = 0
        for tt in range(NTT):
            tsl = slice(tt * TT, (tt + 1) * TT)
            b.dma('pool', qm[:], self.qmT.rearrange("(c d) t -> d c t", d=128)[:, :, tsl], reads=[d_], writes=[qm])
            for h in range(4):
                pts = []
                for mt in range(2):
                    pl = self.ps[4 + mt]
                    for dc in range(2):
                        b.op('pe', lambda e, h=h, mt=mt, dc=dc: e.matmul(pl[:], mkT[:, h * 2 + dc, mt * 128:(mt + 1) * 128], qm[:, h * 2 + dc, :],
                                                                         start=(dc == 0), stop=(dc == 1)), reads=[mkT, qm], writes=[pl])
                    pt_ = pe_[it % 4]
                    it += 1
                    b.op('act', lambda e, pt_=pt_: e.activation(out=pt_[:], in_=pl[:], func=AF.Exp), reads=[pl], writes=[pt_])
                    pts.append(pt_)
                pden = self.ps[6]
                for mt in range(2):
                    b.op('pe', lambda e, mt=mt: e.matmul(pden[:], onesb[:], pts[mt][:], start=(mt == 0), stop=(mt == 1)), reads=[onesb, pts[mt]], writes=[pden])
                rcp = rc[h % 2]
                b.op('dve', lambda e: e.reciprocal(out=rcp[:], in_=pden[:]), reads=[pden], writes=[rcp])
                for dc in range(2):
                    po = self.ps[dc]
                    for mt in range(2):
                        b.op('pe', lambda e, h=h, mt=mt, dc=dc: e.matmul(po[:], mv[:, mt, h * 256 + dc * 128:h * 256 + (dc + 1) * 128], pts[mt][:],
                                                                         start=(mt == 0), stop=(mt == 1)), reads=[mv, pts[mt]], writes=[po])
                    y = yo[dc]
                    b.op('dve', lambda e: e.tensor_tensor(out=y[:], in0=po[:], in1=rcp[:], op=ALU.mult), reads=[po, rcp], writes=[y])
                    r0 = h * 256 + dc * 128
                    b.dma('pool', self.ycT[r0:r0 + 128, tsl], y[:], reads=[y], writes=[d_])
                    if l == 0 and 'dbg_yc' in self.dbg:
                        b.dma('pool', self.dbg['dbg_yc'][r0:r0 + 128, tsl], y[:], reads=[y], writes=[d_])

    def phase_merge(self, l):
        b = self.b
        self.ffn_bufs()
        xt, hT = self.xt, self.hT
        d_ = b.dram('mrg_d')
        xd = b.dram('mrg_x')
        W4 = self.W4[l]
        ysrc = (self.yaT, self.ybT, self.ycT)
        gts = self.stage
        for tt in range(NTT):
            tsl = slice(tt * TT, (tt + 1) * TT)
            b.dma('pool', xt[:], self.xview(self.xres, tt), reads=[xd], writes=[xt])
            for j in range(3):
                for half in range(2):
                    wdb = self.wd[2 * j + half]
                    b.dma('pool', r32(wdb[:].rearrange("p (c t) -> p c t", c=4)),
                          r32(ysrc[j][half * 512:(half + 1) * 512, tsl].rearrange("(c p) t -> p c t", p=128)), reads=[d_], writes=[wdb])
            for dq in range(4):
                wbs = []
                for j in range(3):
                    wb = self.wb[self.wbi % 3]
                    self.wbi += 1
                    self.wload(wb, W4, R_WBR + (j * 4 + dq) * 128)
                    wbs.append(wb)
                for dd in range(4):
                    dch = dq * 4 + dd
                    for j in range(3):
                        pb = self.ps[j]
                        wv = wbs[j][:].rearrange("p (c d) -> p c d", c=8)
                        for c in range(8):
                            ybuf = self.wd[2 * j + c // 4]
                            b.op('pe', lambda e, c=c, dd=dd, wv=wv, ybuf=ybuf: e.matmul(pb[:], r32(wv[:, c, dd * 128:(dd + 1) * 128]),
                                                                                        r32(ybuf[:, (c % 4) * 512:(c % 4 + 1) * 512]),
                                                                                        start=(c == 0), stop=(c == 7)), reads=[wbs[j], ybuf], writes=[pb])
                    for j in range(3):
                        g = gts[j]
                        b.dma('pool', g[:], self.gtT[j * 2048 + dch * 128:j * 2048 + (dch + 1) * 128, tsl], reads=[d_], writes=[g])
                    b.op('dve', lambda e: e.tensor_tensor(out=self.rs[:], in0=self.ps[0][:], in1=gts[0][:], op=ALU.mult), reads=[self.ps[0], gts[0]], writes=[self.rs])
                    b.op('dve', lambda e: e.tensor_tensor(out=self.sg[0][:], in0=self.ps[1][:], in1=gts[1][:], op=ALU.mult), reads=[self.ps[1], gts[1]], writes=[self.sg[0]])
                    b.op('dve', lambda e: e.tensor_tensor(out=self.sg[1][:], in0=self.ps[2][:], in1=gts[2][:], op=ALU.mult), reads=[self.ps[2], gts[2]], writes=[self.sg[1]])
                    b.op('dve', lambda e: e.tensor_tensor(out=self.rs[:], in0=self.rs[:], in1=self.sg[0][:], op=ALU.add), reads=[self.rs, self.sg[0]], writes=[self.rs])
                    b.op('dve', lambda e, dch=dch: e.tensor_tensor(out=r32(hT[:, dch, :]), in0=self.rs[:], in1=self.sg[1][:], op=ALU.add),
                         reads=[self.rs, self.sg[1]], writes=[hT])
            for ti in range(8):
                wb = self.wb[self.wbi % 3]
                self.wbi += 1
                self.wload(wb, W4, R_OUT + ti * 128)
                wv = wb[:].rearrange("p (k c) -> p k c", k=KC)
                for h2 in range(2):
                    dch = ti * 2 + h2
                    po = self.ps[4 + dch % 2]
                    for k in range(KC):
                        b.op('pe', lambda e, k=k, h2=h2: e.matmul(po[:], r32(wv[:, k, h2 * 128:(h2 + 1) * 128]), r32(hT[:, k, :]),
                                                                  start=(k == 0), stop=(k == KC - 1)), reads=[wb, hT], writes=[po])
                    b.op('dve', lambda e, dch=dch: e.tensor_tensor(out=xt[:, dch, :], in0=po[:], in1=xt[:, dch, :], op=ALU.add), reads=[po, xt], writes=[xt])
            if l == 0 and 'dbg_x2' in self.dbg:
                b.dma('pool', self.xview(self.dbg['dbg_x2'], tt), xt[:], reads=[xt], writes=[xd])
            self.rmsnorm_T(xt, hT, self.v16[:, 32:48], self.v16)
            self.ffn_tile(xt, hT, l, 2)
            b.dma('pool', self.xview(self.xres, tt), xt[:], reads=[xt], writes=[xd])
            if self.xout is not None:
                b.dma('pool', self.xview(self.xout, tt), xt[:], reads=[xt], writes=[xd])

    def final_norm(self):
        b = self.b
        self.ffn_bufs()
        xd = b.dram('fin_x')
        for tt in range(NTT):
            b.dma('pool', self.xt[:], self.xview(self.xres, tt), reads=[xd], writes=[self.xt])
            self.rmsnorm_T(self.xt, self.hT, self.fng[:], self.fng)
            b.dma('pool', self.xview(self.yout, tt), self.hT[:], reads=[self.hT], writes=[xd])


def _t5_bucket(d):
    d = np.maximum(d, 0)
    df = np.maximum(d, 1).astype(np.float32)
    large = 16 + (np.log(df / 16) / math.log(8) * 16).astype(np.int32)
    large = np.minimum(large, 31)
    return np.where(d < 16, d, large)


def _tile16(w, ncol_tiles):
    return np.ascontiguousarray(w.reshape(16, 128, ncol_tiles, 256).transpose(2, 1, 0, 3)).reshape(ncol_tiles * 128, 4096)


def _prep_weights(inp, layers):
    nl = len(layers)
    w4 = np.zeros((nl, R4, 4096), np.float32)
    w2 = np.zeros((nl, R2, 2048), np.float32)
    for li, l in enumerate(layers):
        for (r0, key) in ((R_GU1, 'w_ff1_gu'), (R_GU2, 'w_ff2_gu')):
            wg = inp[key][l]
            w4[li, r0:r0 + 5504] = np.ascontiguousarray(wg.reshape(16, 128, 2, FC, 128).transpose(3, 1, 0, 2, 4)).reshape(5504, 4096)
        w2[li, 0:5504] = inp['w_ff1_down'][l]
        w2[li, 5504:] = inp['w_ff2_down'][l]
        wi = inp['w_in'][l]
        wr = np.zeros((2048, NCH * 128), np.float32)
        wr[:, 0:1792] = wi[:, 0:1792]
        wr[:, CH_KI * 128:CH_KI * 128 + 64] = wi[:, 1792:1856]
        wr[:, CH_WI * 128:CH_WI * 128 + 4] = wi[:, 1856:1860]
        wr[:, CH_XL * 128:] = wi[:, 1860:]
        w4[li, R_WIN:R_WIN + 5632] = _tile16(wr, NCH // 2)
        w4[li, R_MKV:R_MKV + 1024] = _tile16(inp['w_mem_kv'][l], 8)
        wb = inp['w_branch'][l]
        w4[li, R_WBR:R_WBR + 1536] = np.ascontiguousarray(wb.reshape(3, 8, 128, 4, 512).transpose(0, 3, 2, 1, 4)).reshape(1536, 4096)
        w4[li, R_OUT:R_OUT + 1024] = _tile16(inp['w_out'][l], 8)
    return w4, w2


def _shard(w, secs, c):
    return np.concatenate([w[:, r0 + c * (rn // 8): r0 + (c + 1) * (rn // 8)] for (r0, rn) in secs], axis=1)


_PROG_CACHE = {}


def _get_prog(nl, debug=()):
    key = (nl, tuple(sorted(debug)))
    if key not in _PROG_CACHE:
        _PROG_CACHE[key] = Prog(nl, debug)
    return _PROG_CACHE[key]


def _pk(v):
    return np.ascontiguousarray(np.asarray(v, np.float32).reshape(-1, 128).T)


def make_in_maps(inp, layers=None, x_shards=None):
    inp = {k: np.asarray(v) for k, v in inp.items()}
    if layers is None:
        layers = list(range(DEPTH))
    nl = len(layers)
    w4, w2 = _prep_weights(inp, layers)
    x = inp['x'].reshape(2 * S, D)
    vec16 = np.stack([np.concatenate([_pk(inp['norm_ff1'][l]), _pk(inp['norm_mix'][l]), _pk(inp['norm_ff2'][l]), _pk(inp['mem_norm'][l])], axis=1)
                      for l in layers])
    lrup = np.stack([np.concatenate([_pk(inp['conv_w'][l][j]) for j in range(4)] + [_pk(inp['conv_b'][l]), _pk(inp['b_a'][l]), _pk(inp['b_i'][l]),
                                                                                       _pk(inp['lam'][l])], axis=1) for l in layers])
    wai = np.stack([np.concatenate([np.ascontiguousarray(inp['w_a'][l].transpose(1, 0, 2)).reshape(128, 1024),
                                    np.ascontiguousarray(inp['w_i'][l].transpose(1, 0, 2)).reshape(128, 1024)], axis=1) for l in layers])
    lnp = np.stack([np.stack([inp['idx_ln_g'][l], inp['idx_ln_b'][l]], axis=1) for l in layers])
    rb = inp['rel_bias']
    u = np.arange(256)[:, None]
    s_ = np.arange(128)[None, :]
    bk = _t5_bucket(u - s_)
    mt = rb[bk]
    mtab = np.ascontiguousarray(mt.reshape(2, 128, 128, 8).transpose(1, 3, 0, 2)).reshape(128, 8 * 2 * 128)
    b31 = np.ascontiguousarray(rb[31:32, :])
    maps = []
    for c in range(NCORES):
        bi, cl = c // 4, c % 4
        pos = (cl * NT + np.arange(NT)).astype(np.float32)
        sel = np.zeros((1, 8), np.float32)
        if cl > 0:
            sel[0, cl - 1] = 1.0
        sel[0, 4:4 + cl] = 1.0
        m = dict(
            xin=(np.ascontiguousarray(x[c * NT:(c + 1) * NT].T) if x_shards is None else np.ascontiguousarray(x_shards[c])),
            memT=np.ascontiguousarray(inp['mem'][bi].T),
            w4s=np.ascontiguousarray(_shard(w4, SEC4, c)),
            w2s=np.ascontiguousarray(_shard(w2, SEC2, c)),
            vec16=vec16.astype(np.float32), fnorm=_pk(inp['final_norm']), lrup=lrup.astype(np.float32), wai=wai.astype(np.float32),
            lnp=lnp.astype(np.float32), mtab=mtab.astype(np.float32), b31=b31.astype(np.float32),
            qposr=pos[None, :].copy(), qposc=np.ascontiguousarray(pos.reshape(16, 128).T), sel=sel,
        )
        maps.append(m)
    return maps


def kernel(**inputs):
    prog = _get_prog(DEPTH)
    maps = make_in_maps(inputs)
    res = run_bass_kernel_spmd(prog.nc, maps, core_ids=list(range(NCORES)))
    out = np.concatenate([np.asarray(r["yout"]).T for r in res.results], axis=0)
    return out.reshape(2, S, D).astype(np.float32)
```

```python
import math
import os
import numpy as np
import concourse.bass as bass
import concourse.mybir as mybir
from concourse.bass_utils import run_bass_kernel_spmd

F32 = mybir.dt.float32
F32R = mybir.dt.float32r
BF16 = mybir.dt.bfloat16
AF = mybir.ActivationFunctionType
ALU = mybir.AluOpType
AX = mybir.AxisListType

NCORES = 8
D = 2048
KC = 16
DFF = 5504
FC = 43
EPS = 1e-6
S = 8192
NT = 2048
TT = 512
NTT = NT // TT
DEPTH = 2
NMEM = 256
TOPK = 256
NEG = -1.0e30
BISECT = 18

CH_QA, CH_KA, CH_VA, CH_QI, CH_KI, CH_WI, CH_XL, CH_GL, CH_QM, CH_GT = 0, 8, 10, 12, 14, 15, 16, 24, 32, 40
NCH = 88
R_GU1, R_WIN, R_MKV, R_WBR, R_OUT, R_GU2 = 0, 5504, 5504 + 5632, 5504 + 5632 + 1024, 5504 + 5632 + 1024 + 1536, 5504 + 5632 + 1024 + 1536 + 1024
R4 = R_GU2 + 5504
R2 = 2 * 5504
SEC4 = [(R_GU1, 5504), (R_WIN, 5632), (R_MKV, 1024), (R_WBR, 1536), (R_OUT, 1024), (R_GU2, 5504)]
SEC2 = [(0, 5504), (5504, 5504)]


class Buf:
    def __init__(self, name, t=None):
        self.name = name
        self.t = t
        self.w = None
        self.r = {}
        self.lsem = None
        self.ltot = 0
        self.ssem = None
        self.stot = 0

    def __getitem__(self, k):
        return self.t[k]


class B:
    def __init__(self, nc):
        self.nc = nc
        self.E = {'pe': nc.tensor, 'dve': nc.vector, 'act': nc.scalar, 'pool': nc.gpsimd, 'sp': nc.sync}
        self.sem = {k: nc.alloc_semaphore('c_' + k) for k in self.E}
        self.cnt = {k: 0 for k in self.E}
        self.seen = {k: {} for k in self.E}
        self.dsems = {}
        self.free_dsems = []
        self.csems = []
        self.bg_csems = []
        self.ninst = 0
        self.nwait = 0
        self.uid = 0

    def view(self, name, ap):
        self.uid += 1
        return Buf('%s_%d' % (name, self.uid), ap)

    def ps(self, name, shape=(128, 512), dtype=F32):
        return Buf(name, self.nc.alloc_psum_tensor(name, list(shape), dtype))

    def dram(self, name):
        return Buf(name, None)

    def _wait(self, e, toks):
        best = {}
        for t in toks:
            n = t[2]
            if n not in best or best[n][1] < t[1]:
                best[n] = t
        se = self.seen[e]
        for t in sorted(best.values(), key=lambda t: -len(t[3])):
            s, v, n, snap = t
            if se.get(n, 0) < v:
                self.E[e].wait_ge(s, v)
                se[n] = v
                self.ninst += 1
                self.nwait += 1
            for k, kv in snap.items():
                if se.get(k, 0) < kv:
                    se[k] = kv

    def _deps(self, e, reads, writes):
        toks = []
        for b in reads:
            if b.w is not None:
                toks.append(b.w)
        for b in writes:
            if b.w is not None:
                toks.append(b.w)
            toks.extend(b.r.values())
        if e == 'pe':
            toks = [t for t in toks if t[2] != 'c_pe']
        return toks

    def _commit(self, tok, reads, writes):
        for b in writes:
            b.w = tok
            b.r = {}
        for b in reads:
            if b in writes:
                continue
            b.r[tok[2]] = tok

    def op(self, e, fn, reads=(), writes=()):
        self._wait(e, self._deps(e, reads, writes))
        ins = fn(self.E[e])
        self.cnt[e] += 1
        ins.then_inc(self.sem[e], 1)
        self.ninst += 1
        snap = dict(self.seen[e])
        snap['c_' + e] = self.cnt[e]
        self._commit((self.sem[e], self.cnt[e], 'c_' + e, snap), reads, writes)
        return ins

    def _dsem(self, key):
        if key not in self.dsems:
            if self.free_dsems:
                ent = self.free_dsems.pop()
            else:
                ent = [self.nc.alloc_semaphore('d%d' % len(self.dsems)), 0, 'd%d' % len(self.dsems)]
            self.dsems[key] = ent
        return self.dsems[key]

    def dma(self, q, out, in_, reads=(), writes=(), owner=None, **kw):
        if owner is None:
            owner = [b for b in list(writes) + list(reads) if b.t is not None][0]
        kind = 'l' if owner in writes else 's'
        self._wait(q, self._deps(q, reads, writes))
        ins = self.E[q].dma_start(out=out, in_=in_, **kw)
        ent = self._dsem(kind + owner.name)
        ent[1] += 16
        ins.then_inc(ent[0], 16)
        self.ninst += 1
        self._commit((ent[0], ent[1], ent[2], dict(self.seen[q])), reads, writes)
        return ins

    def custom(self, e, ins_fn, sem_inc, reads=(), writes=(), background=False):
        self._wait(e, self._deps(e, reads, writes))
        ins = ins_fn(self.E[e])
        self.uid += 1
        nm = 'cc%d' % self.uid
        ent = [self.nc.alloc_semaphore(nm), sem_inc, nm]
        (self.bg_csems if background else self.csems).append(ent)
        ins.then_inc(ent[0], sem_inc)
        self.ninst += 1
        self._commit((ent[0], ent[1], ent[2], dict(self.seen[e])), reads, writes)
        return ins

    def drain(self, e='sp'):
        toks = [(v[0], v[1], v[2], {}) for v in list(self.dsems.values()) + self.csems if v[1]]
        for k in self.E:
            if self.cnt[k]:
                toks.append((self.sem[k], self.cnt[k], 'c_' + k, {}))
        self._wait(e, toks)

    def barrier(self):
        self.drain('sp')
        self.nc.all_engine_barrier()
        for e in self.E:
            self.seen[e] = dict(self.seen['sp'])
        for k in list(self.dsems.keys()):
            self.free_dsems.append(self.dsems.pop(k))


class Arena:
    def __init__(self, b, nc, nwords, name):
        self.b = b
        self.nc = nc
        self.n = nwords
        nc.alloc_sbuf_tensor(name, [128, nwords], F32)
        self.base = nc.sbuf_base - nwords * 4
        self.off = 0

    def reset(self):
        self.off = 0

    def take(self, name, words, shape=None, dtype=F32, parts=128):
        words = (words + 7) // 8 * 8
        assert self.off + words <= self.n, (name, self.off, words, self.n)
        nel = words * (2 if dtype == BF16 else 1)
        if shape is None:
            shp = [parts, nel]
        else:
            shp = [parts] + list(shape[1:])
            assert int(np.prod(shp[1:])) <= nel, (name, shp, nel)
        self.b.uid += 1
        t = self.nc.alloc_sbuf_tensor_at('%s_%d' % (name, self.b.uid), shp, dtype, offset=self.base + self.off * 4)
        self.off += words
        return Buf('%s_%d' % (name, self.b.uid), t)


def r32(ap):
    return ap.bitcast(F32R)


class WSplit:
    def __init__(self, secs):
        self.secs = secs
        self.bufs = {}

    def buf_for(self, row):
        for (r0, rn, t) in self.secs:
            if r0 <= row < r0 + rn:
                return self.bufs[r0]
        raise KeyError(row)

    def __getitem__(self, key):
        rs, cs = key
        for (r0, rn, t) in self.secs:
            if r0 <= rs.start and rs.stop <= r0 + rn:
                return t[rs.start - r0:rs.stop - r0, cs]
        raise KeyError(key)


class Prog:
    def __init__(self, nlayers=DEPTH, debug=()):
        self.debug = set(debug)
        nc = bass.Bass("TRN2", target_bir_lowering=False)
        nc.dge_precook = False
        self.nc = nc
        self.nl = nlayers
        dt = nc.dram_tensor

        def ext_in(name, shape, dtype=F32):
            return dt(name, list(shape), dtype, kind="ExternalInput").ap()

        def ext_out(name, shape, dtype=F32):
            return dt(name, list(shape), dtype, kind="ExternalOutput").ap()

        def internal(name, shape, dtype=F32):
            return dt(name, list(shape), dtype)

        self.xin = ext_in("xin", [D, NT])
        self.memT = ext_in("memT", [D, NMEM])
        self.w4s = ext_in("w4s", [nlayers, R4 // 8, 4096])
        self.w2s = ext_in("w2s", [nlayers, R2 // 8, 2048])
        self.vec16 = ext_in("vec16", [nlayers, 128, 64])
        self.fnorm = ext_in("fnorm", [128, 16])
        self.lrup = ext_in("lrup", [nlayers, 128, 64])
        self.wai = ext_in("wai", [nlayers, 128, 2048])
        self.lnp = ext_in("lnp", [nlayers, 64, 2])
        self.mtab = ext_in("mtab", [128, 8 * 2 * 128])
        self.b31 = ext_in("b31", [1, 8])
        self.qposr = ext_in("qposr", [1, NT])
        self.qposc = ext_in("qposc", [128, 16])
        self.sel = ext_in("sel", [1, 8])
        self.yout = ext_out("yout", [D, NT])
        self.xout = ext_out("xout", [D, NT]) if nlayers == 1 else None
        self.W4 = [WSplit([(r0, rn, internal("W4_%d_%d" % (l, r0), [rn, 4096])) for (r0, rn) in SEC4]) for l in range(nlayers)]
        self.W2 = [WSplit([(r0, rn, internal("W2_%d_%d" % (l, r0), [rn, 2048])) for (r0, rn) in SEC2]) for l in range(nlayers)]
        self.w4b = internal("w4b", [nlayers, R4 // 8, 4096])
        self.w2b = internal("w2b", [nlayers, R2 // 8, 2048])
        self.xres = internal("xres", [D, NT]).ap()
        self.qaT = internal("qaT", [1024, NT], BF16).ap()
        self.kT_own = internal("kT_own", [256, NT], BF16)
        self.kT_all = internal("kT_all", [4 * 256, NT], BF16)
        self.v_own = internal("v_own", [NT, 256], BF16)
        self.v_all = internal("v_all", [4 * NT, 256], BF16)
        self.ki_own = internal("ki_own", [64, NT])
        self.ki_all = internal("ki_all", [4 * 64, NT])
        self.tail_own = internal("tail_own", [128, 32])
        self.tail_all = internal("tail_all", [4 * 128, 32])
        self.ends_own = internal("ends_own", [128, 16])
        self.ends_all = internal("ends_all", [4 * 128, 16])
        self.qiT = internal("qiT", [256, NT]).ap()
        self.ws = internal("ws", [NT, 4]).ap()
        self.xlT = internal("xlT", [1024, NT]).ap()
        self.glT = internal("glT", [1024, NT]).ap()
        self.qmT = internal("qmT", [1024, NT], BF16).ap()
        self.gtT = internal("gtT", [6144, NT]).ap()
        self.hlT = internal("hlT", [1024, NT]).ap()
        self.pcT = internal("pcT", [1024, NT]).ap()
        self.yaT = internal("yaT", [1024, NT]).ap()
        self.ybT = internal("ybT", [1024, NT]).ap()
        self.ycT = internal("ycT", [1024, NT]).ap()
        self.mskT = internal("mskT", [64, 128, NT], BF16).ap()
        self.dbg = {}
        for name, shape in (("dbg_x1", [D, NT]), ("dbg_ki", [256, NT]), ("dbg_ya", [1024, NT]), ("dbg_yb", [1024, NT]),
                            ("dbg_yc", [1024, NT]), ("dbg_x2", [D, NT])):
            if name in self.debug:
                self.dbg[name] = ext_out(name, shape)

        with nc.cleanup_on_exit():
            self.b = B(nc)
            self.build()
            self.b.csems += self.b.bg_csems
            self.b.barrier()

    def build(self):
        b, nc = self.b, self.nc
        ARW = 49664
        self.ar = Arena(b, nc, ARW, "arena")
        self.pa = Arena(b, nc, 3072, "persist")
        pa = self.pa
        self.ps = [b.ps('ps%d' % i) for i in range(8)]
        self.ones0 = pa.take('ones0', 128)
        self.ones = pa.take('ones', 128)
        self.epsb = pa.take('epsb', 8)
        self.zero = pa.take('zero', 512)
        self.iota = pa.take('iota', 512)
        self.pidx = pa.take('pidx', 8)
        self.qpc = pa.take('qpc', 16)
        self.selt = pa.take('selt', 8)
        self.b31t = pa.take('b31t', 8)
        self.fng = pa.take('fng', 16)
        self.v16 = pa.take('v16', 64)
        self.lrp = pa.take('lrp', 64)
        self.clru = pa.take('clru', 8)
        self.lnpt = pa.take('lnpt', 8, parts=64)
        self.small = pa.take('small', 256)
        self.ident = pa.take('ident', 64, shape=None, dtype=BF16)
        self.identf = pa.take('identf', 128)
        b.op('dve', lambda e: e.memset(self.ones0[:], 1.0), writes=[self.ones0])
        b.op('dve', lambda e: e.tensor_copy(out=r32(self.ones[:]), in_=self.ones0[:]), reads=[self.ones0], writes=[self.ones])
        b.op('dve', lambda e: e.memset(self.epsb[:], EPS), writes=[self.epsb])
        b.op('dve', lambda e: e.memset(self.zero[:], 0.0), writes=[self.zero])
        b.op('pool', lambda e: e.iota(self.iota[:], pattern=[[1, 512]], base=0, channel_multiplier=0,
                                      allow_small_or_imprecise_dtypes=True), writes=[self.iota])
        b.op('pool', lambda e: e.iota(self.pidx[:, 0:1], pattern=[[0, 1]], base=0, channel_multiplier=1,
                                      allow_small_or_imprecise_dtypes=True), writes=[self.pidx])
        b.op('dve', lambda e: e.tensor_scalar(out=self.identf[:], in0=self.iota[:, 0:128], scalar1=self.pidx[:, 0:1],
                                              scalar2=None, op0=ALU.is_equal), reads=[self.iota, self.pidx], writes=[self.identf])
        b.op('dve', lambda e: e.tensor_copy(out=self.ident[:], in_=self.identf[:]), reads=[self.identf], writes=[self.ident])
        b.dma('pool', self.qpc[:], self.qposc, writes=[self.qpc])
        b.dma('pool', self.selt[:], self.sel[0].partition_broadcast(128), writes=[self.selt])
        b.dma('pool', self.b31t[:], self.b31[0].partition_broadcast(128), writes=[self.b31t])
        b.dma('pool', self.fng[:], self.fnorm, writes=[self.fng])

        self.emit_weights(0)

        for l in range(self.nl):
            self.layer(l)
        self.final_norm()

    def emit_weights(self, l):
        b = self.b
        g8 = [list(range(NCORES))]
        wtmp = b.dram('wtmp%d' % l)
        for (src, dst, nr) in ((self.w4s, self.w4b, R4 // 8), (self.w2s, self.w2b, R2 // 8)):
            for r0 in range(0, nr, 256):
                r1 = min(nr, r0 + 256)
                b.dma('sp', dst[l, r0:r1, :], src[l, r0:r1, :], reads=[], writes=[wtmp], owner=self.small)
        order = [(4, SEC4[0]), (2, SEC2[0]), (4, SEC4[1]), (4, SEC4[2]), (4, SEC4[3]), (4, SEC4[4]), (4, SEC4[5]), (2, SEC2[1])]
        for which, (r0, rn) in order:
            src = (self.w4b if which == 4 else self.w2b)
            dst = (self.W4[l] if which == 4 else self.W2[l])
            wbuf = b.dram('W%d_%d_%d' % (which, l, r0))
            dst.bufs[r0] = wbuf
            i_ap = src[l, r0 // 8:(r0 + rn) // 8, :]
            o_ap = dst[r0:r0 + rn, :]
            b.custom('pool', lambda e, i_ap=i_ap, o_ap=o_ap: e.collective_compute(
                "AllGather", ALU.bypass, replica_groups=g8, ins=[i_ap.opt()], outs=[o_ap.opt()]), 1,
                reads=[wtmp], writes=[wbuf], background=(l > 0))

    def rmsnorm_T(self, xt, hT, g_ap, gbuf, n=TT):
        b = self.b
        ps_s = self.ps[6]
        b.op('act', lambda e: e.activation(out=r32(hT[:, :, 0:n]), in_=xt[:, :, 0:n], func=AF.Square), reads=[xt], writes=[hT])
        for k in range(KC):
            b.op('pe', lambda e, k=k: e.matmul(ps_s[:, 0:n], r32(self.ones[:]), r32(hT[:, k, 0:n]), start=(k == 0), stop=(k == KC - 1)),
                 reads=[self.ones, hT], writes=[ps_s])
        b.op('act', lambda e: e.activation(out=self.rs[:, 0:n], in_=ps_s[:, 0:n], func=AF.Sqrt, bias=self.epsb[:, 0:1], scale=1.0 / D),
             reads=[ps_s, self.epsb], writes=[self.rs])
        b.op('dve', lambda e: e.reciprocal(out=self.rstd[:, 0:n], in_=self.rs[:, 0:n]), reads=[self.rs], writes=[self.rstd])
        for k in range(KC):
            b.op('dve', lambda e, k=k: e.scalar_tensor_tensor(out=r32(hT[:, k, 0:n]), in0=xt[:, k, 0:n], scalar=g_ap[:, k:k + 1],
                                                              in1=self.rstd[:, 0:n], op0=ALU.mult, op1=ALU.mult),
                 reads=[xt, gbuf, self.rstd], writes=[hT])

    def wload(self, wb, W, row0, nrows=128, width=4096, q='sp'):
        self.b.dma(q, r32(wb[:, 0:width]), r32(W[row0:row0 + nrows, :]), reads=[W.buf_for(row0)], writes=[wb])

    def ffn_tile(self, xt, hT, l, which):
        b = self.b
        W4, W2 = self.W4[l], self.W2[l]
        rg = R_GU1 if which == 1 else R_GU2
        rd = 0 if which == 1 else 5504
        G = 4
        groups = [list(range(s, min(s + G, FC))) for s in range(0, FC, G)]
        it = 0
        for gi, grp in enumerate(groups):
            ag = self.actg[gi % 2]
            wds = []
            for jj, j in enumerate(grp):
                wb = self.wb[self.wbi % 3]
                self.wbi += 1
                self.wload(wb, W4, rg + j * 128)
                wd = self.wd[self.wdi % 6]
                self.wdi += 1
                self.wload(wd, W2, rd + j * 128, width=2048)
                wds.append(wd)
                pg, pu = self.ps[(it % 2) * 2], self.ps[(it % 2) * 2 + 1]
                wv = wb[:].rearrange("p (k c) -> p k c", k=KC)
                for k in range(KC):
                    b.op('pe', lambda e, k=k: e.matmul(pg[:], r32(wv[:, k, 0:128]), r32(hT[:, k, :]), start=(k == 0), stop=(k == KC - 1)),
                         reads=[wb, hT], writes=[pg])
                for k in range(KC):
                    b.op('pe', lambda e, k=k: e.matmul(pu[:], r32(wv[:, k, 128:256]), r32(hT[:, k, :]), start=(k == 0), stop=(k == KC - 1)),
                         reads=[wb, hT], writes=[pu])
                sg = self.sg[it % 2]
                b.op('act', lambda e: e.activation(out=sg[:], in_=pg[:], func=AF.Silu), reads=[pg], writes=[sg])
                b.op('dve', lambda e, jj=jj: e.tensor_tensor(out=r32(ag[:, jj, :]), in0=sg[:], in1=pu[:], op=ALU.mult),
                     reads=[sg, pu], writes=[ag])
                it += 1
            for d in range(KC):
                pd = self.ps[4 + d % 2]
                for jj in range(len(grp)):
                    b.op('pe', lambda e, jj=jj, d=d: e.matmul(pd[:], r32(wds[jj][:, d * 128:(d + 1) * 128]), r32(ag[:, jj, :]),
                                                              start=(jj == 0), stop=(jj == len(grp) - 1)),
                         reads=[wds[jj], ag], writes=[pd])
                b.op('dve', lambda e, d=d: e.scalar_tensor_tensor(out=xt[:, d, :], in0=pd[:], scalar=0.5, in1=xt[:, d, :],
                                                                  op0=ALU.mult, op1=ALU.add),
                     reads=[pd, xt], writes=[xt])

    def ffn_bufs(self):
        ar = self.ar
        ar.reset()
        self.xt = ar.take('xt', KC * TT, (128, KC, TT))
        self.hT = ar.take('hT', KC * TT, (128, KC, TT))
        self.wb = [ar.take('wb%d' % i, 4096) for i in range(3)]
        self.wd = [ar.take('wd%d' % i, 2048) for i in range(6)]
        self.actg = [ar.take('ag%d' % i, 4 * TT, (128, 4, TT)) for i in range(2)]
        self.sg = [ar.take('sg%d' % i, TT) for i in range(2)]
        self.stage = [ar.take('st%d' % i, TT) for i in range(3)]
        self.rs = ar.take('rs', TT)
        self.rstd = ar.take('rstd', TT)
        self.kxr = ar.take('kxr', TT)
        self.sqr = ar.take('sqr', TT)
        self.wbi = 0
        self.wdi = 0

    def xview(self, ap, tt):
        return ap.rearrange("(k p) t -> p k t", p=128)[:, :, tt * TT:(tt + 1) * TT]

    def layer(self, l):
        b = self.b
        b.dma('pool', self.v16[:], self.vec16[l], writes=[self.v16])
        b.dma('pool', self.lrp[:], self.lrup[l], writes=[self.lrp])
        b.dma('pool', self.lnpt[:, 0:2], self.lnp[l], writes=[self.lnpt])
        self.phase_a(l)
        b.barrier()
        self.exchange1()
        b.barrier()
        self.phase_lru1(l)
        b.barrier()
        self.exchange2()
        b.barrier()
        if l + 1 < self.nl:
            self.emit_weights(l + 1)
        self.phase_lru2(l)
        b.barrier()
        self.phase_sel(l)
        b.barrier()
        self.phase_attn(l)
        b.barrier()
        self.phase_mem(l)
        b.barrier()
        self.phase_merge(l)
        b.barrier()

    def phase_a(self, l):
        b = self.b
        self.ffn_bufs()
        xsrc = self.xin if l == 0 else self.xres
        xd = b.dram('xd')
        pd_ = b.dram('projd')
        xt, hT = self.xt, self.hT
        lng, lnb = self.lnpt[:, 0:1], self.lnpt[:, 1:2]
        W4 = self.W4[l]
        att_scale = 128 ** -0.5
        for tt in range(NTT):
            tsl = slice(tt * TT, (tt + 1) * TT)
            b.dma('pool', xt[:], self.xview(xsrc, tt), reads=[xd], writes=[xt])
            self.rmsnorm_T(xt, hT, self.v16[:, 0:16], self.v16)
            self.ffn_tile(xt, hT, l, 1)
            b.dma('pool', self.xview(self.xres, tt), xt[:], reads=[xt], writes=[xd])
            if l == 0 and 'dbg_x1' in self.dbg:
                b.dma('pool', self.xview(self.dbg['dbg_x1'], tt), xt[:], reads=[xt], writes=[xd])
            self.rmsnorm_T(xt, hT, self.v16[:, 16:32], self.v16)
            si = 0
            for ti in range(NCH // 2):
                wb = self.wb[self.wbi % 3]
                self.wbi += 1
                self.wload(wb, W4, R_WIN + ti * 128)
                wv = wb[:].rearrange("p (k c) -> p k c", k=KC)
                for h in range(2):
                    ch = ti * 2 + h
                    if CH_VA <= ch < CH_VA + 2 or ch == CH_WI:
                        if ch == CH_VA + 1:
                            continue
                        ncol = 256 if ch == CH_VA else 4
                        c0 = 0 if ch == CH_VA else 128
                        for ts in range(TT // 128):
                            pp = self.ps[(ts % 2) * 2]
                            for k in range(KC):
                                b.op('pe', lambda e, k=k, ts=ts: e.matmul(pp[:, 0:ncol], r32(hT[:, k, ts * 128:(ts + 1) * 128]),
                                                                          r32(wv[:, k, c0:c0 + ncol]), start=(k == 0), stop=(k == KC - 1)),
                                     reads=[wb, hT], writes=[pp])
                            st = self.stage[si % 3]
                            si += 1
                            r0 = tt * TT + ts * 128
                            if ch == CH_VA:
                                stb = st[:].bitcast(BF16)
                                b.op('act', lambda e: e.copy(out=stb[:, 0:256], in_=pp[:, 0:256]), reads=[pp], writes=[st])
                                b.dma('pool', self.v_own[r0:r0 + 128, :], stb[:, 0:256], reads=[st], writes=[pd_])
                            else:
                                b.op('act', lambda e: e.activation(out=st[:, 0:4], in_=pp[:, 0:4], func=AF.Copy, scale=0.5 * 0.125),
                                     reads=[pp], writes=[st])
                                b.dma('pool', self.ws[r0:r0 + 128, :], st[:, 0:4], reads=[st], writes=[pd_])
                        continue
                    pp = self.ps[(ch % 2) * 2]
                    for k in range(KC):
                        b.op('pe', lambda e, k=k, h=h: e.matmul(pp[:], r32(wv[:, k, h * 128:(h + 1) * 128]), r32(hT[:, k, :]),
                                                                start=(k == 0), stop=(k == KC - 1)),
                             reads=[wb, hT], writes=[pp])
                    st = self.stage[si % 3]
                    si += 1
                    if ch < CH_KA:
                        stb = st[:].bitcast(BF16)
                        b.op('act', lambda e: e.activation(out=stb[:, 0:TT], in_=pp[:], func=AF.Copy, scale=att_scale), reads=[pp], writes=[st])
                        b.dma('pool', self.qaT[ch * 128:(ch + 1) * 128, tsl], stb[:, 0:TT], reads=[st], writes=[pd_])
                    elif ch < CH_VA:
                        stb = st[:].bitcast(BF16)
                        b.op('act', lambda e: e.copy(out=stb[:, 0:TT], in_=pp[:]), reads=[pp], writes=[st])
                        c = ch - CH_KA
                        b.dma('pool', self.kT_own[c * 128:(c + 1) * 128, tsl], stb[:, 0:TT], reads=[st], writes=[pd_])
                    elif ch < CH_KI:
                        b.op('act', lambda e: e.copy(out=st[:], in_=pp[:]), reads=[pp], writes=[st])
                        c = ch - CH_QI
                        b.dma('pool', self.qiT[c * 128:(c + 1) * 128, tsl], st[:], reads=[st], writes=[pd_])
                    elif ch == CH_KI:
                        kx, xc_, sq_, kr_, sr_ = self.stage[0], self.stage[1], self.stage[2], self.kxr, self.sqr
                        si = 0
                        p2 = self.ps[6]
                        b.op('act', lambda e: e.copy(out=r32(kr_[0:64, :]), in_=pp[0:64, :]), reads=[pp], writes=[kr_])
                        b.op('act', lambda e: e.copy(out=kx[0:64, :], in_=pp[0:64, :]), reads=[pp], writes=[kx])
                        b.op('pe', lambda e: e.matmul(p2[0:64, :], r32(self.ones[0:64, 0:64]), r32(kr_[0:64, :]), start=True, stop=True),
                             reads=[self.ones, kr_], writes=[p2])
                        b.op('dve', lambda e: e.scalar_tensor_tensor(out=xc_[0:64, :], in0=p2[0:64, :], scalar=-1.0 / 64, in1=kx[0:64, :],
                                                                     op0=ALU.mult, op1=ALU.add), reads=[p2, kx], writes=[xc_])
                        b.op('act', lambda e: e.activation(out=r32(sr_[0:64, :]), in_=xc_[0:64, :], func=AF.Square), reads=[xc_], writes=[sr_])
                        b.op('pe', lambda e: e.matmul(p2[0:64, :], r32(self.ones[0:64, 0:64]), r32(sr_[0:64, :]), start=True, stop=True),
                             reads=[self.ones, sr_], writes=[p2])
                        b.op('act', lambda e: e.activation(out=sq_[0:64, :], in_=p2[0:64, :], func=AF.Sqrt, bias=self.epsb[0:64, 0:1], scale=1.0 / 64),
                             reads=[p2, self.epsb], writes=[sq_])
                        b.op('dve', lambda e: e.reciprocal(out=kx[0:64, :], in_=sq_[0:64, :]), reads=[sq_], writes=[kx])
                        b.op('dve', lambda e: e.tensor_tensor(out=xc_[0:64, :], in0=xc_[0:64, :], in1=kx[0:64, :], op=ALU.mult),
                             reads=[xc_, kx], writes=[xc_])
                        b.op('dve', lambda e: e.tensor_scalar(out=xc_[0:64, :], in0=xc_[0:64, :], scalar1=lng[0:64, :], scalar2=lnb[0:64, :],
                                                              op0=ALU.mult, op1=ALU.add), reads=[xc_, self.lnpt], writes=[xc_])
                        b.dma('pool', self.ki_own[0:64, tsl], xc_[0:64, :], reads=[xc_], writes=[pd_])
                        if l == 0 and 'dbg_ki' in self.dbg:
                            b.dma('pool', self.dbg['dbg_ki'][0:64, tsl], xc_[0:64, :], reads=[xc_], writes=[pd_])
                    elif ch < CH_GL:
                        b.op('act', lambda e: e.copy(out=st[:], in_=pp[:]), reads=[pp], writes=[st])
                        c = ch - CH_XL
                        b.dma('pool', self.xlT[c * 128:(c + 1) * 128, tsl], st[:], reads=[st], writes=[pd_])
                        if tt == NTT - 1:
                            b.dma('pool', self.tail_own[:, c * 4:(c + 1) * 4], st[:, TT - 4:TT], reads=[st], writes=[pd_])
                    elif ch < CH_QM:
                        b.op('act', lambda e: e.copy(out=st[:], in_=pp[:]), reads=[pp], writes=[st])
                        c = ch - CH_GL
                        b.dma('pool', self.glT[c * 128:(c + 1) * 128, tsl], st[:], reads=[st], writes=[pd_])
                    elif ch < CH_GT:
                        stb = st[:].bitcast(BF16)
                        b.op('act', lambda e: e.activation(out=stb[:, 0:TT], in_=pp[:], func=AF.Copy, scale=1.0 / 16), reads=[pp], writes=[st])
                        c = ch - CH_QM
                        b.dma('pool', self.qmT[c * 128:(c + 1) * 128, tsl], stb[:, 0:TT], reads=[st], writes=[pd_])
                    else:
                        b.op('act', lambda e: e.activation(out=st[:], in_=pp[:], func=AF.Sigmoid), reads=[pp], writes=[st])
                        c = ch - CH_GT
                        b.dma('pool', self.gtT[c * 128:(c + 1) * 128, tsl], st[:], reads=[st], writes=[pd_])

    def exchange1(self):
        b = self.b
        g4 = [[0, 1, 2, 3], [4, 5, 6, 7]]
        x = b.dram('ex1')
        for (i_t, o_t) in ((self.kT_own, self.kT_all), (self.v_own, self.v_all), (self.ki_own, self.ki_all), (self.tail_own, self.tail_all)):
            b.custom('pool', lambda e, i_t=i_t, o_t=o_t: e.collective_compute(
                "AllGather", ALU.bypass, replica_groups=g4, ins=[i_t.ap().opt()], outs=[o_t.ap().opt()]), 1, reads=[], writes=[x])

    def exchange2(self):
        b = self.b
        g4 = [[0, 1, 2, 3], [4, 5, 6, 7]]
        x = b.dram('ex2')
        b.custom('pool', lambda e: e.collective_compute(
            "AllGather", ALU.bypass, replica_groups=g4, ins=[self.ends_own.ap().opt()], outs=[self.ends_all.ap().opt()]), 1, reads=[], writes=[x])

    def phase_lru1(self, l):
        b, ar = self.b, self.ar
        ar.reset()
        T = TT
        xl = ar.take('xl', 8 * (T + 4), (128, 8, T + 4))
        xc = ar.take('xc', 8 * T, (128, 8, T))
        xcf = ar.take('xcf', 8 * T, (128, 8, T))
        sr = ar.take('sr', 8 * T, (128, 8, T))
        si = ar.take('si', 8 * T, (128, 8, T))
        a = ar.take('a', 8 * T, (128, 8, T))
        u = ar.take('u', 8 * T, (128, 8, T))
        hl = ar.take('hl', 8 * T, (128, 8, T))
        pc = ar.take('pc', 8 * T, (128, 8, T))
        wai = ar.take('wai', 2048)
        wair = ar.take('wair', 2048)
        tl = ar.take('tl', 128, (128, 4, 32))
        car = ar.take('car', 16)
        lrp = self.lrp
        cw = [lrp[:, j * 8:(j + 1) * 8] for j in range(4)]
        cb, ba, bi, lam = lrp[:, 32:40], lrp[:, 40:48], lrp[:, 48:56], lrp[:, 56:64]
        d_ = b.dram('lru_d')
        b.dma('pool', wai[:], self.wai[l], writes=[wai])
        b.op('dve', lambda e: e.tensor_copy(out=r32(wair[:]), in_=wai[:]), reads=[wai], writes=[wair])
        wa = r32(wair[:]).rearrange("p (w n d) -> p w n d", w=2, n=8)
        b.op('act', lambda e: e.activation(out=self.clru[:], in_=lam, func=AF.Exp, scale=-1.0), reads=[lrp], writes=[self.clru])
        b.op('act', lambda e: e.activation(out=self.clru[:], in_=self.clru[:], func=AF.Ln, bias=self.ones0[:, 0:1], scale=1.0),
             reads=[self.clru, self.ones0], writes=[self.clru])
        b.op('dve', lambda e: e.tensor_scalar(out=self.clru[:], in0=self.clru[:], scalar1=-8.0, scalar2=None, op0=ALU.mult),
             reads=[self.clru], writes=[self.clru])
        b.dma('pool', tl[:], self.tail_all.ap().rearrange("(r p) q -> p r q", p=128), reads=[d_], writes=[tl])
        halo = xl
        b.op('dve', lambda e: e.tensor_scalar(out=xl[:, :, 0:4], in0=tl[:, 0, :].rearrange("p (n q) -> p n q", q=4), scalar1=self.selt[:, 0:1],
                                              scalar2=None, op0=ALU.mult), reads=[tl, self.selt], writes=[xl])
        for r in range(1, 4):
            b.op('dve', lambda e, r=r: e.scalar_tensor_tensor(out=xl[:, :, 0:4], in0=tl[:, r, :].rearrange("p (n q) -> p n q", q=4),
                                                              scalar=self.selt[:, r:r + 1], in1=xl[:, :, 0:4], op0=ALU.mult, op1=ALU.add),
                 reads=[tl, self.selt, xl], writes=[xl])
        b.op('dve', lambda e: e.memset(car[:, 0:8], 1.0), writes=[car])
        b.op('dve', lambda e: e.memset(car[:, 8:16], 0.0), writes=[car])
        for tt in range(NT // T):
            tsl = slice(tt * T, (tt + 1) * T)
            b.dma('pool', xl[:, :, 4:4 + T], self.xlT.rearrange("(n p) t -> p n t", p=128)[:, :, tsl], reads=[d_], writes=[xl])
            for n in range(8):
                b.op('dve', lambda e, n=n: e.tensor_scalar(out=xcf[:, n, :], in0=xl[:, n, 1:1 + T], scalar1=cw[0][:, n:n + 1], scalar2=cb[:, n:n + 1],
                                                           op0=ALU.mult, op1=ALU.add), reads=[xl, lrp], writes=[xcf])
                for j in range(1, 4):
                    b.op('dve', lambda e, n=n, j=j: e.scalar_tensor_tensor(out=xcf[:, n, :], in0=xl[:, n, j + 1:j + 1 + T], scalar=cw[j][:, n:n + 1],
                                                                           in1=xcf[:, n, :], op0=ALU.mult, op1=ALU.add),
                         reads=[xl, lrp, xcf], writes=[xcf])
            b.op('act', lambda e: e.copy(out=r32(xc[:]), in_=xcf[:]), reads=[xcf], writes=[xc])
            for n in range(8):
                pr, pi = self.ps[(n % 2) * 2], self.ps[(n % 2) * 2 + 1]
                b.op('pe', lambda e, n=n: e.matmul(pr[:], wa[:, 0, n, :], r32(xc[:, n, :]), start=True, stop=True), reads=[wair, xc], writes=[pr])
                b.op('pe', lambda e, n=n: e.matmul(pi[:], wa[:, 1, n, :], r32(xc[:, n, :]), start=True, stop=True), reads=[wair, xc], writes=[pi])
                b.op('act', lambda e, n=n: e.activation(out=sr[:, n, :], in_=pr[:], func=AF.Sigmoid, bias=ba[:, n:n + 1], scale=1.0),
                     reads=[pr, lrp], writes=[sr])
                b.op('act', lambda e, n=n: e.activation(out=si[:, n, :], in_=pi[:], func=AF.Sigmoid, bias=bi[:, n:n + 1], scale=1.0),
                     reads=[pi, lrp], writes=[si])
            for n in range(8):
                b.op('act', lambda e, n=n: e.activation(out=a[:, n, :], in_=sr[:, n, :], func=AF.Exp, scale=self.clru[:, n:n + 1]),
                     reads=[sr, self.clru], writes=[a])
            b.op('dve', lambda e: e.tensor_tensor(out=u[:], in0=a[:], in1=a[:], op=ALU.mult), reads=[a], writes=[u])
            b.op('dve', lambda e: e.tensor_scalar(out=u[:], in0=u[:], scalar1=-1.0, scalar2=1.0, op0=ALU.mult, op1=ALU.add), reads=[u], writes=[u])
            b.op('dve', lambda e: e.tensor_scalar(out=u[:], in0=u[:], scalar1=0.0, scalar2=None, op0=ALU.max), reads=[u], writes=[u])
            b.op('act', lambda e: e.activation(out=u[:], in_=u[:], func=AF.Sqrt), reads=[u], writes=[u])
            b.op('dve', lambda e: e.tensor_tensor(out=u[:], in0=u[:], in1=si[:], op=ALU.mult), reads=[u, si], writes=[u])
            b.op('dve', lambda e: e.tensor_tensor(out=u[:], in0=u[:], in1=xcf[:], op=ALU.mult), reads=[u, xcf], writes=[u])
            for n in range(8):
                b.op('dve', lambda e, n=n: e.tensor_tensor_scan(out=hl[:, n, :], data0=a[:, n, :], data1=u[:, n, :], initial=car[:, 8 + n:9 + n],
                                                                op0=ALU.mult, op1=ALU.add), reads=[a, u, car], writes=[hl])
                b.op('dve', lambda e, n=n: e.tensor_tensor_scan(out=pc[:, n, :], data0=a[:, n, :], data1=self.zero[:, 0:T], initial=car[:, n:n + 1],
                                                                op0=ALU.mult, op1=ALU.add), reads=[a, self.zero, car], writes=[pc])
            b.op('dve', lambda e: e.tensor_copy(out=car[:, 0:8], in_=pc[:, :, T - 1]), reads=[pc], writes=[car])
            b.op('dve', lambda e: e.tensor_copy(out=car[:, 8:16], in_=hl[:, :, T - 1]), reads=[hl], writes=[car])
            b.dma('pool', self.hlT.rearrange("(n p) t -> p n t", p=128)[:, :, tsl], hl[:], reads=[hl], writes=[d_])
            b.dma('pool', self.pcT.rearrange("(n p) t -> p n t", p=128)[:, :, tsl], pc[:], reads=[pc], writes=[d_])
            if tt + 1 < NT // T:
                b.op('dve', lambda e: e.tensor_copy(out=xl[:, :, 0:4], in_=xl[:, :, T:T + 4]), reads=[xl], writes=[xl])
        b.dma('pool', self.ends_own.ap(), car[:], reads=[car], writes=[d_])

    def phase_lru2(self, l):
        b, ar = self.b, self.ar
        ar.reset()
        T = TT
        hl = ar.take('hl', 8 * T, (128, 8, T))
        pc = ar.take('pc', 8 * T, (128, 8, T))
        gl = ar.take('gl', 8 * T, (128, 8, T))
        t1 = ar.take('t1', 8 * T, (128, 8, T))
        en = ar.take('en', 64, (128, 4, 16))
        hin = ar.take('hin', 8)
        hnw = ar.take('hnw', 8)
        d_ = b.dram('lru2_d')
        b.dma('pool', en[:], self.ends_all.ap().rearrange("(r p) q -> p r q", p=128), reads=[d_], writes=[en])
        b.op('dve', lambda e: e.memset(hin[:], 0.0), writes=[hin])
        for r in range(3):
            b.op('dve', lambda e, r=r: e.tensor_tensor(out=hnw[:], in0=en[:, r, 0:8], in1=hin[:], op=ALU.mult), reads=[en, hin], writes=[hnw])
            b.op('dve', lambda e, r=r: e.tensor_tensor(out=hnw[:], in0=hnw[:], in1=en[:, r, 8:16], op=ALU.add), reads=[en, hnw], writes=[hnw])
            b.op('dve', lambda e: e.tensor_tensor(out=hnw[:], in0=hnw[:], in1=hin[:], op=ALU.subtract), reads=[hnw, hin], writes=[hnw])
            b.op('dve', lambda e, r=r: e.scalar_tensor_tensor(out=hin[:], in0=hnw[:], scalar=self.selt[:, 4 + r:5 + r], in1=hin[:],
                                                              op0=ALU.mult, op1=ALU.add), reads=[hnw, self.selt, hin], writes=[hin])
        c1 = math.sqrt(2.0 / math.pi)
        for tt in range(NT // T):
            tsl = slice(tt * T, (tt + 1) * T)
            for (dst, src) in ((hl, self.hlT), (pc, self.pcT), (gl, self.glT)):
                b.dma('pool', dst[:], src.rearrange("(n p) t -> p n t", p=128)[:, :, tsl], reads=[d_], writes=[dst])
            for n in range(8):
                b.op('dve', lambda e, n=n: e.scalar_tensor_tensor(out=hl[:, n, :], in0=pc[:, n, :], scalar=hin[:, n:n + 1], in1=hl[:, n, :],
                                                                  op0=ALU.mult, op1=ALU.add), reads=[pc, hin, hl], writes=[hl])
            b.op('act', lambda e: e.activation(out=t1[:], in_=gl[:], func=AF.Square), reads=[gl], writes=[t1])
            b.op('dve', lambda e: e.tensor_scalar(out=t1[:], in0=t1[:], scalar1=0.044715, scalar2=1.0, op0=ALU.mult, op1=ALU.add), reads=[t1], writes=[t1])
            b.op('dve', lambda e: e.tensor_tensor(out=t1[:], in0=t1[:], in1=gl[:], op=ALU.mult), reads=[t1, gl], writes=[t1])
            b.op('act', lambda e: e.activation(out=t1[:], in_=t1[:], func=AF.Sigmoid, scale=2.0 * c1), reads=[t1], writes=[t1])
            b.op('dve', lambda e: e.tensor_tensor(out=t1[:], in0=t1[:], in1=gl[:], op=ALU.mult), reads=[t1, gl], writes=[t1])
            b.op('dve', lambda e: e.tensor_tensor(out=t1[:], in0=t1[:], in1=hl[:], op=ALU.mult), reads=[t1, hl], writes=[t1])
            b.dma('pool', self.ybT.rearrange("(n p) t -> p n t", p=128)[:, :, tsl], t1[:], reads=[t1], writes=[d_])
            if l == 0 and 'dbg_yb' in self.dbg:
                b.dma('pool', self.dbg['dbg_yb'].rearrange("(n p) t -> p n t", p=128)[:, :, tsl], t1[:], reads=[t1], writes=[d_])

    def phase_sel(self, l):
        b, ar = self.b, self.ar
        ar.reset()
        KI = ar.take('KI', S, parts=64)
        sc = ar.take('sc', S)
        pf = ar.take('pf', S)
        mA = ar.take('mA', S // 2, dtype=BF16)
        mC = ar.take('mC', S // 2, dtype=BF16)
        qi = ar.take('qi', 4 * 128, (64, 4, 128), parts=64)
        qir = ar.take('qir', 4 * 128, (64, 4, 128), parts=64)
        rl = [ar.take('rl%d' % i, 512) for i in range(8)]
        pn = [ar.take('pn%d' % i, 512) for i in range(2)]
        Dg = ar.take('Dg', 4 * 128, (128, 4, 128))
        idr = ar.take('idr', 128)
        wst = ar.take('wst', 64, (128, 16, 4))
        wab = ar.take('wab', 64, (128, 16, 4))
        wsg = ar.take('wsg', 64, (128, 16, 4))
        qrel = ar.take('qrel', 256, (128, 16, 16))
        sm = ar.take('sm', 16)
        sm2t = ar.take('sm2', 8)
        sm2 = sm2t
        tst = [ar.take('tst%d' % i, 512, dtype=BF16) for i in range(2)]
        d_ = b.dram('sel_d')
        for r in range(4):
            b.dma('pool', r32(KI[0:64, r * NT:(r + 1) * NT]), r32(self.ki_all[r * 64:(r + 1) * 64, :]), reads=[d_], writes=[KI])
        b.dma('pool', wst[:], self.ws.rearrange("(i p) h -> p i h", p=128), reads=[d_], writes=[wst])
        b.op('act', lambda e: e.activation(out=wab[:], in_=wst[:], func=AF.Abs), reads=[wst], writes=[wab])
        b.op('act', lambda e: e.activation(out=wsg[:], in_=wst[:], func=AF.Sign), reads=[wst], writes=[wsg])
        for cc in range(16):
            b.op('dve', lambda e, cc=cc: e.tensor_scalar(out=qrel[:, :, cc], in0=self.qpc[:], scalar1=-512.0 * cc, scalar2=None, op0=ALU.add),
                 reads=[self.qpc], writes=[qrel])
        lo, hi, w0, mm, tmp, need, c2, mmw = (sm[:, i:i + 1] for i in range(8))
        smid = ar.take('smid', 8)
        sB = ar.take('sB', 8)
        sC = ar.take('sC', 8)
        mid, accb, cnt = smid[:, 0:1], sB[:, 0:1], sC[:, 0:1]
        b.op('dve', lambda e: e.tensor_copy(out=r32(idr[:]), in_=self.identf[:]), reads=[self.identf], writes=[idr])
        pst = self.ps[7]
        pT = self.ps[5]
        for i in range(NT // 128):
            b.dma('pool', qi[:], self.qiT.rearrange("(h d) t -> d h t", d=64)[:, :, i * 128:(i + 1) * 128], reads=[d_], writes=[qi])
            b.op('dve', lambda e: e.tensor_copy(out=r32(qir[:]), in_=qi[:]), reads=[qi], writes=[qir])
            for h in range(4):
                b.op('dve', lambda e, h=h, i=i: e.tensor_scalar(out=r32(Dg[:, h, :]), in0=self.identf[:], scalar1=wsg[:, i, h:h + 1], scalar2=None,
                                                                op0=ALU.mult), reads=[self.identf, wsg], writes=[Dg])
            def emit_qk(cc):
                csl = slice(cc * 512, (cc + 1) * 512)
                for h in range(4):
                    pp = self.ps[h % 4]
                    b.op('pe', lambda e, h=h, pp=pp: e.matmul(pp[:], r32(qir[0:64, h, :]), r32(KI[0:64, csl]), start=True, stop=True),
                         reads=[qir, KI], writes=[pp])
                    rr = rl[(cc % 2) * 4 + h]
                    b.op('act', lambda e, h=h, rr=rr, pp=pp: e.activation(out=r32(rr[:]), in_=pp[:], func=AF.Relu, scale=wab[:, i, h:h + 1]),
                         reads=[pp, wab], writes=[rr])

            emit_qk(0)
            for cc in range(16):
                csl = slice(cc * 512, (cc + 1) * 512)
                pS = self.ps[4 + 2 * (cc % 2)]
                pnt = pn[cc % 2]
                b.op('dve', lambda e, cc=cc, pnt=pnt: e.tensor_scalar(out=r32(pnt[:]), in0=self.iota[:], scalar1=qrel[:, i, cc:cc + 1], scalar2=NEG,
                                                                     op0=ALU.is_gt, op1=ALU.mult), reads=[self.iota, qrel], writes=[pnt])
                if cc + 1 < 16:
                    emit_qk(cc + 1)
                for h in range(4):
                    rr = rl[(cc % 2) * 4 + h]
                    b.op('pe', lambda e, h=h, pS=pS, rr=rr: e.matmul(pS[:], r32(Dg[:, h, :]), r32(rr[:]), start=(h == 0), stop=False),
                         reads=[Dg, rr], writes=[pS])
                b.op('pe', lambda e, pS=pS, pnt=pnt: e.matmul(pS[:], r32(idr[:]), r32(pnt[:]), start=False, stop=True), reads=[idr, pnt], writes=[pS])
                b.op('act', lambda e, csl=csl, pS=pS: e.copy(out=sc[:, csl], in_=pS[:]), reads=[pS], writes=[sc])
            b.op('dve', lambda e: e.tensor_reduce(out=hi, in_=sc[:], axis=AX.X, op=ALU.max), reads=[sc], writes=[sm])
            b.op('dve', lambda e: e.tensor_scalar(out=pf[:], in0=sc[:], scalar1=0.5 * NEG, scalar2=-2.0 * NEG, op0=ALU.is_lt, op1=ALU.mult),
                 reads=[sc], writes=[pf])
            b.op('dve', lambda e: e.tensor_tensor(out=pf[:], in0=pf[:], in1=sc[:], op=ALU.add), reads=[pf, sc], writes=[pf])
            b.op('dve', lambda e: e.tensor_reduce(out=lo, in_=pf[:], axis=AX.X, op=ALU.min), reads=[pf], writes=[sm])
            b.op('dve', lambda e: e.tensor_scalar(out=lo, in0=lo, scalar1=-1.0, scalar2=None, op0=ALU.add), reads=[sm], writes=[sm])
            HS = S // 2
            b.op('dve', lambda e: e.memset(mid, 0.0), writes=[smid])
            b.op('dve', lambda e: e.tensor_tensor(out=mid, in0=mid, in1=lo, op=ALU.max), reads=[sm, smid], writes=[smid])
            b.op('dve', lambda e: e.tensor_tensor(out=mid, in0=mid, in1=hi, op=ALU.min), reads=[sm, smid], writes=[smid])
            b.op('dve', lambda e: e.tensor_scalar(out=mC[:], in0=sc[:], scalar1=mid, scalar2=0.0, op0=ALU.is_gt, op1=ALU.add, accum_out=cnt),
                 reads=[sc, smid], writes=[mC, sC])
            b.op('dve', lambda e: e.tensor_scalar(out=mm, in0=cnt, scalar1=float(TOPK), scalar2=None, op0=ALU.is_gt), reads=[sC], writes=[sm])
            b.op('dve', lambda e: e.tensor_tensor(out=tmp, in0=mid, in1=lo, op=ALU.subtract), reads=[sm, smid], writes=[sm])
            b.op('dve', lambda e: e.scalar_tensor_tensor(out=lo, in0=tmp, scalar=mm, in1=lo, op0=ALU.mult, op1=ALU.add), reads=[sm], writes=[sm])
            b.op('dve', lambda e: e.tensor_tensor(out=tmp, in0=hi, in1=mid, op=ALU.subtract), reads=[sm, smid], writes=[sm])
            b.op('dve', lambda e: e.scalar_tensor_tensor(out=hi, in0=tmp, scalar=mm, in1=mid, op0=ALU.mult, op1=ALU.add), reads=[sm, smid], writes=[sm])
            b.op('dve', lambda e: e.tensor_tensor(out=w0, in0=hi, in1=lo, op=ALU.subtract), reads=[sm], writes=[sm])
            for k in range(1, BISECT):
                f = 2.0 ** (-k)
                b.op('dve', lambda e, f=f: e.scalar_tensor_tensor(out=mid, in0=w0, scalar=f, in1=lo, op0=ALU.mult, op1=ALU.add), reads=[sm], writes=[smid])
                b.op('act', lambda e: e.activation(out=mA[:, HS:S], in_=sc[:, HS:S], func=AF.Sign, bias=mid, scale=-1.0, accum_out=accb),
                     reads=[sc, smid], writes=[mA, sB])
                b.op('dve', lambda e: e.tensor_scalar(out=mC[:, 0:HS], in0=sc[:, 0:HS], scalar1=mid, scalar2=0.0, op0=ALU.is_gt, op1=ALU.add, accum_out=cnt),
                     reads=[sc, smid], writes=[mC, sC])
                b.op('dve', lambda e: e.scalar_tensor_tensor(out=c2, in0=cnt, scalar=2.0, in1=accb, op0=ALU.mult, op1=ALU.subtract), reads=[sC, sB], writes=[sm])
                b.op('dve', lambda e, f=f: e.tensor_scalar(out=mmw, in0=c2, scalar1=float(2 * TOPK - HS), scalar2=f, op0=ALU.is_gt, op1=ALU.mult), reads=[sm], writes=[sm])
                b.op('dve', lambda e: e.scalar_tensor_tensor(out=lo, in0=mmw, scalar=w0, in1=lo, op0=ALU.mult, op1=ALU.add), reads=[sm], writes=[sm])
            b.op('dve', lambda e: e.scalar_tensor_tensor(out=hi, in0=w0, scalar=2.0 ** (-(BISECT - 1)), in1=lo, op0=ALU.mult, op1=ALU.add), reads=[sm], writes=[sm])
            b.op('dve', lambda e: e.tensor_scalar(out=mA[:], in0=sc[:], scalar1=hi, scalar2=0.0, op0=ALU.is_gt, op1=ALU.add, accum_out=cnt),
                 reads=[sc, sm], writes=[mA, sC])
            b.op('dve', lambda e: e.tensor_scalar(out=need, in0=cnt, scalar1=-1.0, scalar2=float(TOPK), op0=ALU.mult, op1=ALU.add), reads=[sC], writes=[sm])
            b.op('dve', lambda e: e.scalar_tensor_tensor(out=mC[:], in0=sc[:], scalar=lo, in1=mA[:], op0=ALU.is_gt, op1=ALU.subtract),
                 reads=[sc, sm, mA], writes=[mC])
            b.op('dve', lambda e: e.tensor_tensor_scan(out=pf[:], data0=mC[:], data1=mC[:], initial=0.0, op0=ALU.add, op1=ALU.max),
                 reads=[mC], writes=[pf])
            b.op('dve', lambda e: e.scalar_tensor_tensor(out=mC[:], in0=pf[:], scalar=need, in1=mC[:], op0=ALU.is_le, op1=ALU.mult),
                 reads=[pf, sm, mC], writes=[mC])
            b.op('dve', lambda e: e.tensor_tensor(out=mA[:], in0=mA[:], in1=mC[:], op=ALU.add), reads=[mA, mC], writes=[mA])
            for jg in range(8):
                ts_ = tst[jg % 2]
                pTb = pT.t[:].bitcast(BF16)
                for jj in range(8):
                    j = jg * 8 + jj
                    b.op('pe', lambda e, j=j, jj=jj: e.transpose(pTb[:, jj * 128:(jj + 1) * 128], mA[:, j * 128:(j + 1) * 128], self.ident[:]),
                         reads=[mA, self.ident], writes=[pT])
                b.op('act', lambda e: e.copy(out=ts_[:], in_=pTb[:, 0:1024]), reads=[pT], writes=[ts_])
                b.dma('pool', self.mskT[jg * 8:(jg + 1) * 8, :, i * 128:(i + 1) * 128].rearrange("j s t -> s j t"),
                      ts_[:].rearrange("s (j t) -> s j t", j=8), reads=[ts_], writes=[d_])

    def phase_attn(self, l):
        b, ar = self.b, self.ar
        ar.reset()
        KT = ar.take('KT', S, (128, 2, S), dtype=BF16)
        VT = ar.take('VT', 64 * 128, (128, 64, 256), dtype=BF16)
        QT = ar.take('QT', 8 * 256, (128, 8, TT), dtype=BF16)
        MTf = ar.take('MTf', 2048, (128, 8, 2, 128))
        MT = ar.take('MT', 1024, (128, 8, 2, 128), dtype=BF16)
        E0 = ar.take('E0', TT)
        qpr = ar.take('qpr', NT)
        onesb = ar.take('onesb', 64, dtype=BF16)
        mk = [ar.take('mk%d' % i, TT // 2, dtype=BF16) for i in range(4)]
        OH = ar.take('OH', 65 * TT // 2, (128, 65, TT), dtype=BF16)
        ee = [ar.take('ee%d' % i, TT // 2, dtype=BF16) for i in range(6)]
        pp_ = [ar.take('pp%d' % i, TT // 2, dtype=BF16) for i in range(6)]
        rc = [ar.take('rc%d' % i, TT) for i in range(2)]
        yo = [ar.take('yo%d' % i, TT) for i in range(2)]
        d_ = b.dram('att_d')
        for r in range(4):
            for kv in range(2):
                b.dma('pool', KT[:, kv, r * NT:(r + 1) * NT], self.kT_all[r * 256 + kv * 128:r * 256 + (kv + 1) * 128, :], reads=[d_], writes=[KT])
        b.dma('pool', VT[:], self.v_all.ap().rearrange("(j p) f -> p j f", p=128), reads=[d_], writes=[VT])
        b.dma('pool', MTf[:].rearrange("p h u s -> p (h u s)"), self.mtab, reads=[d_], writes=[MTf])
        b.dma('pool', qpr[:], self.qposr[0].partition_broadcast(128), reads=[d_], writes=[qpr])
        b.op('dve', lambda e: e.tensor_copy(out=onesb[:], in_=self.ones0[:]), reads=[self.ones0], writes=[onesb])
        for h in range(8):
            b.op('dve', lambda e, h=h: e.tensor_scalar(out=MT[:, h], in0=MTf[:, h], scalar1=self.b31t[:, h:h + 1], scalar2=None, op0=ALU.subtract),
                 reads=[MTf, self.b31t], writes=[MT])
        self._mi = 0
        for m in range(NTT):
            tsl = slice(m * TT, (m + 1) * TT)
            b.dma('pool', QT[:], self.qaT.rearrange("(h d) t -> d h t", d=128)[:, :, tsl], reads=[d_], writes=[QT])
            b.op('dve', lambda e: e.tensor_scalar(out=E0[:], in0=qpr[:, tsl], scalar1=self.pidx[:, 0:1], scalar2=None, op0=ALU.subtract),
                 reads=[qpr, self.pidx], writes=[E0])
            for j in range(65):
                b.op('dve', lambda e, j=j: e.tensor_scalar(out=OH[:, j, :], in0=E0[:], scalar1=128.0 * j, scalar2=None, op0=ALU.is_equal),
                     reads=[E0], writes=[OH])
            for hp in range(4):
                acc = [self.ps[0], self.ps[1], self.ps[2], self.ps[3]]
                steps = [(j, hh) for j in range(64) for hh in range(2)]
                mts = {}

                def emit_logits(n):
                    j, hh = steps[n]
                    if hh == 0:
                        mt = mk[self._mi % 4]
                        self._mi += 1
                        b.dma('sp', mt[:], self.mskT[j, :, tsl], reads=[d_], writes=[mt])
                        mts[j] = mt
                    h = hp * 2 + hh
                    kv = h // 4
                    pl = self.ps[4 + n % 4]
                    b.op('pe', lambda e: e.matmul(pl[:], KT[:, kv, j * 128:(j + 1) * 128], QT[:, h, :], start=True, stop=False),
                         reads=[KT, QT], writes=[pl])
                    b.op('pe', lambda e: e.matmul(pl[:], MT[:, h, 0, :], OH[:, j, :], start=False, stop=False), reads=[MT, OH], writes=[pl])
                    b.op('pe', lambda e: e.matmul(pl[:], MT[:, h, 1, :], OH[:, j + 1, :], start=False, stop=True), reads=[MT, OH], writes=[pl])

                def emit_rest(n):
                    j, hh = steps[n]
                    h = hp * 2 + hh
                    kv = h // 4
                    pl = self.ps[4 + n % 4]
                    mt = mts[j]
                    et = ee[n % 6]
                    b.op('act', lambda e: e.activation(out=et[:], in_=pl[:], func=AF.Exp, bias=self.b31t[:, h:h + 1], scale=1.0),
                         reads=[pl, self.b31t], writes=[et])
                    pt_ = pp_[n % 6]
                    b.op('dve', lambda e: e.tensor_tensor(out=pt_[:], in0=et[:], in1=mt[:], op=ALU.mult), reads=[et, mt], writes=[pt_])
                    b.op('pe', lambda e: e.matmul(acc[hh * 2][:], VT[:, j, kv * 128:(kv + 1) * 128], pt_[:], start=(j == 0), stop=(j == 63)),
                         reads=[VT, pt_], writes=[acc[hh * 2]])
                    b.op('pe', lambda e: e.matmul(acc[hh * 2 + 1][:], onesb[:], pt_[:], start=(j == 0), stop=(j == 63)),
                         reads=[onesb, pt_], writes=[acc[hh * 2 + 1]])

                emit_logits(0)
                emit_logits(1)
                for n in range(len(steps)):
                    if n + 2 < len(steps):
                        emit_logits(n + 2)
                    emit_rest(n)
                for hh in range(2):
                    h = hp * 2 + hh
                    rcp, y = rc[hh], yo[hh]
                    b.op('dve', lambda e: e.reciprocal(out=rcp[:], in_=acc[hh * 2 + 1][:]), reads=[acc[hh * 2 + 1]], writes=[rcp])
                    b.op('dve', lambda e: e.tensor_tensor(out=y[:], in0=acc[hh * 2][:], in1=rcp[:], op=ALU.mult), reads=[acc[hh * 2], rcp], writes=[y])
                    b.dma('pool', self.yaT[h * 128:(h + 1) * 128, tsl], y[:], reads=[y], writes=[d_])
                    if l == 0 and 'dbg_ya' in self.dbg:
                        b.dma('pool', self.dbg['dbg_ya'][h * 128:(h + 1) * 128, tsl], y[:], reads=[y], writes=[d_])

    def phase_mem(self, l):
        b, ar = self.b, self.ar
        ar.reset()
        mx = ar.take('mx', KC * NMEM, (128, KC, NMEM))
        mh = ar.take('mh', KC * NMEM, (128, KC, NMEM))
        wb = [ar.take('mwb%d' % i, 4096) for i in range(2)]
        mkT = ar.take('mkT', 8 * NMEM // 2, (128, 8, NMEM), dtype=BF16)
        mv = ar.take('mv', 2 * 1024 // 2, (128, 2, 1024), dtype=BF16)
        qm = ar.take('qm', 8 * TT // 2, (128, 8, TT), dtype=BF16)
        pe_ = [ar.take('pe%d' % i, TT // 2, dtype=BF16) for i in range(4)]
        onesb = ar.take('onesb', 64, dtype=BF16)
        rc = [ar.take('rc%d' % i, TT) for i in range(2)]
        yo = [ar.take('yo%d' % i, TT) for i in range(2)]
        self.rs = ar.take('rs', TT)
        self.rstd = ar.take('rstd', TT)
        d_ = b.dram('mem_d')
        b.op('dve', lambda e: e.tensor_copy(out=onesb[:], in_=self.ones0[:]), reads=[self.ones0], writes=[onesb])
        b.dma('pool', mx[:], self.memT.rearrange("(k p) m -> p k m", p=128), reads=[d_], writes=[mx])
        self.rmsnorm_T(mx, mh, self.v16[:, 48:64], self.v16, n=NMEM)
        W4 = self.W4[l]
        for ti in range(8):
            w = wb[ti % 2]
            self.wload(w, W4, R_MKV + ti * 128)
            wv = w[:].rearrange("p (k c) -> p k c", k=KC)
            if ti < 4:
                for h2 in range(2):
                    ch = ti * 2 + h2
                    pp = self.ps[ch % 2]
                    for k in range(KC):
                        b.op('pe', lambda e, k=k, h2=h2: e.matmul(pp[:, 0:NMEM], r32(wv[:, k, h2 * 128:(h2 + 1) * 128]), r32(mh[:, k, :]),
                                                                  start=(k == 0), stop=(k == KC - 1)), reads=[w, mh], writes=[pp])
                    b.op('act', lambda e, ch=ch: e.copy(out=mkT[:, ch, :], in_=pp[:, 0:NMEM]), reads=[pp], writes=[mkT])
            else:
                f0 = (ti - 4) * 256
                for mt in range(2):
                    pp = self.ps[2 + mt]
                    for k in range(KC):
                        b.op('pe', lambda e, k=k, mt=mt: e.matmul(pp[:, 0:256], r32(mh[:, k, mt * 128:(mt + 1) * 128]), r32(wv[:, k, :]),
                                                                  start=(k == 0), stop=(k == KC - 1)), reads=[w, mh], writes=[pp])
                    b.op('act', lambda e, mt=mt, f0=f0: e.copy(out=mv[:, mt, f0:f0 + 256], in_=pp[:, 0:256]), reads=[pp], writes=[mv])
        it = 0
        for tt in range(NTT):
            tsl = slice(tt * TT, (tt + 1) * TT)
            b.dma('pool', qm[:], self.qmT.rearrange("(c d) t -> d c t", d=128)[:, :, tsl], reads=[d_], writes=[qm])
            for h in range(4):
                pts = []
                for mt in range(2):
                    pl = self.ps[4 + mt]
                    for dc in range(2):
                        b.op('pe', lambda e, h=h, mt=mt, dc=dc: e.matmul(pl[:], mkT[:, h * 2 + dc, mt * 128:(mt + 1) * 128], qm[:, h * 2 + dc, :],
                                                                         start=(dc == 0), stop=(dc == 1)), reads=[mkT, qm], writes=[pl])
                    pt_ = pe_[it % 4]
                    it += 1
                    b.op('act', lambda e, pt_=pt_: e.activation(out=pt_[:], in_=pl[:], func=AF.Exp), reads=[pl], writes=[pt_])
                    pts.append(pt_)
                pden = self.ps[6]
                for mt in range(2):
                    b.op('pe', lambda e, mt=mt: e.matmul(pden[:], onesb[:], pts[mt][:], start=(mt == 0), stop=(mt == 1)), reads=[onesb, pts[mt]], writes=[pden])
                rcp = rc[h % 2]
                b.op('dve', lambda e: e.reciprocal(out=rcp[:], in_=pden[:]), reads=[pden], writes=[rcp])
                for dc in range(2):
                    po = self.ps[dc]
                    for mt in range(2):
                        b.op('pe', lambda e, h=h, mt=mt, dc=dc: e.matmul(po[:], mv[:, mt, h * 256 + dc * 128:h * 256 + (dc + 1) * 128], pts[mt][:],
                                                                         start=(mt == 0), stop=(mt == 1)), reads=[mv, pts[mt]], writes=[po])
                    y = yo[dc]
                    b.op('dve', lambda e: e.tensor_tensor(out=y[:], in0=po[:], in1=rcp[:], op=ALU.mult), reads=[po, rcp], writes=[y])
                    r0 = h * 256 + dc * 128
                    b.dma('pool', self.ycT[r0:r0 + 128, tsl], y[:], reads=[y], writes=[d_])
                    if l == 0 and 'dbg_yc' in self.dbg:
                        b.dma('pool', self.dbg['dbg_yc'][r0:r0 + 128, tsl], y[:], reads=[y], writes=[d_])

    def phase_merge(self, l):
        b = self.b
        self.ffn_bufs()
        xt, hT = self.xt, self.hT
        d_ = b.dram('mrg_d')
        xd = b.dram('mrg_x')
        W4 = self.W4[l]
        ysrc = (self.yaT, self.ybT, self.ycT)
        gts = self.stage
        for tt in range(NTT):
            tsl = slice(tt * TT, (tt + 1) * TT)
            b.dma('pool', xt[:], self.xview(self.xres, tt), reads=[xd], writes=[xt])
            for j in range(3):
                for half in range(2):
                    wdb = self.wd[2 * j + half]
                    b.dma('pool', r32(wdb[:].rearrange("p (c t) -> p c t", c=4)),
                          r32(ysrc[j][half * 512:(half + 1) * 512, tsl].rearrange("(c p) t -> p c t", p=128)), reads=[d_], writes=[wdb])
            for dq in range(4):
                wbs = []
                for j in range(3):
                    wb = self.wb[self.wbi % 3]
                    self.wbi += 1
                    self.wload(wb, W4, R_WBR + (j * 4 + dq) * 128)
                    wbs.append(wb)
                for dd in range(4):
                    dch = dq * 4 + dd
                    for j in range(3):
                        pb = self.ps[j]
                        wv = wbs[j][:].rearrange("p (c d) -> p c d", c=8)
                        for c in range(8):
                            ybuf = self.wd[2 * j + c // 4]
                            b.op('pe', lambda e, c=c, dd=dd, wv=wv, ybuf=ybuf: e.matmul(pb[:], r32(wv[:, c, dd * 128:(dd + 1) * 128]),
                                                                                        r32(ybuf[:, (c % 4) * 512:(c % 4 + 1) * 512]),
                                                                                        start=(c == 0), stop=(c == 7)), reads=[wbs[j], ybuf], writes=[pb])
                    for j in range(3):
                        g = gts[j]
                        b.dma('pool', g[:], self.gtT[j * 2048 + dch * 128:j * 2048 + (dch + 1) * 128, tsl], reads=[d_], writes=[g])
                    b.op('dve', lambda e: e.tensor_tensor(out=self.rs[:], in0=self.ps[0][:], in1=gts[0][:], op=ALU.mult), reads=[self.ps[0], gts[0]], writes=[self.rs])
                    b.op('dve', lambda e: e.tensor_tensor(out=self.sg[0][:], in0=self.ps[1][:], in1=gts[1][:], op=ALU.mult), reads=[self.ps[1], gts[1]], writes=[self.sg[0]])
                    b.op('dve', lambda e: e.tensor_tensor(out=self.sg[1][:], in0=self.ps[2][:], in1=gts[2][:], op=ALU.mult), reads=[self.ps[2], gts[2]], writes=[self.sg[1]])
                    b.op('dve', lambda e: e.tensor_tensor(out=self.rs[:], in0=self.rs[:], in1=self.sg[0][:], op=ALU.add), reads=[self.rs, self.sg[0]], writes=[self.rs])
                    b.op('dve', lambda e, dch=dch: e.tensor_tensor(out=r32(hT[:, dch, :]), in0=self.rs[:], in1=self.sg[1][:], op=ALU.add),
                         reads=[self.rs, self.sg[1]], writes=[hT])
            for ti in range(8):
                wb = self.wb[self.wbi % 3]
                self.wbi += 1
                self.wload(wb, W4, R_OUT + ti * 128)
                wv = wb[:].rearrange("p (k c) -> p k c", k=KC)
                for h2 in range(2):
                    dch = ti * 2 + h2
                    po = self.ps[4 + dch % 2]
                    for k in range(KC):
                        b.op('pe', lambda e, k=k, h2=h2: e.matmul(po[:], r32(wv[:, k, h2 * 128:(h2 + 1) * 128]), r32(hT[:, k, :]),
                                                                  start=(k == 0), stop=(k == KC - 1)), reads=[wb, hT], writes=[po])
                    b.op('dve', lambda e, dch=dch: e.tensor_tensor(out=xt[:, dch, :], in0=po[:], in1=xt[:, dch, :], op=ALU.add), reads=[po, xt], writes=[xt])
            if l == 0 and 'dbg_x2' in self.dbg:
                b.dma('pool', self.xview(self.dbg['dbg_x2'], tt), xt[:], reads=[xt], writes=[xd])
            self.rmsnorm_T(xt, hT, self.v16[:, 32:48], self.v16)
            self.ffn_tile(xt, hT, l, 2)
            b.dma('pool', self.xview(self.xres, tt), xt[:], reads=[xt], writes=[xd])
            if self.xout is not None:
                b.dma('pool', self.xview(self.xout, tt), xt[:], reads=[xt], writes=[xd])

    def final_norm(self):
        b = self.b
        self.ffn_bufs()
        xd = b.dram('fin_x')
        for tt in range(NTT):
            b.dma('pool', self.xt[:], self.xview(self.xres, tt), reads=[xd], writes=[self.xt])
            self.rmsnorm_T(self.xt, self.hT, self.fng[:], self.fng)
            b.dma('pool', self.xview(self.yout, tt), self.hT[:], reads=[self.hT], writes=[xd])


def _t5_bucket(d):
    d = np.maximum(d, 0)
    df = np.maximum(d, 1).astype(np.float32)
    large = 16 + (np.log(df / 16) / math.log(8) * 16).astype(np.int32)
    large = np.minimum(large, 31)
    return np.where(d < 16, d, large)


def _tile16(w, ncol_tiles):
    return np.ascontiguousarray(w.reshape(16, 128, ncol_tiles, 256).transpose(2, 1, 0, 3)).reshape(ncol_tiles * 128, 4096)


def _prep_weights(inp, layers):
    nl = len(layers)
    w4 = np.zeros((nl, R4, 4096), np.float32)
    w2 = np.zeros((nl, R2, 2048), np.float32)
    for li, l in enumerate(layers):
        for (r0, key) in ((R_GU1, 'w_ff1_gu'), (R_GU2, 'w_ff2_gu')):
            wg = inp[key][l]
            w4[li, r0:r0 + 5504] = np.ascontiguousarray(wg.reshape(16, 128, 2, FC, 128).transpose(3, 1, 0, 2, 4)).reshape(5504, 4096)
        w2[li, 0:5504] = inp['w_ff1_down'][l]
        w2[li, 5504:] = inp['w_ff2_down'][l]
        wi = inp['w_in'][l]
        wr = np.zeros((2048, NCH * 128), np.float32)
        wr[:, 0:1792] = wi[:, 0:1792]
        wr[:, CH_KI * 128:CH_KI * 128 + 64] = wi[:, 1792:1856]
        wr[:, CH_WI * 128:CH_WI * 128 + 4] = wi[:, 1856:1860]
        wr[:, CH_XL * 128:] = wi[:, 1860:]
        w4[li, R_WIN:R_WIN + 5632] = _tile16(wr, NCH // 2)
        w4[li, R_MKV:R_MKV + 1024] = _tile16(inp['w_mem_kv'][l], 8)
        wb = inp['w_branch'][l]
        w4[li, R_WBR:R_WBR + 1536] = np.ascontiguousarray(wb.reshape(3, 8, 128, 4, 512).transpose(0, 3, 2, 1, 4)).reshape(1536, 4096)
        w4[li, R_OUT:R_OUT + 1024] = _tile16(inp['w_out'][l], 8)
    return w4, w2


def _shard(w, secs, c):
    return np.concatenate([w[:, r0 + c * (rn // 8): r0 + (c + 1) * (rn // 8)] for (r0, rn) in secs], axis=1)


_PROG_CACHE = {}


def _get_prog(nl, debug=()):
    key = (nl, tuple(sorted(debug)))
    if key not in _PROG_CACHE:
        _PROG_CACHE[key] = Prog(nl, debug)
    return _PROG_CACHE[key]


def _pk(v):
    return np.ascontiguousarray(np.asarray(v, np.float32).reshape(-1, 128).T)


def make_in_maps(inp, layers=None, x_shards=None):
    inp = {k: np.asarray(v) for k, v in inp.items()}
    if layers is None:
        layers = list(range(DEPTH))
    nl = len(layers)
    w4, w2 = _prep_weights(inp, layers)
    x = inp['x'].reshape(2 * S, D)
    vec16 = np.stack([np.concatenate([_pk(inp['norm_ff1'][l]), _pk(inp['norm_mix'][l]), _pk(inp['norm_ff2'][l]), _pk(inp['mem_norm'][l])], axis=1)
                      for l in layers])
    lrup = np.stack([np.concatenate([_pk(inp['conv_w'][l][j]) for j in range(4)] + [_pk(inp['conv_b'][l]), _pk(inp['b_a'][l]), _pk(inp['b_i'][l]),
                                                                                       _pk(inp['lam'][l])], axis=1) for l in layers])
    wai = np.stack([np.concatenate([np.ascontiguousarray(inp['w_a'][l].transpose(1, 0, 2)).reshape(128, 1024),
                                    np.ascontiguousarray(inp['w_i'][l].transpose(1, 0, 2)).reshape(128, 1024)], axis=1) for l in layers])
    lnp = np.stack([np.stack([inp['idx_ln_g'][l], inp['idx_ln_b'][l]], axis=1) for l in layers])
    rb = inp['rel_bias']
    u = np.arange(256)[:, None]
    s_ = np.arange(128)[None, :]
    bk = _t5_bucket(u - s_)
    mt = rb[bk]
    mtab = np.ascontiguousarray(mt.reshape(2, 128, 128, 8).transpose(1, 3, 0, 2)).reshape(128, 8 * 2 * 128)
    b31 = np.ascontiguousarray(rb[31:32, :])
    maps = []
    for c in range(NCORES):
        bi, cl = c // 4, c % 4
        pos = (cl * NT + np.arange(NT)).astype(np.float32)
        sel = np.zeros((1, 8), np.float32)
        if cl > 0:
            sel[0, cl - 1] = 1.0
        sel[0, 4:4 + cl] = 1.0
        m = dict(
            xin=(np.ascontiguousarray(x[c * NT:(c + 1) * NT].T) if x_shards is None else np.ascontiguousarray(x_shards[c])),
            memT=np.ascontiguousarray(inp['mem'][bi].T),
            w4s=np.ascontiguousarray(_shard(w4, SEC4, c)),
            w2s=np.ascontiguousarray(_shard(w2, SEC2, c)),
            vec16=vec16.astype(np.float32), fnorm=_pk(inp['final_norm']), lrup=lrup.astype(np.float32), wai=wai.astype(np.float32),
            lnp=lnp.astype(np.float32), mtab=mtab.astype(np.float32), b31=b31.astype(np.float32),
            qposr=pos[None, :].copy(), qposc=np.ascontiguousarray(pos.reshape(16, 128).T), sel=sel,
        )
        maps.append(m)
    return maps


def kernel(**inputs):
    prog = _get_prog(DEPTH)
    maps = make_in_maps(inputs)
    res = run_bass_kernel_spmd(prog.nc, maps, core_ids=list(range(NCORES)))
    out = np.concatenate([np.asarray(r["yout"]).T for r in res.results], axis=0)
    return out.reshape(2, S, D).astype(np.float32)
```
